# Optimizing a Trainium2 kernel written in Bass

```python
import jax, jax.numpy as jnp
from jax import lax
import numpy as np

D_MODEL = 1024
BATCH = 16
SEQ = 4096
DEPTH = 2

N_BRANCH = 4
MIX = D_MODEL // 4
ML_HEADS = 4
ML_DH = MIX // ML_HEADS
ML_CHUNK = 64
GLA_HEADS = 4
GLA_DH = MIX // GLA_HEADS
GLA_RANK = 16
GLA_TAU = 16.0
GLA_CHUNK = 16
CONF_WIDTH = 31
SC_WIDTH = 3
FFN_WIDTH = 3
FFN_HIDDEN = ((8 * D_MODEL // 3 + 255) // 256) * 256
EPS = 1e-6

IN_SPLITS = (
    ("ml_q", MIX), ("ml_k", MIX), ("ml_v", MIX), ("ml_o", MIX),
    ("ml_i", ML_HEADS), ("ml_f", ML_HEADS),
    ("gla_q", MIX), ("gla_k", MIX), ("gla_v", MIX), ("gla_r", MIX), ("gla_a", GLA_RANK),
    ("conf", 2 * MIX),
    ("sc", 3 * MIX),
    ("gate", N_BRANCH * D_MODEL),
)
IN_WIDTH = sum(w for _, w in IN_SPLITS)

kernel_name = "hybrid_gated_mlstm_gla_conformer_shortconv_block"


def rms_norm(x, g):
    xf = x.astype(jnp.float32)
    y = xf * lax.rsqrt(jnp.mean(xf * xf, axis=-1, keepdims=True) + EPS)
    return (y * g.astype(jnp.float32)).astype(x.dtype)


def layer_norm(x, g, b):
    xf = x.astype(jnp.float32)
    mu = jnp.mean(xf, axis=-1, keepdims=True)
    xc = xf - mu
    y = xc * lax.rsqrt(jnp.mean(xc * xc, axis=-1, keepdims=True) + EPS)
    return (y * g.astype(jnp.float32) + b.astype(jnp.float32)).astype(x.dtype)


def head_rms_norm(t, g, n_heads):
    Bn, S, H, d = t.shape
    tf = t.astype(jnp.float32)
    y = tf * lax.rsqrt(jnp.mean(tf * tf, axis=-1, keepdims=True) + EPS)
    y = y * g.astype(jnp.float32).reshape(n_heads, d)
    return y.reshape(Bn, S, H * d)


def causal_dwconv(x, w):
    K, C = w.shape
    return lax.conv_general_dilated(
        x, w[:, None, :].astype(x.dtype), window_strides=(1,), padding=[(K - 1, 0)],
        dimension_numbers=("NWC", "WIO", "NWC"), feature_group_count=C)


def split_columns(proj):
    out = {}
    off = 0
    for name, width in IN_SPLITS:
        out[name] = proj[..., off:off + width]
        off += width
    return out


def split_heads(t, n_heads):
    return t.reshape(t.shape[:2] + (n_heads, t.shape[-1] // n_heads))


def to_chunks(t, L):
    Bn, S, H = t.shape[:3]
    t = t.reshape((Bn, S // L, L, H) + t.shape[3:])
    return jnp.moveaxis(t, (1, 3), (0, 2))


def from_chunks(t):
    NC, Bn, H, L = t.shape[:4]
    t = jnp.moveaxis(t, (0, 2), (1, 3))
    return t.reshape((Bn, NC * L, H) + t.shape[4:])


def mlstm_chunked(q, k, v, i_pre, f_pre):
    f32 = jnp.float32
    Bn, S, H, Dk = q.shape
    Dv = v.shape[-1]
    L = ML_CHUNK
    xs = (to_chunks(q.astype(f32) * (Dk ** -0.5), L), to_chunks(k.astype(f32), L),
          to_chunks(v.astype(f32), L), to_chunks(i_pre.astype(f32), L),
          to_chunks(jax.nn.log_sigmoid(f_pre.astype(f32)), L))
    causal = jnp.tril(jnp.ones((L, L), dtype=bool))

    def step(carry, chunk):
        C, n, m = carry
        qc, kc, vc, ic, fc = chunk
        b = jnp.cumsum(fc, axis=-1)
        logD = jnp.where(causal, b[..., :, None] - b[..., None, :] + ic[..., None, :], -jnp.inf)
        g = b + m[..., None]
        mt = jnp.maximum(g, jnp.max(logD, axis=-1))
        w_inter = jnp.exp(g - mt)
        qk = jnp.einsum('bhtd,bhsd->bhts', qc, kc) * jnp.exp(logD - mt[..., None])
        num = (w_inter[..., None] * jnp.einsum('bhtd,bhde->bhte', qc, C)
               + jnp.einsum('bhts,bhse->bhte', qk, vc))
        den = w_inter * jnp.einsum('bhtd,bhd->bht', qc, n) + jnp.sum(qk, axis=-1)
        h = num / jnp.maximum(jnp.abs(den), jnp.exp(-mt))[..., None]
        bL = b[..., -1]
        a = bL[..., None] - b + ic
        m_new = jnp.maximum(bL + m, jnp.max(a, axis=-1))
        w = jnp.exp(a - m_new[..., None])
        decay = jnp.exp(bL + m - m_new)
        C = decay[..., None, None] * C + jnp.einsum('bhs,bhsd,bhse->bhde', w, kc, vc)
        n = decay[..., None] * n + jnp.einsum('bhs,bhsd->bhd', w, kc)
        return (C, n, m_new), h

    init = (jnp.zeros((Bn, H, Dk, Dv), f32), jnp.zeros((Bn, H, Dk), f32), jnp.zeros((Bn, H), f32))
    _, h = lax.scan(step, init, xs)
    return from_chunks(h)


def gla_chunked(q, k, v, log_a):
    f32 = jnp.float32
    Bn, S, H, Dk = q.shape
    Dv = v.shape[-1]
    L = GLA_CHUNK
    xs = (to_chunks(q.astype(f32) * (Dk ** -0.5), L), to_chunks(k.astype(f32), L),
          to_chunks(v.astype(f32), L), to_chunks(log_a.astype(f32), L))
    causal = jnp.tril(jnp.ones((L, L), dtype=bool))

    def step(state, chunk):
        qc, kc, vc, lac = chunk
        b = jnp.cumsum(lac, axis=2)
        bL = b[:, :, -1:, :]
        q_dec = qc * jnp.exp(b)
        attn = jnp.where(causal, jnp.einsum('bhtd,bhsd->bhts', q_dec, kc * jnp.exp(-b)), 0.0)
        o = jnp.einsum('bhtd,bhde->bhte', q_dec, state) + jnp.einsum('bhts,bhse->bhte', attn, vc)
        k_dec = kc * jnp.exp(bL - b)
        state = jnp.exp(bL[:, :, 0, :])[..., None] * state + jnp.einsum('bhsd,bhse->bhde', k_dec, vc)
        return state, o

    _, o = lax.scan(step, jnp.zeros((Bn, H, Dk, Dv), f32), xs)
    return from_chunks(o)


def token_mixer(h, w_in, ml_i_bias, ml_f_bias, ml_norm_g, w_ml_out, gla_w_a2, gla_a_bias,
                gla_norm_g, w_gla_out, conf_dw_w, conf_dw_b, conf_ln_g, conf_ln_b, w_conf_out,
                conf_out_b, sc_dw_w, w_sc_out, merge_gate_b, w_o):
    Bn, S, _ = h.shape
    p = split_columns(h @ w_in)
    ml = mlstm_chunked(split_heads(p["ml_q"], ML_HEADS), split_heads(p["ml_k"], ML_HEADS),
                       split_heads(p["ml_v"], ML_HEADS), p["ml_i"] + ml_i_bias, p["ml_f"] + ml_f_bias)
    ml = head_rms_norm(ml, ml_norm_g, ML_HEADS).astype(h.dtype)
    y_ml = (ml * jax.nn.sigmoid(p["ml_o"])) @ w_ml_out
    log_a = jax.nn.log_sigmoid((p["gla_a"] @ gla_w_a2 + gla_a_bias).astype(jnp.float32)) / GLA_TAU
    gl = gla_chunked(split_heads(p["gla_q"], GLA_HEADS), split_heads(p["gla_k"], GLA_HEADS),
                     split_heads(p["gla_v"], GLA_HEADS), split_heads(log_a, GLA_HEADS))
    gl = head_rms_norm(gl, gla_norm_g, GLA_HEADS).astype(h.dtype)
    y_gla = (gl * jax.nn.silu(p["gla_r"])) @ w_gla_out
    a, g = jnp.split(p["conf"], 2, axis=-1)
    u = causal_dwconv(a * jax.nn.sigmoid(g), conf_dw_w) + conf_dw_b
    u = jax.nn.silu(layer_norm(u, conf_ln_g, conf_ln_b))
    y_conf = u @ w_conf_out + conf_out_b
    bg, cg, xv = jnp.split(p["sc"], 3, axis=-1)
    y_sc = (bg * causal_dwconv(cg * xv, sc_dw_w)) @ w_sc_out
    gates = jax.nn.sigmoid(p["gate"] + merge_gate_b).reshape(Bn, S, N_BRANCH, D_MODEL)
    merged = (gates[:, :, 0] * y_ml + gates[:, :, 1] * y_gla
              + gates[:, :, 2] * y_conf + gates[:, :, 3] * y_sc)
    return merged @ w_o


def conv_ffn(h, w_up, dw_w, w_down):
    u = causal_dwconv(h @ w_up, dw_w)
    a, v = jnp.split(u, 2, axis=-1)
    return (jax.nn.silu(a) * v) @ w_down


def setup_inputs(seed: int = 0) -> dict:
    key = jax.random.key(seed)
    keys = list(jax.random.split(key, 40))

    def nrm(shape, scale):
        return jax.random.normal(keys.pop(), shape, jnp.float32) * scale

    L = DEPTH
    D = D_MODEL
    return {
        "x": nrm((BATCH, SEQ, D), 1.0),
        "c": nrm((BATCH, D), 1.0),
        "ada_w": nrm((L, D, 6 * D), 0.5 * D ** -0.5),
        "ada_b": nrm((L, 6 * D), 0.02),
        "tm_pre_g": 1.0 + nrm((L, D), 0.05),
        "tm_post_g": 1.0 + nrm((L, D), 0.05),
        "cm_pre_g": 1.0 + nrm((L, D), 0.05),
        "cm_post_g": 1.0 + nrm((L, D), 0.05),
        "w_in": nrm((L, D, IN_WIDTH), D ** -0.5),
        "ml_i_bias": nrm((L, ML_HEADS), 0.1),
        "ml_f_bias": 3.0 + nrm((L, ML_HEADS), 0.5),
        "ml_norm_g": 1.0 + nrm((L, MIX), 0.05),
        "w_ml_out": nrm((L, MIX, D), MIX ** -0.5),
        "gla_w_a2": nrm((L, GLA_RANK, MIX), GLA_RANK ** -0.5),
        "gla_a_bias": nrm((L, MIX), 0.1),
        "gla_norm_g": 1.0 + nrm((L, MIX), 0.05),
        "w_gla_out": nrm((L, MIX, D), MIX ** -0.5),
        "conf_dw_w": nrm((L, CONF_WIDTH, MIX), CONF_WIDTH ** -0.5),
        "conf_dw_b": nrm((L, MIX), 0.02),
        "conf_ln_g": 1.0 + nrm((L, MIX), 0.05),
        "conf_ln_b": nrm((L, MIX), 0.02),
        "w_conf_out": nrm((L, MIX, D), MIX ** -0.5),
        "conf_out_b": nrm((L, D), 0.02),
        "sc_dw_w": nrm((L, SC_WIDTH, MIX), SC_WIDTH ** -0.5),
        "w_sc_out": nrm((L, MIX, D), MIX ** -0.5),
        "merge_gate_b": nrm((L, N_BRANCH * D), 0.02),
        "w_o": nrm((L, D, D), D ** -0.5),
        "ffn_w_up": nrm((L, D, 2 * FFN_HIDDEN), D ** -0.5),
        "ffn_dw_w": nrm((L, FFN_WIDTH, 2 * FFN_HIDDEN), FFN_WIDTH ** -0.5),
        "ffn_w_down": nrm((L, FFN_HIDDEN, D), FFN_HIDDEN ** -0.5),
    }


def reference(x, c, ada_w, ada_b, tm_pre_g, tm_post_g, cm_pre_g, cm_post_g, w_in, ml_i_bias,
              ml_f_bias, ml_norm_g, w_ml_out, gla_w_a2, gla_a_bias, gla_norm_g, w_gla_out,
              conf_dw_w, conf_dw_b, conf_ln_g, conf_ln_b, w_conf_out, conf_out_b, sc_dw_w,
              w_sc_out, merge_gate_b, w_o, ffn_w_up, ffn_dw_w, ffn_w_down):
    c_act = jax.nn.silu(c)
    for l in range(DEPTH):
        mod = (c_act @ ada_w[l] + ada_b[l])[:, None, :]
        sh_t, sc_t, g_t, sh_c, sc_c, g_c = jnp.split(mod, 6, axis=-1)
        h = rms_norm(x, tm_pre_g[l]) * (1.0 + sc_t) + sh_t
        y = token_mixer(h, w_in[l], ml_i_bias[l], ml_f_bias[l], ml_norm_g[l], w_ml_out[l],
                        gla_w_a2[l], gla_a_bias[l], gla_norm_g[l], w_gla_out[l], conf_dw_w[l],
                        conf_dw_b[l], conf_ln_g[l], conf_ln_b[l], w_conf_out[l], conf_out_b[l],
                        sc_dw_w[l], w_sc_out[l], merge_gate_b[l], w_o[l])
        x = x + g_t * rms_norm(y, tm_post_g[l])
        h = rms_norm(x, cm_pre_g[l]) * (1.0 + sc_c) + sh_c
        y = conv_ffn(h, ffn_w_up[l], ffn_dw_w[l], ffn_w_down[l])
        x = x + g_c * rms_norm(y, cm_post_g[l])
    return x
```

```python
import contextlib
import numpy as np
import concourse.bass as bass
import concourse.mybir as mybir
from concourse.bass_utils import run_bass_kernel_spmd

F32 = mybir.dt.float32
BF16 = mybir.dt.bfloat16
AF = mybir.ActivationFunctionType
ALU = mybir.AluOpType
AX = mybir.AxisListType

D = 1024
MIX = 256
HID = 2816
INW = 7448
DEPTH = 2
SEQ = 4096
BATCH = 16
NCORES = 8
T = 256
NS = T // 128
NSLOT = 4
SLOTS_PER_LAYER = 39
EPS = 1e-6
PVL = 326
BRL = 776
GATE0 = 3352
DBG_LEVEL = 99


class Sem:
    def __init__(self, h):
        self.h = h
        self.count = 0


class Buf:
    __slots__ = ("w", "r", "excl")

    def __init__(self, excl=False):
        self.w = None
        self.r = {}
        self.excl = excl


class Eng:
    def __init__(self, e, sem, is_pe=False):
        self.e = e
        self.sem = sem
        self.seen = {}
        self.is_pe = is_pe
        self.hook = None
        self._inhook = False

    def _sync(self, reads, writes):
        need = {}
        for b in reads:
            if b.w is not None and need.get(b.w[0], 0) < b.w[1]:
                need[b.w[0]] = b.w[1]
        for b in writes:
            if b.w is not None and need.get(b.w[0], 0) < b.w[1]:
                need[b.w[0]] = b.w[1]
            for sm, v in b.r.items():
                if need.get(sm, 0) < v:
                    need[sm] = v
        for sm, v in need.items():
            if self.is_pe and sm is self.sem:
                continue
            if self.seen.get(sm, 0) < v:
                self.e.wait_ge(sm.h, v)
                self.seen[sm] = v

    @staticmethod
    def _mark(sm, reads, writes):
        v = sm.count
        for b in reads:
            if b.r.get(sm, 0) < v:
                b.r[sm] = v
        for b in writes:
            b.w = (sm, v)
            b.r = {}

    def op(self, fn, R=(), W=(), drain=False):
        if drain and self.sem.count > 0 and self.seen.get(self.sem, 0) < self.sem.count:
            self.e.wait_ge(self.sem.h, self.sem.count)
            self.seen[self.sem] = self.sem.count
        ex = [b for b in R if b.excl]
        if ex:
            W = list(W) + ex
        self._sync(R, W)
        inst = fn()
        self.sem.count += 1
        inst.then_inc(self.sem.h, 1)
        self._mark(self.sem, R, W)
        if self.hook is not None and not self._inhook:
            self._inhook = True
            self.hook()
            self._inhook = False

    def dma(self, out, in_, dsem, R=(), W=()):
        self._sync(R, W)
        inst = self.e.dma_start(out=out, in_=in_)
        dsem.count += 16
        inst.then_inc(dsem.h, 16)
        self._mark(dsem, R, W)

    def wait_all(self, sems):
        for sm in sems:
            if sm.count > 0 and self.seen.get(sm, 0) < sm.count:
                self.e.wait_ge(sm.h, sm.count)
                self.seen[sm] = sm.count


def build(nseq=2, ntiles=SEQ // T, depth=DEPTH, dbg=None):
    nc = bass.Bass("TRN2", target_bir_lowering=False)
    ntok = ntiles * T
    dt = nc.dram_tensor
    x_d = dt("x", [nseq, ntok, D], F32, kind="ExternalInput").ap()
    out_d = dt("out", [nseq, ntok, D], F32, kind="ExternalOutput").ap()
    cT_d = dt("cT", [128, 16], F32, kind="ExternalInput").ap()
    pvec_d = dt("pvec", [128, DEPTH * PVL], F32, kind="ExternalInput").ap()
    brow_d = dt("brow", [128, DEPTH * BRL], F32, kind="ExternalInput").ap()
    w2_d = dt("w2", [16, DEPTH * 256], F32, kind="ExternalInput").ap()
    const_d = dt("consts", [128, 1152], F32, kind="ExternalInput").ap()
    adaw_d = dt("ada_w", [DEPTH, D, 6 * D], F32, kind="ExternalInput").ap()
    win_d = dt("w_in", [DEPTH, D, INW], F32, kind="ExternalInput").ap()
    wml_d = dt("w_ml_out", [DEPTH, MIX, D], F32, kind="ExternalInput").ap()
    wgl_d = dt("w_gla_out", [DEPTH, MIX, D], F32, kind="ExternalInput").ap()
    wcf_d = dt("w_conf_out", [DEPTH, MIX, D], F32, kind="ExternalInput").ap()
    wsc_d = dt("w_sc_out", [DEPTH, MIX, D], F32, kind="ExternalInput").ap()
    wo_d = dt("w_o", [DEPTH, D, D], F32, kind="ExternalInput").ap()
    wup_d = dt("ffn_w_up", [DEPTH, D, 2 * HID], F32, kind="ExternalInput").ap()
    wdn_d = dt("ffn_w_down", [DEPTH, HID, D], F32, kind="ExternalInput").ap()
    scr_d = dt("wscratch", [DEPTH * SLOTS_PER_LAYER, 128, 4096], BF16, kind="Internal").ap()
    dbg_d = {}
    if dbg:
        for name, shape in dbg.items():
            dbg_d[name] = dt("dbg_" + name, list(shape), F32, kind="ExternalOutput").ap()

    es = contextlib.ExitStack()
    with es:
        def sb(name, shape, dtype=F32):
            return es.enter_context(nc.sbuf_tensor("sb_" + name, list(shape), dtype))

        def sem(name):
            return Sem(es.enter_context(nc.semaphore(name)))

        PE = Eng(nc.tensor, sem("s_pe"), is_pe=True)
        ACT = Eng(nc.scalar, sem("s_act"))
        DVE = Eng(nc.vector, sem("s_dve"))
        POOL = Eng(nc.gpsimd, sem("s_pool"))
        SP = Eng(nc.sync, sem("s_sp"))

        consts = sb("consts", [128, 1152])
        pvec = sb("pvec", [128, DEPTH * PVL])
        brow = sb("brow", [128, DEPTH * BRL])
        w2 = sb("w2", [16, DEPTH * 256])
        cact = sb("cact", [128, 16])
        cst_b = Buf()
        s_c = sem("s_const")
        for t_, d_ in ((consts, const_d), (pvec, pvec_d), (brow, brow_d), (w2, w2_d), (cact, cT_d)):
            SP.dma(t_[:, :], d_, s_c, W=[cst_b])
        ident = consts[:, 0:128]
        ones = consts[:, 128:256]
        Umat = consts[:, 256:384]
        U16 = consts[:, 384:512]
        SU16 = consts[:, 512:640]
        mask4 = consts[:, 640:1152].rearrange("p (h t) -> p h t", t=128)

        prep_sems = {}
        scr_b = {}

        def prep(l, s, out_ap, in_ap, grp):
            key = (l, grp)
            if key not in prep_sems:
                prep_sems[key] = sem("s_prep%d_%d" % key)
            sm = prep_sems[key]
            inst = nc.gpsimd.dma_start(out=out_ap, in_=in_ap)
            sm.count += 16
            inst.then_inc(sm.h, 16)
            scr_b.setdefault((l, s), Buf())

        slot_grp = {}

        def scr_view(l, s, pat, **kw):
            return scr_d[l * SLOTS_PER_LAYER + s].rearrange(pat, **kw)

        def k1024(src, c0, n):
            return src.rearrange("(kc p) c -> p kc c", p=128)[:, :, c0:c0 + n]

        for l in range(depth):
            wi = win_d[l]
            for s, (c0, n) in {0: (0, 512), 1: (512, 512), 3: (1032, 512), 4: (1544, 512),
                               5: (2072, 512), 6: (2584, 512), 7: (3096, 256)}.items():
                prep(l, s, scr_view(l, s, "p (kc c) -> p kc c", c=512)[:, :, 0:n], k1024(wi, c0, n), 0)
                slot_grp[(l, s)] = 0
            prep(l, 2, scr_view(l, 2, "p (kc c) -> p kc c", c=512)[:, :, 0:8], k1024(wi, 1024, 8), 0)
            prep(l, 2, scr_view(l, 2, "p (kc c) -> p kc c", c=512)[:, :, 8:24], k1024(wi, 2056, 16), 0)
            slot_grp[(l, 2)] = 0
            for j in range(8):
                for b in range(4):
                    prep(l, 8 + j, scr_view(l, 8 + j, "p (kc c) -> p kc c", c=512)[:, :, b * 128:(b + 1) * 128],
                         k1024(wi, GATE0 + b * 1024 + j * 128, 128), 1)
                slot_grp[(l, 8 + j)] = 1
            for s, (wa, wb) in {16: (wml_d, wgl_d), 17: (wcf_d, wsc_d)}.items():
                v = scr_view(l, s, "p (m c) -> p m c", c=1024)
                prep(l, s, v[:, 0:2, :], wa[l].rearrange("(kc p) c -> p kc c", p=128), 1)
                prep(l, s, v[:, 2:4, :], wb[l].rearrange("(kc p) c -> p kc c", p=128), 1)
                slot_grp[(l, s)] = 1
            for n in range(2):
                prep(l, 18 + n, scr_view(l, 18 + n, "p (kc c) -> p kc c", c=512), k1024(wo_d[l], n * 512, 512), 1)
                slot_grp[(l, 18 + n)] = 1
            for g in range(11):
                v = scr_view(l, 20 + g, "p (kc c) -> p kc c", c=512)
                prep(l, 20 + g, v[:, :, 0:256], k1024(wup_d[l], g * 256, 256), 2)
                prep(l, 20 + g, v[:, :, 256:512], k1024(wup_d[l], HID + g * 256, 256), 2)
                slot_grp[(l, 20 + g)] = 2
            for q in range(6):
                nk = 4 if q < 5 else 2
                v = scr_view(l, 31 + q, "p (m c) -> p m c", c=1024)
                prep(l, 31 + q, v[:, 0:nk, :], wdn_d[l].rearrange("(kc p) c -> p kc c", p=128)[:, 4 * q:4 * q + nk, :], 2)
                slot_grp[(l, 31 + q)] = 2
        for (l, s), b in scr_b.items():
            sm = prep_sems[(l, slot_grp[(l, s)])]
            b.w = (sm, sm.count)

        slots = [sb("wslot%d" % i, [128, 4096], BF16) for i in range(NSLOT)]
        slot_b = [Buf() for _ in range(NSLOT)]
        slot_s = [sem("s_slot%d" % i) for i in range(NSLOT)]
        slot_ctr = [0]

        def next_slot():
            i = slot_ctr[0] % NSLOT
            slot_ctr[0] += 1
            return i

        hold_t = [sb("whold%d" % i, [128, 4096], BF16) for i in range(2)]
        hold_b = [Buf(), Buf()]
        hold_s = [sem("s_hold%d" % i) for i in range(2)]

        def get_slot(l, s, hold=None):
            if hold is not None:
                SP.dma(hold_t[hold][:, :], scr_d[l * SLOTS_PER_LAYER + s], hold_s[hold], R=[scr_b[(l, s)]], W=[hold_b[hold]])
                return hold_t[hold], hold_b[hold]
            i = next_slot()
            dst = slots[i][:, :]
            src = scr_d[l * SLOTS_PER_LAYER + s]
            if s == 2:
                dst = dst.rearrange("p (kc c) -> p kc c", c=512)[:, :, 0:24]
                src = src.rearrange("p (kc c) -> p kc c", c=512)[:, :, 0:24]
            elif s == 7:
                dst = dst.rearrange("p (kc c) -> p kc c", c=512)[:, :, 0:256]
                src = src.rearrange("p (kc c) -> p kc c", c=512)[:, :, 0:256]
            elif s == 36:
                dst = dst[:, 0:2048]
                src = src[:, 0:2048]
            elif s >= 37:
                dst = dst[:, 0:3968]
                src = src[:, 0:3968]
            SP.dma(dst, src, slot_s[i], R=[scr_b[(l, s)]], W=[slot_b[i]])
            return slots[i], slot_b[i]

        ps_all = es.enter_context(nc.psum_tensor("ps", [128, 4096], F32))
        bank_b = [Buf(excl=True) for _ in range(8)]
        bank_ctr = [0]

        def bank():
            i = bank_ctr[0] % 8
            bank_ctr[0] += 1
            return ps_all[:, i * 512:(i + 1) * 512], bank_b[i]

        NSCR = 8
        scr_t = [sb("scr%d" % i, [128, 544]) for i in range(NSCR)]
        scr_bf = [Buf() for _ in range(NSCR)]
        scr_ctr = [0]

        def scratch():
            i = scr_ctr[0] % NSCR
            scr_ctr[0] += 1
            return scr_t[i], scr_bf[i]

        NSM = 16
        sm_t = [sb("sm%d" % i, [128, 16]) for i in range(NSM)]
        sm_bf = [Buf() for _ in range(NSM)]
        sm_ctr = [0]

        def small():
            i = sm_ctr[0] % NSM
            sm_ctr[0] += 1
            return sm_t[i], sm_bf[i]

        xt = sb("xt", [128, NS, D])
        xt_b = [Buf() for _ in range(NS)]
        s_xin = sem("s_xin")
        s_xout = sem("s_xout")
        xn = [sb("xn%d" % i, [128, D]) for i in range(2)]
        xn_b = [Buf(), Buf()]
        hT = sb("hT", [128, 8, T], BF16)
        hT_b = [[Buf(), Buf()] for _ in range(NS)]
        fq = {}
        for nm in ("mlq", "mlk", "gq", "gk"):
            fq[nm] = (sb(nm + "T", [128, 2, T], BF16), [Buf(), Buf()])
        tk = {}
        for nm in ("mlk", "mlv", "sigo", "glk", "gv", "sgr"):
            tk[nm] = (sb(nm + "_tok", [128, NS, 256], BF16), [Buf() for _ in range(NS)])
        iff = sb("iff", [128, NS, 8])
        iff_b = [Buf() for _ in range(NS)]
        gaT = sb("gaT", [16, T])
        gaT_b = Buf()
        zc = sb("zc", [128, 2, 30 + T], BF16)
        zc_b = [Buf(), Buf()]
        zs = sb("zs", [128, 2, 2 + T])
        zs_b = [Buf(), Buf()]
        bgs = sb("bgs", [128, 2, T], BF16)
        bgs_b = [Buf(), Buf()]
        cgs = sb("cgs", [128, 2, T])
        cgs_b = [Buf(), Buf()]
        uconv = sb("uconv", [128, 2, T])
        uconv_b = [Buf(), Buf()]
        zT = {}
        for nm in ("ml", "gla", "conf", "sc"):
            zT[nm] = (sb("z" + nm + "T", [128, 2, T], BF16), [Buf() for _ in range(max(NS, 2))])
        gates = [sb("gates%d" % i, [128, 4, T], BF16) for i in range(2)]
        gates_b = [Buf(), Buf()]
        mergedT = sb("mergedT", [128, 8, T], BF16)
        merged_b = [Buf() for _ in range(8)]
        gff = sb("gff", [128, 22, T], BF16)
        gff_b = [Buf() for _ in range(22)]
        halo = sb("halo", [128, DEPTH, 44, 2])
        halo_b = [[Buf() for _ in range(44)] for _ in range(DEPTH)]
        Cst = sb("Cst", [128, DEPTH, 2, 72])
        Cst_b = [[Buf(), Buf()] for _ in range(DEPTH)]
        Cbf = sb("Cbf", [128, DEPTH, 2, 72], BF16)
        Cbf_b = [[Buf(), Buf()] for _ in range(DEPTH)]
        Sst = sb("Sst", [128, DEPTH, 2, 64])
        Sst_b = [[Buf(), Buf()] for _ in range(DEPTH)]
        Sbf = sb("Sbf", [128, DEPTH, 2, 64], BF16)
        Sbf_b = [[Buf(), Buf()] for _ in range(DEPTH)]
        zch_b = [[Buf(), Buf()] for _ in range(DEPTH)]
        zch = sb("zch", [128, DEPTH, 2, 30], BF16)
        zsh = sb("zsh", [128, DEPTH, 2, 2])
        zsh_b = [[Buf(), Buf()] for _ in range(DEPTH)]
        Grow = sb("Grow", [128, DEPTH * 2, D])
        Grow_b = [Buf() for _ in range(DEPTH * 2)]
        modT = sb("modT", [128, DEPTH * 2 * 48])
        modT_b = Buf()
        modA = sb("modA", [128, DEPTH * 2 * 2 * 8])
        modA_b = Buf()
        mt = {}

        def mtile(name, shape, dtype=F32, n=None):
            if n is None:
                n = 2 if dtype == BF16 else 1
            if name not in mt:
                mt[name] = ([sb("%s_%d" % (name, i), shape, dtype) for i in range(n)], [Buf() for _ in range(n)], [0])
            tl, bl, c = mt[name]
            i = c[0] % n
            c[0] += 1
            return tl[i], bl[i]

        def dbg_dump(name, ap, b, idx=None):
            if dbg and name in dbg_d:
                dst = dbg_d[name] if idx is None else dbg_d[name][idx]
                sm_ = dbg_sems.setdefault(name, sem("s_dbg_" + name))
                POOL.dma(dst, ap, sm_, R=b)

        dbg_sems = {}
        dbg_state = []

        ACT.op(lambda: nc.scalar.activation(out=cact[:, :], in_=cact[:, :], func=AF.Silu), R=[cst_b], W=[cst_b])
        cact3 = cact[:, :].rearrange("p (kc i) -> p kc i", i=2)
        mod_ps, mod_pb = bank()
        for l in range(depth):
            for g in range(24):
                i = next_slot()
                sl32 = slots[i][:, :].bitcast(F32).rearrange("p (kc c) -> p kc c", c=256)
                SP.dma(sl32, adaw_d[l].rearrange("(kc p) c -> p kc c", p=128)[:, :, g * 256:(g + 1) * 256],
                       slot_s[i], W=[slot_b[i]])
                for fc in range(2):
                    col = (l * 48 + g * 2 + fc) * 2
                    for kc in range(8):
                        PE.op(lambda kc=kc, fc=fc, col=col, sl32=sl32: nc.tensor.matmul(
                            mod_ps[:, col:col + 2], lhsT=sl32[:, kc, fc * 128:(fc + 1) * 128], rhs=cact3[:, kc, :],
                            start=(kc == 0), stop=(kc == 7)), R=[slot_b[i], cst_b], W=[mod_pb])
        for l in range(depth):
            for i in range(2):
                o = (l * 2 + i) * 48
                src = mod_ps[:, l * 96:(l + 1) * 96].rearrange("p (v i) -> p v i", i=2)[:, :, i]
                DVE.op(lambda o=o, src=src, l=l: nc.vector.tensor_tensor(
                    out=modT[:, o:o + 48], in0=src, in1=pvec[:, l * PVL + 32:l * PVL + 80], op=ALU.add),
                    R=[mod_pb, cst_b], W=[modT_b])
        for l in range(depth):
            for i in range(2):
                o = (l * 2 + i) * 48
                for which in range(2):
                    a = ((l * 2 + i) * 2 + which) * 8
                    DVE.op(lambda o=o, a=a, which=which, l=l: nc.vector.scalar_tensor_tensor(
                        out=modA[:, a:a + 8], in0=modT[:, o + which * 24 + 8:o + which * 24 + 16], scalar=1.0,
                        in1=pvec[:, l * PVL + which * 16:l * PVL + which * 16 + 8], op0=ALU.add, op1=ALU.mult),
                        R=[modT_b, cst_b], W=[modA_b])

        diag_sems = []
        for l in range(depth):
            for c in range(2):
                s_diag = sem("s_diag%d_%d" % (l, c))
                diag_sems.append(s_diag)
                i_ = next_slot()
                for j in range(31):
                    DVE.op(lambda i_=i_, j=j, l=l, c=c: nc.vector.tensor_scalar(
                        out=slots[i_][:, j * 128:(j + 1) * 128], in0=ident,
                        scalar1=pvec[:, l * PVL + 80 + c * 31 + j:l * PVL + 80 + c * 31 + j + 1], scalar2=None, op0=ALU.mult),
                        R=[cst_b], W=[slot_b[i_]])
                scr_b[(l, 37 + c)] = Buf()
                SP.dma(scr_d[l * SLOTS_PER_LAYER + 37 + c][:, 0:3968], slots[i_][:, 0:3968], s_diag,
                       R=[slot_b[i_]], W=[scr_b[(l, 37 + c)]])
        def modA_ap(l, i, which):
            a = ((l * 2 + i) * 2 + which) * 8
            return modA[:, a:a + 8]

        def modB_ap(l, i, which):
            o = (l * 2 + i) * 48 + which * 24
            return modT[:, o:o + 8]

        def seq_init(i):
            for l in range(depth):
                for which in range(2):
                    o = (l * 2 + i) * 48 + which * 24 + 16
                    gv_t, gv_b = small()
                    DVE.op(lambda o=o, l=l, which=which, gv_t=gv_t: nc.vector.tensor_tensor(
                        out=gv_t[:, 0:8], in0=modT[:, o:o + 8],
                        in1=pvec[:, l * PVL + 8 + which * 16:l * PVL + 16 + which * 16], op=ALU.mult),
                        R=[modT_b, cst_b], W=[gv_b])
                    for half in range(2):
                        pb_ap, pb_b = bank()
                        for q in range(4):
                            kc = half * 4 + q
                            vb_t, vb_b = scratch()
                            DVE.op(lambda vb_t=vb_t, gv_t=gv_t, kc=kc: nc.vector.tensor_scalar(
                                out=vb_t[:, 0:128], in0=ones, scalar1=gv_t[:, kc:kc + 1], scalar2=None, op0=ALU.mult),
                                R=[cst_b, gv_b], W=[vb_b])
                            PE.op(lambda vb_t=vb_t, q=q, pb_ap=pb_ap: nc.tensor.matmul(
                                pb_ap[:, q * 128:(q + 1) * 128], lhsT=vb_t[:, 0:128], rhs=ident, start=True, stop=True),
                                R=[vb_b, cst_b], W=[pb_b])
                        ACT.op(lambda l=l, which=which, half=half, pb_ap=pb_ap: nc.scalar.copy(
                            out=Grow[:, l * 2 + which, half * 512:(half + 1) * 512], in_=pb_ap),
                            R=[pb_b], W=[Grow_b[l * 2 + which]])
            for l in range(depth):
                for p in range(2):
                    DVE.op(lambda l=l, p=p: nc.vector.memset(Cst[:, l, p, :], 0.0), W=[Cst_b[l][p]])
                    DVE.op(lambda l=l, p=p: nc.vector.memset(Cbf[:, l, p, :], 0.0), W=[Cbf_b[l][p]])
                    DVE.op(lambda l=l, p=p: nc.vector.memset(Sst[:, l, p, :], 0.0), W=[Sst_b[l][p]])
                    DVE.op(lambda l=l, p=p: nc.vector.memset(Sbf[:, l, p, :], 0.0), W=[Sbf_b[l][p]])
                    DVE.op(lambda l=l, p=p: nc.vector.memset(zch[:, l, p, :], 0.0), W=[zch_b[l][p]])
                    DVE.op(lambda l=l, p=p: nc.vector.memset(zsh[:, l, p, :], 0.0), W=[zsh_b[l][p]])
                for ch in range(44):
                    pass
                DVE.op(lambda l=l: nc.vector.memset(halo[:, l, :, :], 0.0), W=halo_b[l])

        def prenorm(l, i, which):
            A = modA_ap(l, i, which)
            Bv = modB_ap(l, i, which)
            for s in range(NS):
                xs = xt[:, s, :]
                xn_t, xn_bb = xn[s % 2], xn_b[s % 2]
                st_t, st_b = small()
                ACT.op(lambda xn_t=xn_t, xs=xs, st_t=st_t: nc.scalar.activation(
                    out=xn_t[:, :], in_=xs, func=AF.Square, accum_out=st_t[:, 0:1]), R=[xt_b[s]], W=[xn_bb, st_b])
                rsqrt(st_t, st_b, 0, 1, 2, 1.0 / D, 1)
                DVE.op(lambda xn_t=xn_t, xs=xs, st_t=st_t: nc.vector.tensor_scalar(
                    out=xn_t[:, :], in0=xs, scalar1=st_t[:, 2:3], scalar2=None, op0=ALU.mult),
                    R=[xt_b[s], st_b], W=[xn_bb])
                for half in range(2):
                    pb_ap, pb_b = bank()
                    for q in range(4):
                        kc = half * 4 + q
                        PE.op(lambda xn_t=xn_t, kc=kc, q=q, pb_ap=pb_ap: nc.tensor.transpose(
                            pb_ap[:, q * 128:(q + 1) * 128], xn_t[:, kc * 128:(kc + 1) * 128], ident),
                            R=[xn_bb, cst_b], W=[pb_b])
                    for q in range(4):
                        kc = half * 4 + q
                        if half == 0:
                            ACT.op(lambda kc=kc, q=q, pb_ap=pb_ap, s=s: nc.scalar.activation(
                                out=hT[:, kc, s * 128:(s + 1) * 128], in_=pb_ap[:, q * 128:(q + 1) * 128],
                                func=AF.Identity, scale=A[:, kc:kc + 1], bias=Bv[:, kc:kc + 1]),
                                R=[pb_b, modA_b, modT_b], W=[hT_b[s][half]])
                        else:
                            DVE.op(lambda kc=kc, q=q, pb_ap=pb_ap, s=s: nc.vector.tensor_scalar(
                                out=hT[:, kc, s * 128:(s + 1) * 128], in0=pb_ap[:, q * 128:(q + 1) * 128],
                                scalar1=A[:, kc:kc + 1], scalar2=Bv[:, kc:kc + 1], op0=ALU.mult, op1=ALU.add),
                                R=[pb_b, modA_b, modT_b], W=[hT_b[s][half]])

        epsc = sb("epsc", [128, 8])
        eps_b = Buf()
        DVE.op(lambda: nc.vector.memset(epsc[:, 0:1], EPS), W=[cst_b])
        DVE.op(lambda: nc.vector.memset(epsc[:, 1:2], 1.0), W=[cst_b])
        DVE.op(lambda: nc.vector.memset(epsc[:, 2:6], -0.5), W=[cst_b])
        mhalf = epsc[:, 2:6]

        def rsqrt(t_, b_, src, mid, dst, scale, n):
            DVE.op(lambda: nc.vector.tensor_scalar(out=t_[:, mid:mid + n], in0=t_[:, src:src + n], scalar1=scale, scalar2=EPS,
                                                   op0=ALU.mult, op1=ALU.add), R=[b_], W=[b_])
            POOL.op(lambda: nc.gpsimd.tensor_tensor(out=t_[:, dst:dst + n], in0=t_[:, mid:mid + n], in1=mhalf[:, 0:n], op=ALU.pow),
                    R=[b_, cst_b], W=[b_])
        eps_ap = epsc[:, 0:1]
        one_ap = epsc[:, 1:2]

        def fgroup(wt, wb, c0, m, pb_ap, pb_b, s_lo=0, s_hi=NS):
            w3 = wt[:, :].rearrange("p (kc c) -> p kc c", c=512)
            n0, n1 = s_lo * 128, s_hi * 128
            for kc in range(8):
                PE.op(lambda kc=kc: nc.tensor.matmul(pb_ap[0:m, n0:n1], lhsT=w3[:, kc, c0:c0 + m], rhs=hT[:, kc, n0:n1],
                                                     start=(kc == 0), stop=(kc == 7)),
                      R=[wb] + [b_ for s_ in range(s_lo, s_hi) for b_ in hT_b[s_]], W=[pb_b])

        def tgroup(wt, wb, c0, n, s, pb_ap, pb_b):
            w3 = wt[:, :].rearrange("p (kc c) -> p kc c", c=512)
            for kc in range(8):
                PE.op(lambda kc=kc: nc.tensor.matmul(pb_ap[:, 0:n], lhsT=hT[:, kc, s * 128:(s + 1) * 128],
                                                     rhs=w3[:, kc, c0:c0 + n], start=(kc == 0), stop=(kc == 7)),
                      R=[wb] + hT_b[s], W=[pb_b])

        def token_mixer(l, i):
            pv = l * PVL
            br = l * BRL
            prenorm(l, i, 0)
            first = dbg and not dbg_state
            if first:
                dbg_state.append(1)
                dbg_dump("hT", hT[:, :, :], [b_ for x_ in hT_b for b_ in x_])
            if DBG_LEVEL < 1.1:
                return
            wt, wb = get_slot(l, 0)
            for nm, cbase, scale in (("mlq", 0, 0.125), ("mlk", 256, 1.0)):
                for c in range(2):
                    pb_ap, pb_b = bank()
                    fgroup(wt, wb, cbase + c * 128, 128, pb_ap, pb_b)
                    ACT.op(lambda nm=nm, c=c, pb_ap=pb_ap, scale=scale: nc.scalar.activation(
                        out=fq[nm][0][:, c, :], in_=pb_ap[:, 0:T], func=AF.Copy, scale=scale), R=[pb_b], W=[fq[nm][1][c]])
            for s in range(NS):
                pb_ap, pb_b = bank()
                tgroup(wt, wb, 256, 256, s, pb_ap, pb_b)
                ACT.op(lambda s=s, pb_ap=pb_ap: nc.scalar.copy(out=tk["mlk"][0][:, s, :], in_=pb_ap[:, 0:256]),
                       R=[pb_b], W=[tk["mlk"][1][s]])
            if DBG_LEVEL < 1.2:
                return
            wt, wb = get_slot(l, 1)
            for s in range(NS):
                pb_ap, pb_b = bank()
                tgroup(wt, wb, 0, 512, s, pb_ap, pb_b)
                DVE.op(lambda s=s, pb_ap=pb_ap: nc.vector.tensor_copy(out=tk["mlv"][0][:, s, :], in_=pb_ap[:, 0:256]),
                       R=[pb_b], W=[tk["mlv"][1][s]])
                ACT.op(lambda s=s, pb_ap=pb_ap: nc.scalar.activation(
                    out=tk["sigo"][0][:, s, :], in_=pb_ap[:, 256:512], func=AF.Sigmoid), R=[pb_b], W=[tk["sigo"][1][s]])
            if DBG_LEVEL < 1.4:
                return
            wt, wb = get_slot(l, 2)
            for s in range(NS):
                pb_ap, pb_b = bank()
                tgroup(wt, wb, 0, 8, s, pb_ap, pb_b)
                DVE.op(lambda s=s, pb_ap=pb_ap: nc.vector.tensor_tensor(
                    out=iff[:, s, :], in0=pb_ap[:, 0:8], in1=brow[:, br:br + 8], op=ALU.add),
                    R=[pb_b, cst_b], W=[iff_b[s]])
            pb_ap, pb_b = bank()
            fgroup(wt, wb, 8, 16, pb_ap, pb_b)
            ACT.op(lambda pb_ap=pb_ap: nc.scalar.copy(out=gaT[:, :], in_=pb_ap[0:16, 0:T]), R=[pb_b], W=[gaT_b])
            if DBG_LEVEL < 1.6:
                return
            wt, wb = get_slot(l, 3)
            for nm, cbase, scale in (("gq", 0, 0.125), ("gk", 256, 1.0)):
                for c in range(2):
                    pb_ap, pb_b = bank()
                    fgroup(wt, wb, cbase + c * 128, 128, pb_ap, pb_b)
                    ACT.op(lambda nm=nm, c=c, pb_ap=pb_ap, scale=scale: nc.scalar.activation(
                        out=fq[nm][0][:, c, :], in_=pb_ap[:, 0:T], func=AF.Copy, scale=scale), R=[pb_b], W=[fq[nm][1][c]])
            for s in range(NS):
                pb_ap, pb_b = bank()
                tgroup(wt, wb, 256, 256, s, pb_ap, pb_b)
                ACT.op(lambda s=s, pb_ap=pb_ap: nc.scalar.copy(out=tk["glk"][0][:, s, :], in_=pb_ap[:, 0:256]),
                       R=[pb_b], W=[tk["glk"][1][s]])
            wt, wb = get_slot(l, 4)
            for s in range(NS):
                pb_ap, pb_b = bank()
                tgroup(wt, wb, 0, 512, s, pb_ap, pb_b)
                DVE.op(lambda s=s, pb_ap=pb_ap: nc.vector.tensor_copy(out=tk["gv"][0][:, s, :], in_=pb_ap[:, 0:256]),
                       R=[pb_b], W=[tk["gv"][1][s]])
                ACT.op(lambda s=s, pb_ap=pb_ap: nc.scalar.activation(
                    out=tk["sgr"][0][:, s, :], in_=pb_ap[:, 256:512], func=AF.Silu), R=[pb_b], W=[tk["sgr"][1][s]])
            if DBG_LEVEL < 1.8:
                return
            wt, wb = get_slot(l, 5)
            for c in range(2):
                pg_ap, pg_b = bank()
                fgroup(wt, wb, 256 + c * 128, 128, pg_ap, pg_b)
                pa_ap, pa_b = bank()
                fgroup(wt, wb, c * 128, 128, pa_ap, pa_b)
                sg_t, sg_b = scratch()
                ACT.op(lambda pg_ap=pg_ap, sg_t=sg_t: nc.scalar.activation(out=sg_t[:, 0:T], in_=pg_ap[:, 0:T], func=AF.Sigmoid),
                       R=[pg_b], W=[sg_b])
                ACT.op(lambda c=c: nc.scalar.copy(out=zc[:, c, 0:30], in_=zch[:, l, c, :]), R=[zch_b[l][c]], W=[zc_b[c]])
                DVE.op(lambda c=c, pa_ap=pa_ap, sg_t=sg_t: nc.vector.tensor_tensor(
                    out=zc[:, c, 30:30 + T], in0=pa_ap[:, 0:T], in1=sg_t[:, 0:T], op=ALU.mult),
                    R=[pa_b, sg_b], W=[zc_b[c]])
                ACT.op(lambda c=c: nc.scalar.copy(out=zch[:, l, c, :], in_=zc[:, c, T:T + 30]), R=[zc_b[c]], W=[zch_b[l][c]])
            wt, wb = get_slot(l, 6)
            for c in range(2):
                pb_ap, pb_b = bank()
                fgroup(wt, wb, c * 128, 128, pb_ap, pb_b)
                ACT.op(lambda c=c, pb_ap=pb_ap: nc.scalar.copy(out=bgs[:, c, :], in_=pb_ap[:, 0:T]), R=[pb_b], W=[bgs_b[c]])
                pb_ap, pb_b = bank()
                fgroup(wt, wb, 256 + c * 128, 128, pb_ap, pb_b)
                ACT.op(lambda c=c, pb_ap=pb_ap: nc.scalar.copy(out=cgs[:, c, :], in_=pb_ap[:, 0:T]), R=[pb_b], W=[cgs_b[c]])
            wt, wb = get_slot(l, 7)
            for c in range(2):
                pb_ap, pb_b = bank()
                fgroup(wt, wb, c * 128, 128, pb_ap, pb_b)
                ACT.op(lambda c=c: nc.scalar.copy(out=zs[:, c, 0:2], in_=zsh[:, l, c, :]), R=[zsh_b[l][c]], W=[zs_b[c]])
                DVE.op(lambda c=c, pb_ap=pb_ap: nc.vector.tensor_tensor(
                    out=zs[:, c, 2:2 + T], in0=pb_ap[:, 0:T], in1=cgs[:, c, :], op=ALU.mult),
                    R=[pb_b, cgs_b[c]], W=[zs_b[c]])
                ACT.op(lambda c=c: nc.scalar.copy(out=zsh[:, l, c, :], in_=zs[:, c, T:T + 2]), R=[zs_b[c]], W=[zsh_b[l][c]])

            if DBG_LEVEL < 3:
                return
            for c in range(2):
                y_t, y_b = scratch()
                w0 = pvec[:, pv + 148 + c * 3:pv + 148 + c * 3 + 3]
                DVE.op(lambda c=c, y_t=y_t, w0=w0: nc.vector.tensor_scalar(
                    out=y_t[:, 0:T], in0=zs[:, c, 2:2 + T], scalar1=w0[:, 2:3], scalar2=None, op0=ALU.mult),
                    R=[zs_b[c], cst_b], W=[y_b])
                for j in (1, 0):
                    DVE.op(lambda c=c, y_t=y_t, w0=w0, j=j: nc.vector.scalar_tensor_tensor(
                        out=y_t[:, 0:T], in0=zs[:, c, j:j + T], scalar=w0[:, j:j + 1], in1=y_t[:, 0:T],
                        op0=ALU.mult, op1=ALU.add), R=[zs_b[c], cst_b, y_b], W=[y_b])
                DVE.op(lambda c=c, y_t=y_t: nc.vector.tensor_tensor(
                    out=zT["sc"][0][:, c, :], in0=y_t[:, 0:T], in1=bgs[:, c, :], op=ALU.mult),
                    R=[y_b, bgs_b[c]], W=[zT["sc"][1][c]])

            for c in range(2):
                wt, wb = get_slot(l, 37 + c)
                pb_ap, pb_b = bank()
                for j in range(31):
                    PE.op(lambda j=j, c=c, wt=wt, pb_ap=pb_ap: nc.tensor.matmul(
                        pb_ap[:, 0:T], lhsT=wt[:, j * 128:(j + 1) * 128], rhs=zc[:, c, j:j + T],
                        start=(j == 0), stop=(j == 30)), R=[wb, zc_b[c]], W=[pb_b])
                ACT.op(lambda c=c, pb_ap=pb_ap: nc.scalar.activation(
                    out=uconv[:, c, :], in_=pb_ap[:, 0:T], func=AF.Identity, bias=pvec[:, pv + 142 + c:pv + 143 + c]),
                    R=[pb_b, cst_b], W=[uconv_b[c]])
            usq_t, usq_b = mtile("usq", [128, 2, T])
            ACT.op(lambda: nc.scalar.activation(out=usq_t[:, :, :], in_=uconv[:, :, :], func=AF.Square),
                   R=uconv_b, W=[usq_b])
            psum_ap, psum_b = bank()
            psq_ap, psq_b = bank()
            for c in range(2):
                PE.op(lambda c=c: nc.tensor.matmul(psum_ap[:, 0:T], lhsT=ones, rhs=uconv[:, c, :], start=(c == 0), stop=(c == 1)),
                      R=[cst_b, uconv_b[c]], W=[psum_b])
            for c in range(2):
                PE.op(lambda c=c: nc.tensor.matmul(psq_ap[:, 0:T], lhsT=ones, rhs=usq_t[:, c, :], start=(c == 0), stop=(c == 1)),
                      R=[cst_b, usq_b], W=[psq_b])
            mean_t, mean_b = scratch()
            msq_t, msq_b = scratch()
            rs_t, rs_b = scratch()
            ACT.op(lambda: nc.scalar.activation(out=mean_t[:, 0:T], in_=psum_ap[:, 0:T], func=AF.Copy, scale=1.0 / MIX),
                   R=[psum_b], W=[mean_b])
            ACT.op(lambda: nc.scalar.activation(out=msq_t[:, 0:T], in_=psum_ap[:, 0:T], func=AF.Square, scale=1.0 / MIX),
                   R=[psum_b], W=[msq_b])
            DVE.op(lambda: nc.vector.scalar_tensor_tensor(
                out=rs_t[:, 0:T], in0=psq_ap[:, 0:T], scalar=1.0 / MIX, in1=msq_t[:, 0:T], op0=ALU.mult, op1=ALU.subtract),
                R=[psq_b, msq_b], W=[rs_b])
            ACT.op(lambda: nc.scalar.activation(out=rs_t[:, 0:T], in_=rs_t[:, 0:T], func=AF.Ln, bias=eps_ap),
                   R=[rs_b, cst_b], W=[rs_b])
            ACT.op(lambda: nc.scalar.activation(out=rs_t[:, 0:T], in_=rs_t[:, 0:T], func=AF.Exp, scale=-0.5),
                   R=[rs_b], W=[rs_b])
            for c in range(2):
                d_t, d_b = scratch()
                DVE.op(lambda c=c, d_t=d_t: nc.vector.tensor_tensor(
                    out=d_t[:, 0:T], in0=uconv[:, c, :], in1=mean_t[:, 0:T], op=ALU.subtract),
                    R=[uconv_b[c], mean_b], W=[d_b])
                DVE.op(lambda c=c, d_t=d_t: nc.vector.tensor_tensor(
                    out=d_t[:, 0:T], in0=d_t[:, 0:T], in1=rs_t[:, 0:T], op=ALU.mult), R=[d_b, rs_b], W=[d_b])
                ACT.op(lambda c=c, d_t=d_t: nc.scalar.activation(
                    out=zT["conf"][0][:, c, :], in_=d_t[:, 0:T], func=AF.Silu,
                    scale=pvec[:, pv + 144 + c:pv + 145 + c], bias=pvec[:, pv + 146 + c:pv + 147 + c]),
                    R=[d_b, cst_b], W=[zT["conf"][1][c]])

            if DBG_LEVEL >= 4:
                mixers(l, i)
            if first:
                for nm in ("ml", "gla", "conf", "sc"):
                    dbg_dump("z_" + nm, zT[nm][0][:, :, :], zT[nm][1])
            if DBG_LEVEL < 5:
                return
            merge_and_out(l, i, pv, first)

        def mixers(l, i):
            pv = l * PVL
            br = l * BRL
            def mix_gen(s):
                tok = slice(s * 128, (s + 1) * 128)
                e_t, e_b = small()
                ACT.op(lambda e_t=e_t: nc.scalar.activation(out=e_t[:, 0:4], in_=iff[:, s, 4:8], func=AF.Exp, scale=-1.0),
                       R=[iff_b[s]], W=[e_b])
                ACT.op(lambda e_t=e_t: nc.scalar.activation(out=e_t[:, 4:8], in_=e_t[:, 0:4], func=AF.Ln, bias=one_ap),
                       R=[e_b, cst_b], W=[e_b])
                cs_ap, cs_b = bank()
                PE.op(lambda e_t=e_t: nc.tensor.matmul(cs_ap[:, 0:4], lhsT=Umat, rhs=e_t[:, 4:8], start=True, stop=True),
                      R=[cst_b, e_b], W=[cs_b])
                PE.op(lambda e_t=e_t: nc.tensor.matmul(cs_ap[:, 8:12], lhsT=ones, rhs=e_t[:, 4:8], start=True, stop=True),
                      R=[cst_b, e_b], W=[cs_b])
                la_ap, la_b = bank()
                PE.op(lambda: nc.tensor.matmul(la_ap[:, 0:256], lhsT=gaT[:, tok], rhs=w2[:, l * 256:(l + 1) * 256],
                                               start=True, stop=True), R=[gaT_b, cst_b], W=[la_b])
                w_t, w_b = small()
                DVE.op(lambda w_t=w_t: nc.vector.tensor_tensor(out=w_t[:, 0:4], in0=cs_ap[:, 0:4], in1=iff[:, s, 0:4], op=ALU.add),
                       R=[cs_b, iff_b[s]], W=[w_b])
                ACT.op(lambda w_t=w_t: nc.scalar.activation(out=w_t[:, 4:8], in_=w_t[:, 0:4], func=AF.Exp), R=[w_b], W=[w_b])
                ACT.op(lambda w_t=w_t: nc.scalar.activation(out=w_t[:, 8:12], in_=cs_ap[:, 0:4], func=AF.Exp, scale=-1.0),
                       R=[cs_b], W=[w_b])
                ebl_t, ebl_b = small()
                for hh in range(2):
                    ACT.op(lambda hh=hh, ebl_t=ebl_t: nc.scalar.activation(
                        out=ebl_t[hh * 64:(hh + 1) * 64, 0:2],
                        in_=cs_ap[hh * 64:(hh + 1) * 64, 8:12].rearrange("p (a b) -> p a b", b=2)[:, :, hh],
                        func=AF.Exp, scale=-1.0), R=[cs_b], W=[ebl_b])
                xla_t, xla_b = mtile("xla", [128, 256])
                DVE.op(lambda xla_t=xla_t: nc.vector.tensor_tensor(
                    out=xla_t[:, :], in0=la_ap[:, 0:256], in1=brow[:, br + 520:br + 776], op=ALU.add),
                    R=[la_b, cst_b], W=[xla_b])
                ACT.op(lambda xla_t=xla_t: nc.scalar.activation(out=xla_t[:, :], in_=xla_t[:, :], func=AF.Exp, scale=-1.0),
                       R=[xla_b], W=[xla_b])
                ACT.op(lambda xla_t=xla_t: nc.scalar.activation(out=xla_t[:, :], in_=xla_t[:, :], func=AF.Ln, bias=one_ap),
                       R=[xla_b, cst_b], W=[xla_b])
                vx_t, vx_b = mtile("vext", [128, 4, 72], BF16)
                DVE.op(lambda vx_t=vx_t, w_t=w_t: nc.vector.tensor_tensor(
                    out=vx_t[:, :, 0:64], in0=tk["mlv"][0][:, s, :].rearrange("p (h e) -> p h e", e=64),
                    in1=w_t[:, 4:8].unsqueeze(2).to_broadcast([128, 4, 64]), op=ALU.mult),
                    R=[tk["mlv"][1][s], w_b], W=[vx_b])
                DVE.op(lambda vx_t=vx_t, w_t=w_t: nc.vector.tensor_copy(out=vx_t[:, :, 64:65], in_=w_t[:, 4:8].unsqueeze(2)),
                       R=[w_b], W=[vx_b])
                nb_ap, nb_b = bank()
                for p in range(2):
                    PE.op(lambda p=p, xla_t=xla_t: nc.tensor.matmul(
                        nb_ap[:, p * 128:(p + 1) * 128], lhsT=xla_t[:, p * 128:(p + 1) * 128], rhs=U16, start=True, stop=True),
                        R=[xla_b, cst_b], W=[nb_b])
                PE.op(lambda xla_t=xla_t: nc.tensor.matmul(nb_ap[:, 256:512], lhsT=SU16, rhs=xla_t[:, :], start=True, stop=True),
                      R=[xla_b, cst_b], W=[nb_b])
                yield 'a'
                st_ap, st_b = bank()
                for h in range(4):
                    p, hh = h // 2, h % 2
                    rows = slice(hh * 64, (hh + 1) * 64)
                    PE.op(lambda h=h, p=p, rows=rows: nc.tensor.matmul(
                        st_ap[:, h * 128:(h + 1) * 128], lhsT=fq["mlk"][0][rows, p, tok], rhs=fq["mlq"][0][rows, p, tok],
                        start=True, stop=True), R=[fq["mlk"][1][p], fq["mlq"][1][p]], W=[st_b], drain=True)
                pt_t, pt_b = mtile("PT", [128, 4, 128], BF16)
                DVE.op(lambda pt_t=pt_t: nc.vector.tensor_tensor(
                    out=pt_t[:, :, :], in0=st_ap.rearrange("p (h t) -> p h t", t=128), in1=mask4, op=ALU.mult),
                    R=[st_b, cst_b], W=[pt_b])
                yield 'a'
                eq_t, eq_b = mtile("eq", [128, 2, 128])
                ek_t, ek_b = mtile("ek", [128, 2, 128])
                nb3 = nb_ap[:, 0:256].rearrange("p (a t) -> p a t", t=128)
                ACT.op(lambda eq_t=eq_t: nc.scalar.activation(out=eq_t[:, :, :], in_=nb3, func=AF.Exp, scale=-1.0),
                       R=[nb_b], W=[eq_b])
                ACT.op(lambda ek_t=ek_t: nc.scalar.activation(out=ek_t[:, :, :], in_=nb3, func=AF.Exp), R=[nb_b], W=[ek_b])
                ebg_t, ebg_b = small()
                ACT.op(lambda ebg_t=ebg_t: nc.scalar.activation(out=ebg_t[:, 0:2], in_=nb3[:, :, 127], func=AF.Exp, scale=-1.0),
                       R=[nb_b], W=[ebg_b])
                er_t, er_b = mtile("erem", [128, 256])
                ACT.op(lambda er_t=er_t: nc.scalar.activation(out=er_t[:, :], in_=nb_ap[:, 256:512], func=AF.Exp, scale=-1.0),
                       R=[nb_b], W=[er_b])
                qd_t, qd_b = mtile("qd", [128, 2, 128], BF16)
                ki_t, ki_b = mtile("ki", [128, 2, 128], BF16)
                kd_t, kd_b = mtile("kd", [128, 256], BF16)
                DVE.op(lambda qd_t=qd_t, eq_t=eq_t: nc.vector.tensor_tensor(
                    out=qd_t[:, :, :], in0=fq["gq"][0][:, :, tok], in1=eq_t[:, :, :], op=ALU.mult),
                    R=fq["gq"][1] + [eq_b], W=[qd_b])
                DVE.op(lambda ki_t=ki_t, ek_t=ek_t: nc.vector.tensor_tensor(
                    out=ki_t[:, :, :], in0=fq["gk"][0][:, :, tok], in1=ek_t[:, :, :], op=ALU.mult),
                    R=fq["gk"][1] + [ek_b], W=[ki_b])
                DVE.op(lambda kd_t=kd_t, er_t=er_t: nc.vector.tensor_tensor(
                    out=kd_t[:, :], in0=tk["glk"][0][:, s, :], in1=er_t[:, :], op=ALU.mult),
                    R=[tk["glk"][1][s], er_b], W=[kd_b])
                yield 'endA'
                mo_ap, mo_b = bank()
                mo3 = mo_ap[:, 0:288].rearrange("p (h e) -> p h e", e=72)
                for h in range(4):
                    p, hh = h // 2, h % 2
                    rows = slice(hh * 64, (hh + 1) * 64)
                    PE.op(lambda h=h, pt_t=pt_t, vx_t=vx_t: nc.tensor.matmul(
                        mo3[:, h, 0:65], lhsT=pt_t[:, h, :], rhs=vx_t[:, h, 0:65], start=True, stop=False),
                        R=[pt_b, vx_b], W=[mo_b], drain=True)
                    PE.op(lambda h=h, p=p, rows=rows: nc.tensor.matmul(
                        mo3[:, h, 0:65], lhsT=fq["mlq"][0][rows, p, tok], rhs=Cbf[rows, l, p, 0:65], start=False, stop=True),
                        R=[fq["mlq"][1][p], Cbf_b[l][p]], W=[mo_b], drain=True)
                at_ap, at_b = bank()
                for h in range(4):
                    p, hh = h // 2, h % 2
                    rows = slice(hh * 64, (hh + 1) * 64)
                    PE.op(lambda h=h, p=p, rows=rows, ki_t=ki_t, qd_t=qd_t: nc.tensor.matmul(
                        at_ap[:, h * 128:(h + 1) * 128], lhsT=ki_t[rows, p, :], rhs=qd_t[rows, p, :], start=True, stop=True),
                        R=[ki_b, qd_b], W=[at_b], drain=True)
                at_t, at_tb = mtile("AT", [128, 4, 128], BF16)
                DVE.op(lambda at_t=at_t: nc.vector.tensor_tensor(
                    out=at_t[:, :, :], in0=at_ap.rearrange("p (h t) -> p h t", t=128), in1=mask4, op=ALU.mult),
                    R=[at_b, cst_b], W=[at_tb])
                cu_ap, cu_b = bank()
                cu3 = cu_ap[:, 0:144].rearrange("p (a e) -> p a e", e=72)
                for h in range(4):
                    p, hh = h // 2, h % 2
                    rows = slice(hh * 64, (hh + 1) * 64)
                    PE.op(lambda h=h, p=p, rows=rows, vx_t=vx_t: nc.tensor.matmul(
                        cu3[rows, p, 0:65], lhsT=tk["mlk"][0][:, s, h * 64:(h + 1) * 64], rhs=vx_t[:, h, 0:65],
                        start=True, stop=True), R=[tk["mlk"][1][s], vx_b], W=[cu_b], drain=True)
                yield 'endB'
                r_t, r_b = small()
                DVE.op(lambda r_t=r_t, w_t=w_t: nc.vector.tensor_tensor(
                    out=r_t[:, 0:4], in0=mo3[:, :, 64], in1=w_t[:, 8:12], op=ALU.mult), R=[mo_b, w_b], W=[r_b])
                DVE.op(lambda r_t=r_t: nc.vector.tensor_scalar(
                    out=r_t[:, 12:16], in0=r_t[:, 0:4], scalar1=-1.0, scalar2=1.0, op0=ALU.mult, op1=ALU.max), R=[r_b], W=[r_b])
                DVE.op(lambda r_t=r_t: nc.vector.tensor_tensor(
                    out=r_t[:, 0:4], in0=r_t[:, 0:4], in1=r_t[:, 12:16], op=ALU.max), R=[r_b], W=[r_b])
                DVE.op(lambda r_t=r_t: nc.vector.reciprocal(out=r_t[:, 4:8], in_=r_t[:, 0:4]), R=[r_b], W=[r_b])
                DVE.op(lambda r_t=r_t, w_t=w_t: nc.vector.tensor_tensor(
                    out=r_t[:, 8:12], in0=r_t[:, 4:8], in1=w_t[:, 8:12], op=ALU.mult), R=[r_b, w_b], W=[r_b])
                hm_t, hm_b = mtile("hm", [128, 4, 64])
                DVE.op(lambda hm_t=hm_t, r_t=r_t: nc.vector.tensor_tensor(
                    out=hm_t[:, :, :], in0=mo3[:, :, 0:64], in1=r_t[:, 8:12].unsqueeze(2).to_broadcast([128, 4, 64]), op=ALU.mult),
                    R=[mo_b, r_b], W=[hm_b])
                hsq_t, hsq_b = mtile("hsq", [128, 4, 64])
                ACT.op(lambda hm_t=hm_t, hsq_t=hsq_t: nc.scalar.activation(out=hsq_t[:, :, :], in_=hm_t[:, :, :], func=AF.Square),
                       R=[hm_b], W=[hsq_b])
                n_t, n_b = small()
                DVE.op(lambda n_t=n_t, hsq_t=hsq_t: nc.vector.tensor_reduce(out=n_t[:, 0:4], in_=hsq_t[:, :, :], axis=AX.X, op=ALU.add),
                       R=[hsq_b], W=[n_b])
                rsqrt(n_t, n_b, 0, 4, 8, 1.0 / 64, 4)
                og_t, og_b = mtile("og", [128, 256])
                DVE.op(lambda og_t=og_t: nc.vector.tensor_tensor(
                    out=og_t[:, :], in0=tk["sigo"][0][:, s, :], in1=brow[:, br + 8:br + 264], op=ALU.mult),
                    R=[tk["sigo"][1][s], cst_b], W=[og_b])
                DVE.op(lambda hm_t=hm_t, n_t=n_t: nc.vector.tensor_tensor(
                    out=hm_t[:, :, :], in0=hm_t[:, :, :], in1=n_t[:, 8:12].unsqueeze(2).to_broadcast([128, 4, 64]), op=ALU.mult),
                    R=[hm_b, n_b], W=[hm_b])
                zt_t, zt_b = mtile("ztok", [128, 256])
                DVE.op(lambda hm_t=hm_t, og_t=og_t, zt_t=zt_t: nc.vector.tensor_tensor(
                    out=zt_t[:, :], in0=hm_t[:, :, :].rearrange("p h e -> p (h e)"), in1=og_t[:, :], op=ALU.mult),
                    R=[hm_b, og_b], W=[zt_b])
                tp_ap, tp_b = bank()
                for c in range(2):
                    PE.op(lambda c=c, zt_t=zt_t: nc.tensor.transpose(tp_ap[:, c * 128:(c + 1) * 128], zt_t[:, c * 128:(c + 1) * 128], ident),
                          R=[zt_b, cst_b], W=[tp_b])
                ACT.op(lambda: nc.scalar.copy(out=zT["ml"][0][:, :, tok], in_=tp_ap[:, 0:256].rearrange("p (c t) -> p c t", t=128)),
                       R=[tp_b], W=[zT["ml"][1][s]])
                for p in range(2):
                    ct_t, ct_b = mtile("ctmp", [128, 72])
                    DVE.op(lambda p=p, ct_t=ct_t: nc.vector.tensor_tensor(
                        out=ct_t[:, 0:65], in0=cu3[:, p, 0:65], in1=Cst[:, l, p, 0:65], op=ALU.add),
                        R=[cu_b, Cst_b[l][p]], W=[ct_b])
                    DVE.op(lambda p=p, ct_t=ct_t, ebl_t=ebl_t: nc.vector.tensor_scalar(
                        out=Cst[:, l, p, 0:65], in0=ct_t[:, 0:65], scalar1=ebl_t[:, p:p + 1], scalar2=None, op0=ALU.mult),
                        R=[ct_b, ebl_b], W=[Cst_b[l][p]])
                    ACT.op(lambda p=p, ct_t=ct_t, ebl_t=ebl_t: nc.scalar.activation(
                        out=Cbf[:, l, p, 0:65], in_=ct_t[:, 0:65], func=AF.Copy, scale=ebl_t[:, p:p + 1]),
                        R=[ct_b, ebl_b], W=[Cbf_b[l][p]])
                yield 'endC'
                go_ap, go_b = bank()
                for h in range(4):
                    p, hh = h // 2, h % 2
                    rows = slice(hh * 64, (hh + 1) * 64)
                    PE.op(lambda h=h, at_t=at_t: nc.tensor.matmul(
                        go_ap[:, h * 64:(h + 1) * 64], lhsT=at_t[:, h, :], rhs=tk["gv"][0][:, s, h * 64:(h + 1) * 64],
                        start=True, stop=False), R=[at_tb, tk["gv"][1][s]], W=[go_b], drain=True)
                    PE.op(lambda h=h, p=p, rows=rows, qd_t=qd_t: nc.tensor.matmul(
                        go_ap[:, h * 64:(h + 1) * 64], lhsT=qd_t[rows, p, :], rhs=Sbf[rows, l, p, :], start=False, stop=True),
                        R=[qd_b, Sbf_b[l][p]], W=[go_b], drain=True)
                su_ap, su_b = bank()
                su3 = su_ap[:, 0:128].rearrange("p (a e) -> p a e", e=64)
                for h in range(4):
                    p, hh = h // 2, h % 2
                    rows = slice(hh * 64, (hh + 1) * 64)
                    PE.op(lambda h=h, p=p, rows=rows, kd_t=kd_t: nc.tensor.matmul(
                        su3[rows, p, :], lhsT=kd_t[:, h * 64:(h + 1) * 64], rhs=tk["gv"][0][:, s, h * 64:(h + 1) * 64],
                        start=True, stop=True), R=[kd_b, tk["gv"][1][s]], W=[su_b], drain=True)
                gm_t, gm_b = mtile("gm", [128, 4, 64])
                ACT.op(lambda gm_t=gm_t: nc.scalar.copy(out=gm_t[:, :, :], in_=go_ap[:, 0:256].rearrange("p (h e) -> p h e", e=64)),
                       R=[go_b], W=[gm_b])
                gsq_t, gsq_b = mtile("gsq", [128, 4, 64])
                ACT.op(lambda gsq_t=gsq_t: nc.scalar.activation(
                    out=gsq_t[:, :, :], in_=go_ap[:, 0:256].rearrange("p (h e) -> p h e", e=64), func=AF.Square),
                    R=[go_b], W=[gsq_b])
                gn_t, gn_b = small()
                DVE.op(lambda gn_t=gn_t, gsq_t=gsq_t: nc.vector.tensor_reduce(out=gn_t[:, 0:4], in_=gsq_t[:, :, :], axis=AX.X, op=ALU.add),
                       R=[gsq_b], W=[gn_b])
                rsqrt(gn_t, gn_b, 0, 4, 8, 1.0 / 64, 4)
                rg_t, rg_b = mtile("rg", [128, 256])
                DVE.op(lambda rg_t=rg_t: nc.vector.tensor_tensor(
                    out=rg_t[:, :], in0=tk["sgr"][0][:, s, :], in1=brow[:, br + 264:br + 520], op=ALU.mult),
                    R=[tk["sgr"][1][s], cst_b], W=[rg_b])
                DVE.op(lambda gm_t=gm_t, gn_t=gn_t: nc.vector.tensor_tensor(
                    out=gm_t[:, :, :], in0=gm_t[:, :, :], in1=gn_t[:, 8:12].unsqueeze(2).to_broadcast([128, 4, 64]), op=ALU.mult),
                    R=[gm_b, gn_b], W=[gm_b])
                zg_t, zg_b = mtile("zgtok", [128, 256])
                DVE.op(lambda gm_t=gm_t, rg_t=rg_t, zg_t=zg_t: nc.vector.tensor_tensor(
                    out=zg_t[:, :], in0=gm_t[:, :, :].rearrange("p h e -> p (h e)"), in1=rg_t[:, :], op=ALU.mult),
                    R=[gm_b, rg_b], W=[zg_b])
                tg_ap, tg_b = bank()
                for c in range(2):
                    PE.op(lambda c=c, zg_t=zg_t: nc.tensor.transpose(tg_ap[:, c * 128:(c + 1) * 128], zg_t[:, c * 128:(c + 1) * 128], ident),
                          R=[zg_b, cst_b], W=[tg_b])
                ACT.op(lambda: nc.scalar.copy(out=zT["gla"][0][:, :, tok], in_=tg_ap[:, 0:256].rearrange("p (c t) -> p c t", t=128)),
                       R=[tg_b], W=[zT["gla"][1][s]])
                for p in range(2):
                    DVE.op(lambda p=p, ebg_t=ebg_t: nc.vector.scalar_tensor_tensor(
                        out=Sst[:, l, p, :], in0=Sst[:, l, p, :], scalar=ebg_t[:, p:p + 1], in1=su3[:, p, :],
                        op0=ALU.mult, op1=ALU.add), R=[Sst_b[l][p], ebg_b, su_b], W=[Sst_b[l][p]])
                    ACT.op(lambda p=p: nc.scalar.copy(out=Sbf[:, l, p, :], in_=Sst[:, l, p, :]), R=[Sst_b[l][p]], W=[Sbf_b[l][p]])

            def run_until(g, tag):
                for t_ in g:
                    if t_ == tag:
                        return True
                return False

            if NS == 2:
                g0, g1 = mix_gen(0), mix_gen(1)
                a0 = a1 = True
                while a0 or a1:
                    if a0:
                        a0 = next(g0) != 'endA'
                    if a1:
                        a1 = next(g1) != 'endA'
                run_until(g0, 'endB')
                run_until(g0, 'endC')
                run_until(g1, 'endB')
                run_until(g0, None)
                run_until(g1, 'endC')
                run_until(g1, None)
            else:
                for s_ in range(NS):
                    run_until(mix_gen(s_), None)


        def merge_and_out(l, i, pv, first):
            wA, wAb = get_slot(l, 16, hold=0)
            wB, wBb = get_slot(l, 17, hold=1)
            wA3 = wA[:, :].rearrange("p (m c) -> p m c", c=1024)
            wB3 = wB[:, :].rearrange("p (m c) -> p m c", c=1024)
            branches = (("ml", wA3, 0, wAb), ("gla", wA3, 2, wAb), ("conf", wB3, 0, wBb), ("sc", wB3, 2, wBb))
            for j in range(8):
                wt, wb = get_slot(l, 8 + j)
                g_t, g_b = gates[j % 2], gates_b[j % 2]
                for b in range(4):
                    pb_ap, pb_b = bank()
                    fgroup(wt, wb, b * 128, 128, pb_ap, pb_b)
                    ACT.op(lambda b=b, pb_ap=pb_ap, g_t=g_t: nc.scalar.activation(
                        out=g_t[:, b, :], in_=pb_ap[:, 0:T], func=AF.Sigmoid, bias=pvec[:, pv + 162 + b * 8 + j:pv + 163 + b * 8 + j]),
                        R=[pb_b, cst_b], W=[g_b])
                acc_t, acc_b = scratch()
                for b, (nm, w3, m0, wbb) in enumerate(branches):
                    pb_ap, pb_b = bank()
                    for kc in range(2):
                        PE.op(lambda kc=kc, nm=nm, w3=w3, m0=m0, pb_ap=pb_ap: nc.tensor.matmul(
                            pb_ap[:, 0:T], lhsT=w3[:, m0 + kc, j * 128:(j + 1) * 128], rhs=zT[nm][0][:, kc, :],
                            start=(kc == 0), stop=(kc == 1)), R=[wbb] + zT[nm][1], W=[pb_b])
                    if b == 0:
                        DVE.op(lambda pb_ap=pb_ap, g_t=g_t, acc_t=acc_t: nc.vector.tensor_tensor(
                            out=acc_t[:, 0:T], in0=pb_ap[:, 0:T], in1=g_t[:, 0, :], op=ALU.mult), R=[pb_b, g_b], W=[acc_b])
                    else:
                        tm_t, tm_b = scratch()
                        if nm == "conf":
                            DVE.op(lambda pb_ap=pb_ap, g_t=g_t, tm_t=tm_t, b=b: nc.vector.scalar_tensor_tensor(
                                out=tm_t[:, 0:T], in0=pb_ap[:, 0:T], scalar=pvec[:, pv + 154 + j:pv + 155 + j], in1=g_t[:, b, :],
                                op0=ALU.add, op1=ALU.mult), R=[pb_b, g_b, cst_b], W=[tm_b])
                        else:
                            DVE.op(lambda pb_ap=pb_ap, g_t=g_t, tm_t=tm_t, b=b: nc.vector.tensor_tensor(
                                out=tm_t[:, 0:T], in0=pb_ap[:, 0:T], in1=g_t[:, b, :], op=ALU.mult), R=[pb_b, g_b], W=[tm_b])
                        if b < 3:
                            POOL.op(lambda tm_t=tm_t, acc_t=acc_t: nc.gpsimd.tensor_tensor(
                                out=acc_t[:, 0:T], in0=acc_t[:, 0:T], in1=tm_t[:, 0:T], op=ALU.add), R=[tm_b, acc_b], W=[acc_b])
                        else:
                            POOL.op(lambda tm_t=tm_t, acc_t=acc_t: nc.gpsimd.tensor_tensor(
                                out=mergedT[:, j, :], in0=acc_t[:, 0:T], in1=tm_t[:, 0:T], op=ALU.add),
                                R=[tm_b, acc_b], W=[merged_b[j]])
            if first:
                dbg_dump("merged", mergedT[:, :, :], merged_b)
                dbg_dump("grow", Grow[:, 0:2, :], Grow_b[0:2])
            if DBG_LEVEL < 6:
                return
            w0, w0b = get_slot(l, 18)
            w1, w1b = get_slot(l, 19)
            for s in range(NS):
                pbs = []
                for n, (wt, wb) in enumerate(((w0, w0b), (w1, w1b))):
                    pb_ap, pb_b = bank()
                    w3 = wt[:, :].rearrange("p (kc c) -> p kc c", c=512)
                    for kc in range(8):
                        PE.op(lambda kc=kc, w3=w3, pb_ap=pb_ap: nc.tensor.matmul(
                            pb_ap[:, :], lhsT=mergedT[:, kc, s * 128:(s + 1) * 128], rhs=w3[:, kc, :],
                            start=(kc == 0), stop=(kc == 7)), R=[wb, merged_b[kc]], W=[pb_b])
                    pbs.append((pb_ap, pb_b))
                postnorm(l, 0, s, pbs)

        def postnorm(l, which, s, pbs):
            st_t, st_b = small()
            ys = []
            for n, (pb_ap, pb_b) in enumerate(pbs):
                j_t, j_b = scratch()
                ACT.op(lambda n=n, pb_ap=pb_ap, j_t=j_t, st_t=st_t: nc.scalar.activation(
                    out=j_t[:, 0:512], in_=pb_ap[:, :], func=AF.Square, accum_out=st_t[:, n:n + 1]), R=[pb_b], W=[j_b, st_b])
                y_t, y_b = scratch()
                DVE.op(lambda n=n, pb_ap=pb_ap, y_t=y_t: nc.vector.tensor_tensor(
                    out=y_t[:, 0:512], in0=pb_ap[:, :], in1=Grow[:, l * 2 + which, n * 512:(n + 1) * 512], op=ALU.mult),
                    R=[pb_b, Grow_b[l * 2 + which]], W=[y_b])
                ys.append((y_t, y_b))
            DVE.op(lambda st_t=st_t: nc.vector.tensor_tensor(out=st_t[:, 2:3], in0=st_t[:, 0:1], in1=st_t[:, 1:2], op=ALU.add),
                   R=[st_b], W=[st_b])
            rsqrt(st_t, st_b, 2, 3, 4, 1.0 / D, 1)
            for n, (pb_ap, pb_b) in enumerate(pbs):
                y_t, y_b = ys[n]
                DVE.op(lambda n=n, y_t=y_t, st_t=st_t: nc.vector.scalar_tensor_tensor(
                    out=xt[:, s, n * 512:(n + 1) * 512], in0=y_t[:, 0:512], scalar=st_t[:, 4:5], in1=xt[:, s, n * 512:(n + 1) * 512],
                    op0=ALU.mult, op1=ALU.add), R=[y_b, st_b, xt_b[s]], W=[xt_b[s]])

        def ffn(l, i):
            pv = l * PVL
            prenorm(l, i, 1)
            for g in range(11):
                wt, wb = get_slot(l, 20 + g)
                for jj in range(2):
                    j = g * 2 + jj
                    ys = []
                    for part in range(2):
                        ch = part * 22 + j
                        pb_ap, pb_b = bank()
                        fgroup(wt, wb, part * 256 + jj * 128, 128, pb_ap, pb_b)
                        u_t, u_b = scratch()
                        uh_b = Buf()
                        POOL.op(lambda u_t=u_t, ch=ch: nc.gpsimd.tensor_copy(out=u_t[:, 0:2], in_=halo[:, l, ch, :]), R=[halo_b[l][ch]], W=[u_b])
                        ACT.op(lambda u_t=u_t, pb_ap=pb_ap: nc.scalar.copy(out=u_t[:, 2:2 + T], in_=pb_ap[:, 0:T]), R=[pb_b], W=[u_b])
                        POOL.op(lambda u_t=u_t, ch=ch: nc.gpsimd.tensor_copy(out=halo[:, l, ch, :], in_=u_t[:, T:T + 2]), R=[u_b], W=[halo_b[l][ch]])
                        wc = pvec[:, pv + 194 + ch * 3:pv + 194 + ch * 3 + 3]
                        y_t, y_b = scratch()
                        ACT.op(lambda pb_ap=pb_ap, y_t=y_t, wc=wc: nc.scalar.activation(
                            out=y_t[:, 0:T], in_=pb_ap[:, 0:T], func=AF.Identity, scale=wc[:, 2:3]),
                            R=[pb_b, cst_b], W=[y_b])
                        for q in (1, 0):
                            DVE.op(lambda u_t=u_t, y_t=y_t, wc=wc, q=q: nc.vector.scalar_tensor_tensor(
                                out=y_t[:, 0:T], in0=u_t[:, q:q + T], scalar=wc[:, q:q + 1], in1=y_t[:, 0:T],
                                op0=ALU.mult, op1=ALU.add), R=[u_b, cst_b, y_b], W=[y_b])
                        ys.append((y_t, y_b))
                    (ya_t, ya_b), (yv_t, yv_b) = ys
                    ACT.op(lambda ya_t=ya_t: nc.scalar.activation(out=ya_t[:, 0:T], in_=ya_t[:, 0:T], func=AF.Silu), R=[ya_b], W=[ya_b])
                    DVE.op(lambda ya_t=ya_t, yv_t=yv_t, j=j: nc.vector.tensor_tensor(
                        out=gff[:, j, :], in0=ya_t[:, 0:T], in1=yv_t[:, 0:T], op=ALU.mult), R=[ya_b, yv_b], W=[gff_b[j]])
            accs = {}
            for s in range(NS):
                for n in range(2):
                    accs[(s, n)] = bank()
            for q in range(6):
                wt, wb = get_slot(l, 31 + q)
                w3 = wt[:, :].rearrange("p (m c) -> p m c", c=1024)
                nk = 4 if q < 5 else 2
                for s in range(NS):
                    for n in range(2):
                        pb_ap, pb_b = accs[(s, n)]
                        for m in range(nk):
                            j = q * 4 + m
                            PE.op(lambda m=m, j=j, w3=w3, pb_ap=pb_ap, s=s, n=n: nc.tensor.matmul(
                                pb_ap[:, :], lhsT=gff[:, j, s * 128:(s + 1) * 128], rhs=w3[:, m, n * 512:(n + 1) * 512],
                                start=(j == 0), stop=(j == 21)), R=[wb, gff_b[j]], W=[pb_b])
            for s in range(NS):
                postnorm(l, 1, s, [accs[(s, 0)], accs[(s, 1)]])

        for i in range(nseq):
            seq_init(i)
            for tl in range(ntiles):
                src = x_d[i, tl * T:(tl + 1) * T, :].rearrange("(s p) d -> p s d", p=128)
                POOL.dma(xt[:, :, :], src, s_xin, W=xt_b)
                for l in range(depth):
                    if DBG_LEVEL >= 1:
                        token_mixer(l, i)
                    if DBG_LEVEL >= 7:
                        ffn(l, i)
                dst = out_d[i, tl * T:(tl + 1) * T, :].rearrange("(s p) d -> p s d", p=128)
                POOL.dma(dst, xt[:, :, :], s_xout, R=xt_b)
        allsems = [PE.sem, ACT.sem, DVE.sem, POOL.sem, SP.sem, s_xin, s_xout, s_c] + slot_s + hold_s + list(prep_sems.values()) + list(dbg_sems.values()) + diag_sems
        POOL.wait_all(allsems)
        SP.wait_all([s_xout])
    return nc


def host_pack(inp, core):
    f = np.float32
    b0 = core * 2
    cT = np.ascontiguousarray(inp["c"][b0:b0 + 2].reshape(2, 8, 128).transpose(2, 1, 0).reshape(128, 16)).astype(f)
    pvec = np.zeros((128, DEPTH * PVL), f)
    brow = np.zeros((DEPTH * BRL,), f)
    w2 = np.zeros((16, DEPTH * 256), f)

    def fm(v, nch):
        return np.asarray(v, f).reshape(nch, 128).T

    for l in range(DEPTH):
        o = l * PVL
        pvec[:, o + 0:o + 8] = fm(inp["tm_pre_g"][l], 8)
        pvec[:, o + 8:o + 16] = fm(inp["tm_post_g"][l], 8)
        pvec[:, o + 16:o + 24] = fm(inp["cm_pre_g"][l], 8)
        pvec[:, o + 24:o + 32] = fm(inp["cm_post_g"][l], 8)
        pvec[:, o + 32:o + 80] = fm(inp["ada_b"][l], 48)
        cw = np.asarray(inp["conf_dw_w"][l], f)
        for c in range(2):
            pvec[:, o + 80 + c * 31:o + 80 + (c + 1) * 31] = cw[:, c * 128:(c + 1) * 128].T
        pvec[:, o + 142:o + 144] = fm(inp["conf_dw_b"][l], 2)
        pvec[:, o + 144:o + 146] = fm(inp["conf_ln_g"][l], 2)
        pvec[:, o + 146:o + 148] = fm(inp["conf_ln_b"][l], 2)
        sw = np.asarray(inp["sc_dw_w"][l], f)
        for c in range(2):
            pvec[:, o + 148 + c * 3:o + 148 + (c + 1) * 3] = sw[:, c * 128:(c + 1) * 128].T
        pvec[:, o + 154:o + 162] = fm(inp["conf_out_b"][l], 8)
        pvec[:, o + 162:o + 194] = fm(inp["merge_gate_b"][l], 32)
        fw = np.asarray(inp["ffn_dw_w"][l], f)
        for ch in range(44):
            pvec[:, o + 194 + ch * 3:o + 194 + (ch + 1) * 3] = fw[:, ch * 128:(ch + 1) * 128].T
        r = l * BRL
        brow[r + 0:r + 4] = inp["ml_i_bias"][l]
        brow[r + 4:r + 8] = inp["ml_f_bias"][l]
        brow[r + 8:r + 264] = inp["ml_norm_g"][l]
        brow[r + 264:r + 520] = inp["gla_norm_g"][l]
        brow[r + 520:r + 776] = inp["gla_a_bias"][l]
        w2[:, l * 256:(l + 1) * 256] = inp["gla_w_a2"][l]
    brow = np.ascontiguousarray(np.broadcast_to(brow[None, :], (128, DEPTH * BRL)))
    return cT, pvec, brow, w2


def make_consts():
    f = np.float32
    c = np.zeros((128, 1152), f)
    c[:, 0:128] = np.eye(128, dtype=f)
    c[:, 128:256] = 1.0
    s = np.arange(128)[:, None]
    t = np.arange(128)[None, :]
    U = (s <= t).astype(f)
    c[:, 256:384] = U
    c[:, 384:512] = U / 16.0
    c[:, 512:640] = (s > t).astype(f) / 16.0
    c[:, 640:1152] = np.tile(U, (1, 4))
    return c


BIG = ("ada_w", "w_in", "w_ml_out", "w_gla_out", "w_conf_out", "w_sc_out", "w_o", "ffn_w_up", "ffn_w_down")


def make_in_maps(inp, ncores, nseq, ntok):
    consts = make_consts()
    big = {k: np.ascontiguousarray(np.asarray(inp[k], np.float32)) for k in BIG}
    maps = []
    for core in range(ncores):
        cT, pvec, brow, w2 = host_pack(inp, core)
        m = {"x": np.ascontiguousarray(np.asarray(inp["x"][core * 2:core * 2 + nseq, :ntok], np.float32)),
             "cT": cT, "pvec": pvec, "brow": brow, "w2": w2, "consts": consts}
        m.update(big)
        maps.append(m)
    return maps


def kernel(**inputs):
    nc = build()
    maps = make_in_maps(inputs, NCORES, 2, SEQ)
    res = run_bass_kernel_spmd(nc, maps, core_ids=list(range(NCORES)))
    out = np.concatenate([np.asarray(r["out"]) for r in res.results], axis=0)
    return out.astype(np.float32)
```

```python
import contextlib
import numpy as np
import concourse.bass as bass
import concourse.mybir as mybir
from concourse.bass_utils import run_bass_kernel_spmd

F32 = mybir.dt.float32
BF16 = mybir.dt.bfloat16
AF = mybir.ActivationFunctionType
ALU = mybir.AluOpType
AX = mybir.AxisListType

D = 1024
MIX = 256
HID = 2816
INW = 7448
DEPTH = 2
SEQ = 4096
BATCH = 16
NCORES = 8
T = 256
NS = T // 128
NSLOT = 4
SLOTS_PER_LAYER = 39
EPS = 1e-6
PVL = 326
BRL = 776
GATE0 = 3352
DBG_LEVEL = 99


class Sem:
    def __init__(self, h):
        self.h = h
        self.count = 0


class Buf:
    __slots__ = ("w", "r", "excl")

    def __init__(self, excl=False):
        self.w = None
        self.r = {}
        self.excl = excl


class Eng:
    def __init__(self, e, sem, is_pe=False):
        self.e = e
        self.sem = sem
        self.seen = {}
        self.is_pe = is_pe
        self.hook = None
        self._inhook = False

    def _sync(self, reads, writes):
        need = {}
        for b in reads:
            if b.w is not None and need.get(b.w[0], 0) < b.w[1]:
                need[b.w[0]] = b.w[1]
        for b in writes:
            if b.w is not None and need.get(b.w[0], 0) < b.w[1]:
                need[b.w[0]] = b.w[1]
            for sm, v in b.r.items():
                if need.get(sm, 0) < v:
                    need[sm] = v
        for sm, v in need.items():
            if self.is_pe and sm is self.sem:
                continue
            if self.seen.get(sm, 0) < v:
                self.e.wait_ge(sm.h, v)
                self.seen[sm] = v

    @staticmethod
    def _mark(sm, reads, writes):
        v = sm.count
        for b in reads:
            if b.r.get(sm, 0) < v:
                b.r[sm] = v
        for b in writes:
            b.w = (sm, v)
            b.r = {}

    def op(self, fn, R=(), W=(), drain=False):
        if drain and self.sem.count > 0 and self.seen.get(self.sem, 0) < self.sem.count:
            self.e.wait_ge(self.sem.h, self.sem.count)
            self.seen[self.sem] = self.sem.count
        ex = [b for b in R if b.excl]
        if ex:
            W = list(W) + ex
        self._sync(R, W)
        inst = fn()
        self.sem.count += 1
        inst.then_inc(self.sem.h, 1)
        self._mark(self.sem, R, W)
        if self.hook is not None and not self._inhook:
            self._inhook = True
            self.hook()
            self._inhook = False

    def dma(self, out, in_, dsem, R=(), W=()):
        self._sync(R, W)
        inst = self.e.dma_start(out=out, in_=in_)
        dsem.count += 16
        inst.then_inc(dsem.h, 16)
        self._mark(dsem, R, W)

    def wait_all(self, sems):
        for sm in sems:
            if sm.count > 0 and self.seen.get(sm, 0) < sm.count:
                self.e.wait_ge(sm.h, sm.count)
                self.seen[sm] = sm.count


def build(nseq=2, ntiles=SEQ // T, depth=DEPTH, dbg=None):
    nc = bass.Bass("TRN2", target_bir_lowering=False)
    ntok = ntiles * T
    dt = nc.dram_tensor
    x_d = dt("x", [nseq, ntok, D], F32, kind="ExternalInput").ap()
    out_d = dt("out", [nseq, ntok, D], F32, kind="ExternalOutput").ap()
    cT_d = dt("cT", [128, 16], F32, kind="ExternalInput").ap()
    pvec_d = dt("pvec", [128, DEPTH * PVL], F32, kind="ExternalInput").ap()
    brow_d = dt("brow", [128, DEPTH * BRL], F32, kind="ExternalInput").ap()
    w2_d = dt("w2", [16, DEPTH * 256], F32, kind="ExternalInput").ap()
    const_d = dt("consts", [128, 1152], F32, kind="ExternalInput").ap()
    adaw_d = dt("ada_w", [DEPTH, D, 6 * D], F32, kind="ExternalInput").ap()
    win_d = dt("w_in", [DEPTH, D, INW], F32, kind="ExternalInput").ap()
    wml_d = dt("w_ml_out", [DEPTH, MIX, D], F32, kind="ExternalInput").ap()
    wgl_d = dt("w_gla_out", [DEPTH, MIX, D], F32, kind="ExternalInput").ap()
    wcf_d = dt("w_conf_out", [DEPTH, MIX, D], F32, kind="ExternalInput").ap()
    wsc_d = dt("w_sc_out", [DEPTH, MIX, D], F32, kind="ExternalInput").ap()
    wo_d = dt("w_o", [DEPTH, D, D], F32, kind="ExternalInput").ap()
    wup_d = dt("ffn_w_up", [DEPTH, D, 2 * HID], F32, kind="ExternalInput").ap()
    wdn_d = dt("ffn_w_down", [DEPTH, HID, D], F32, kind="ExternalInput").ap()
    scr_d = dt("wscratch", [DEPTH * SLOTS_PER_LAYER, 128, 4096], BF16, kind="Internal").ap()
    dbg_d = {}
    if dbg:
        for name, shape in dbg.items():
            dbg_d[name] = dt("dbg_" + name, list(shape), F32, kind="ExternalOutput").ap()

    es = contextlib.ExitStack()
    with es:
        def sb(name, shape, dtype=F32):
            return es.enter_context(nc.sbuf_tensor("sb_" + name, list(shape), dtype))

        def sem(name):
            return Sem(es.enter_context(nc.semaphore(name)))

        PE = Eng(nc.tensor, sem("s_pe"), is_pe=True)
        ACT = Eng(nc.scalar, sem("s_act"))
        DVE = Eng(nc.vector, sem("s_dve"))
        POOL = Eng(nc.gpsimd, sem("s_pool"))
        SP = Eng(nc.sync, sem("s_sp"))

        consts = sb("consts", [128, 1152])
        pvec = sb("pvec", [128, DEPTH * PVL])
        brow = sb("brow", [128, DEPTH * BRL])
        w2 = sb("w2", [16, DEPTH * 256])
        cact = sb("cact", [128, 16])
        cst_b = Buf()
        s_c = sem("s_const")
        for t_, d_ in ((consts, const_d), (pvec, pvec_d), (brow, brow_d), (w2, w2_d), (cact, cT_d)):
            SP.dma(t_[:, :], d_, s_c, W=[cst_b])
        ident = consts[:, 0:128]
        ones = consts[:, 128:256]
        Umat = consts[:, 256:384]
        U16 = consts[:, 384:512]
        SU16 = consts[:, 512:640]
        mask4 = consts[:, 640:1152].rearrange("p (h t) -> p h t", t=128)

        prep_sems = {}
        scr_b = {}

        def prep(l, s, out_ap, in_ap, grp):
            key = (l, grp)
            if key not in prep_sems:
                prep_sems[key] = sem("s_prep%d_%d" % key)
            sm = prep_sems[key]
            inst = nc.gpsimd.dma_start(out=out_ap, in_=in_ap)
            sm.count += 16
            inst.then_inc(sm.h, 16)
            scr_b.setdefault((l, s), Buf())

        slot_grp = {}

        def scr_view(l, s, pat, **kw):
            return scr_d[l * SLOTS_PER_LAYER + s].rearrange(pat, **kw)

        def k1024(src, c0, n):
            return src.rearrange("(kc p) c -> p kc c", p=128)[:, :, c0:c0 + n]

        for l in range(depth):
            wi = win_d[l]
            for s, (c0, n) in {0: (0, 512), 1: (512, 512), 3: (1032, 512), 4: (1544, 512),
                               5: (2072, 512), 6: (2584, 512), 7: (3096, 256)}.items():
                prep(l, s, scr_view(l, s, "p (kc c) -> p kc c", c=512)[:, :, 0:n], k1024(wi, c0, n), 0)
                slot_grp[(l, s)] = 0
            prep(l, 2, scr_view(l, 2, "p (kc c) -> p kc c", c=512)[:, :, 0:8], k1024(wi, 1024, 8), 0)
            prep(l, 2, scr_view(l, 2, "p (kc c) -> p kc c", c=512)[:, :, 8:24], k1024(wi, 2056, 16), 0)
            slot_grp[(l, 2)] = 0
            for j in range(8):
                for b in range(4):
                    prep(l, 8 + j, scr_view(l, 8 + j, "p (kc c) -> p kc c", c=512)[:, :, b * 128:(b + 1) * 128],
                         k1024(wi, GATE0 + b * 1024 + j * 128, 128), 1)
                slot_grp[(l, 8 + j)] = 1
            for s, (wa, wb) in {16: (wml_d, wgl_d), 17: (wcf_d, wsc_d)}.items():
                v = scr_view(l, s, "p (m c) -> p m c", c=1024)
                prep(l, s, v[:, 0:2, :], wa[l].rearrange("(kc p) c -> p kc c", p=128), 1)
                prep(l, s, v[:, 2:4, :], wb[l].rearrange("(kc p) c -> p kc c", p=128), 1)
                slot_grp[(l, s)] = 1
            for n in range(2):
                prep(l, 18 + n, scr_view(l, 18 + n, "p (kc c) -> p kc c", c=512), k1024(wo_d[l], n * 512, 512), 1)
                slot_grp[(l, 18 + n)] = 1
            for g in range(11):
                v = scr_view(l, 20 + g, "p (kc c) -> p kc c", c=512)
                prep(l, 20 + g, v[:, :, 0:256], k1024(wup_d[l], g * 256, 256), 2)
                prep(l, 20 + g, v[:, :, 256:512], k1024(wup_d[l], HID + g * 256, 256), 2)
                slot_grp[(l, 20 + g)] = 2
            for q in range(6):
                nk = 4 if q < 5 else 2
                v = scr_view(l, 31 + q, "p (m c) -> p m c", c=1024)
                prep(l, 31 + q, v[:, 0:nk, :], wdn_d[l].rearrange("(kc p) c -> p kc c", p=128)[:, 4 * q:4 * q + nk, :], 2)
                slot_grp[(l, 31 + q)] = 2
        for (l, s), b in scr_b.items():
            sm = prep_sems[(l, slot_grp[(l, s)])]
            b.w = (sm, sm.count)

        slots = [sb("wslot%d" % i, [128, 4096], BF16) for i in range(NSLOT)]
        slot_b = [Buf() for _ in range(NSLOT)]
        slot_s = [sem("s_slot%d" % i) for i in range(NSLOT)]
        slot_ctr = [0]

        def next_slot():
            i = slot_ctr[0] % NSLOT
            slot_ctr[0] += 1
            return i

        hold_t = [sb("whold%d" % i, [128, 4096], BF16) for i in range(2)]
        hold_b = [Buf(), Buf()]
        hold_s = [sem("s_hold%d" % i) for i in range(2)]

        def get_slot(l, s, hold=None):
            if hold is not None:
                SP.dma(hold_t[hold][:, :], scr_d[l * SLOTS_PER_LAYER + s], hold_s[hold], R=[scr_b[(l, s)]], W=[hold_b[hold]])
                return hold_t[hold], hold_b[hold]
            i = next_slot()
            dst = slots[i][:, :]
            src = scr_d[l * SLOTS_PER_LAYER + s]
            if s == 2:
                dst = dst.rearrange("p (kc c) -> p kc c", c=512)[:, :, 0:24]
                src = src.rearrange("p (kc c) -> p kc c", c=512)[:, :, 0:24]
            elif s == 7:
                dst = dst.rearrange("p (kc c) -> p kc c", c=512)[:, :, 0:256]
                src = src.rearrange("p (kc c) -> p kc c", c=512)[:, :, 0:256]
            elif s == 36:
                dst = dst[:, 0:2048]
                src = src[:, 0:2048]
            elif s >= 37:
                dst = dst[:, 0:3968]
                src = src[:, 0:3968]
            SP.dma(dst, src, slot_s[i], R=[scr_b[(l, s)]], W=[slot_b[i]])
            return slots[i], slot_b[i]

        ps_all = es.enter_context(nc.psum_tensor("ps", [128, 4096], F32))
        bank_b = [Buf(excl=True) for _ in range(8)]
        bank_ctr = [0]

        def bank():
            i = bank_ctr[0] % 8
            bank_ctr[0] += 1
            return ps_all[:, i * 512:(i + 1) * 512], bank_b[i]

        NSCR = 8
        scr_t = [sb("scr%d" % i, [128, 544]) for i in range(NSCR)]
        scr_bf = [Buf() for _ in range(NSCR)]
        scr_ctr = [0]

        def scratch():
            i = scr_ctr[0] % NSCR
            scr_ctr[0] += 1
            return scr_t[i], scr_bf[i]

        NSM = 16
        sm_t = [sb("sm%d" % i, [128, 16]) for i in range(NSM)]
        sm_bf = [Buf() for _ in range(NSM)]
        sm_ctr = [0]

        def small():
            i = sm_ctr[0] % NSM
            sm_ctr[0] += 1
            return sm_t[i], sm_bf[i]

        xt = sb("xt", [128, NS, D])
        xt_b = [Buf() for _ in range(NS)]
        s_xin = sem("s_xin")
        s_xout = sem("s_xout")
        xn = [sb("xn%d" % i, [128, D]) for i in range(2)]
        xn_b = [Buf(), Buf()]
        hT = sb("hT", [128, 8, T], BF16)
        hT_b = [[Buf(), Buf()] for _ in range(NS)]
        fq = {}
        for nm in ("mlq", "mlk", "gq", "gk"):
            fq[nm] = (sb(nm + "T", [128, 2, T], BF16), [Buf(), Buf()])
        tk = {}
        for nm in ("mlk", "mlv", "sigo", "glk", "gv", "sgr"):
            tk[nm] = (sb(nm + "_tok", [128, NS, 256], BF16), [Buf() for _ in range(NS)])
        iff = sb("iff", [128, NS, 8])
        iff_b = [Buf() for _ in range(NS)]
        gaT = sb("gaT", [16, T])
        gaT_b = Buf()
        zc = sb("zc", [128, 2, 30 + T], BF16)
        zc_b = [Buf(), Buf()]
        zs = sb("zs", [128, 2, 2 + T])
        zs_b = [Buf(), Buf()]
        bgs = sb("bgs", [128, 2, T], BF16)
        bgs_b = [Buf(), Buf()]
        cgs = sb("cgs", [128, 2, T])
        cgs_b = [Buf(), Buf()]
        uconv = sb("uconv", [128, 2, T])
        uconv_b = [Buf(), Buf()]
        zT = {}
        for nm in ("ml", "gla", "conf", "sc"):
            zT[nm] = (sb("z" + nm + "T", [128, 2, T], BF16), [Buf() for _ in range(max(NS, 2))])
        gates = [sb("gates%d" % i, [128, 4, T], BF16) for i in range(2)]
        gates_b = [Buf(), Buf()]
        mergedT = sb("mergedT", [128, 8, T], BF16)
        merged_b = [Buf() for _ in range(8)]
        gff = sb("gff", [128, 22, T], BF16)
        gff_b = [Buf() for _ in range(22)]
        halo = sb("halo", [128, DEPTH, 44, 2])
        halo_b = [[Buf() for _ in range(44)] for _ in range(DEPTH)]
        Cst = sb("Cst", [128, DEPTH, 2, 72])
        Cst_b = [[Buf(), Buf()] for _ in range(DEPTH)]
        Cbf = sb("Cbf", [128, DEPTH, 2, 72], BF16)
        Cbf_b = [[Buf(), Buf()] for _ in range(DEPTH)]
        Sst = sb("Sst", [128, DEPTH, 2, 64])
        Sst_b = [[Buf(), Buf()] for _ in range(DEPTH)]
        Sbf = sb("Sbf", [128, DEPTH, 2, 64], BF16)
        Sbf_b = [[Buf(), Buf()] for _ in range(DEPTH)]
        zch_b = [[Buf(), Buf()] for _ in range(DEPTH)]
        zch = sb("zch", [128, DEPTH, 2, 30], BF16)
        zsh = sb("zsh", [128, DEPTH, 2, 2])
        zsh_b = [[Buf(), Buf()] for _ in range(DEPTH)]
        Grow = sb("Grow", [128, DEPTH * 2, D])
        Grow_b = [Buf() for _ in range(DEPTH * 2)]
        modT = sb("modT", [128, DEPTH * 2 * 48])
        modT_b = Buf()
        modA = sb("modA", [128, DEPTH * 2 * 2 * 8])
        modA_b = Buf()
        mt = {}

        def mtile(name, shape, dtype=F32, n=None):
            if n is None:
                n = 2 if dtype == BF16 else 1
            if name not in mt:
                mt[name] = ([sb("%s_%d" % (name, i), shape, dtype) for i in range(n)], [Buf() for _ in range(n)], [0])
            tl, bl, c = mt[name]
            i = c[0] % n
            c[0] += 1
            return tl[i], bl[i]

        def dbg_dump(name, ap, b, idx=None):
            if dbg and name in dbg_d:
                dst = dbg_d[name] if idx is None else dbg_d[name][idx]
                sm_ = dbg_sems.setdefault(name, sem("s_dbg_" + name))
                POOL.dma(dst, ap, sm_, R=b)

        dbg_sems = {}
        dbg_state = []

        ACT.op(lambda: nc.scalar.activation(out=cact[:, :], in_=cact[:, :], func=AF.Silu), R=[cst_b], W=[cst_b])
        cact3 = cact[:, :].rearrange("p (kc i) -> p kc i", i=2)
        mod_ps, mod_pb = bank()
        for l in range(depth):
            for g in range(24):
                i = next_slot()
                sl32 = slots[i][:, :].bitcast(F32).rearrange("p (kc c) -> p kc c", c=256)
                SP.dma(sl32, adaw_d[l].rearrange("(kc p) c -> p kc c", p=128)[:, :, g * 256:(g + 1) * 256],
                       slot_s[i], W=[slot_b[i]])
                for fc in range(2):
                    col = (l * 48 + g * 2 + fc) * 2
                    for kc in range(8):
                        PE.op(lambda kc=kc, fc=fc, col=col, sl32=sl32: nc.tensor.matmul(
                            mod_ps[:, col:col + 2], lhsT=sl32[:, kc, fc * 128:(fc + 1) * 128], rhs=cact3[:, kc, :],
                            start=(kc == 0), stop=(kc == 7)), R=[slot_b[i], cst_b], W=[mod_pb])
        for l in range(depth):
            for i in range(2):
                o = (l * 2 + i) * 48
                src = mod_ps[:, l * 96:(l + 1) * 96].rearrange("p (v i) -> p v i", i=2)[:, :, i]
                DVE.op(lambda o=o, src=src, l=l: nc.vector.tensor_tensor(
                    out=modT[:, o:o + 48], in0=src, in1=pvec[:, l * PVL + 32:l * PVL + 80], op=ALU.add),
                    R=[mod_pb, cst_b], W=[modT_b])
        for l in range(depth):
            for i in range(2):
                o = (l * 2 + i) * 48
                for which in range(2):
                    a = ((l * 2 + i) * 2 + which) * 8
                    DVE.op(lambda o=o, a=a, which=which, l=l: nc.vector.scalar_tensor_tensor(
                        out=modA[:, a:a + 8], in0=modT[:, o + which * 24 + 8:o + which * 24 + 16], scalar=1.0,
                        in1=pvec[:, l * PVL + which * 16:l * PVL + which * 16 + 8], op0=ALU.add, op1=ALU.mult),
                        R=[modT_b, cst_b], W=[modA_b])

        diag_sems = []
        for l in range(depth):
            for c in range(2):
                s_diag = sem("s_diag%d_%d" % (l, c))
                diag_sems.append(s_diag)
                i_ = next_slot()
                for j in range(31):
                    DVE.op(lambda i_=i_, j=j, l=l, c=c: nc.vector.tensor_scalar(
                        out=slots[i_][:, j * 128:(j + 1) * 128], in0=ident,
                        scalar1=pvec[:, l * PVL + 80 + c * 31 + j:l * PVL + 80 + c * 31 + j + 1], scalar2=None, op0=ALU.mult),
                        R=[cst_b], W=[slot_b[i_]])
                scr_b[(l, 37 + c)] = Buf()
                SP.dma(scr_d[l * SLOTS_PER_LAYER + 37 + c][:, 0:3968], slots[i_][:, 0:3968], s_diag,
                       R=[slot_b[i_]], W=[scr_b[(l, 37 + c)]])
        def modA_ap(l, i, which):
            a = ((l * 2 + i) * 2 + which) * 8
            return modA[:, a:a + 8]

        def modB_ap(l, i, which):
            o = (l * 2 + i) * 48 + which * 24
            return modT[:, o:o + 8]

        def seq_init(i):
            for l in range(depth):
                for which in range(2):
                    o = (l * 2 + i) * 48 + which * 24 + 16
                    gv_t, gv_b = small()
                    DVE.op(lambda o=o, l=l, which=which, gv_t=gv_t: nc.vector.tensor_tensor(
                        out=gv_t[:, 0:8], in0=modT[:, o:o + 8],
                        in1=pvec[:, l * PVL + 8 + which * 16:l * PVL + 16 + which * 16], op=ALU.mult),
                        R=[modT_b, cst_b], W=[gv_b])
                    for half in range(2):
                        pb_ap, pb_b = bank()
                        for q in range(4):
                            kc = half * 4 + q
                            vb_t, vb_b = scratch()
                            DVE.op(lambda vb_t=vb_t, gv_t=gv_t, kc=kc: nc.vector.tensor_scalar(
                                out=vb_t[:, 0:128], in0=ones, scalar1=gv_t[:, kc:kc + 1], scalar2=None, op0=ALU.mult),
                                R=[cst_b, gv_b], W=[vb_b])
                            PE.op(lambda vb_t=vb_t, q=q, pb_ap=pb_ap: nc.tensor.matmul(
                                pb_ap[:, q * 128:(q + 1) * 128], lhsT=vb_t[:, 0:128], rhs=ident, start=True, stop=True),
                                R=[vb_b, cst_b], W=[pb_b])
                        ACT.op(lambda l=l, which=which, half=half, pb_ap=pb_ap: nc.scalar.copy(
                            out=Grow[:, l * 2 + which, half * 512:(half + 1) * 512], in_=pb_ap),
                            R=[pb_b], W=[Grow_b[l * 2 + which]])
            for l in range(depth):
                for p in range(2):
                    DVE.op(lambda l=l, p=p: nc.vector.memset(Cst[:, l, p, :], 0.0), W=[Cst_b[l][p]])
                    DVE.op(lambda l=l, p=p: nc.vector.memset(Cbf[:, l, p, :], 0.0), W=[Cbf_b[l][p]])
                    DVE.op(lambda l=l, p=p: nc.vector.memset(Sst[:, l, p, :], 0.0), W=[Sst_b[l][p]])
                    DVE.op(lambda l=l, p=p: nc.vector.memset(Sbf[:, l, p, :], 0.0), W=[Sbf_b[l][p]])
                    DVE.op(lambda l=l, p=p: nc.vector.memset(zch[:, l, p, :], 0.0), W=[zch_b[l][p]])
                    DVE.op(lambda l=l, p=p: nc.vector.memset(zsh[:, l, p, :], 0.0), W=[zsh_b[l][p]])
                for ch in range(44):
                    pass
                DVE.op(lambda l=l: nc.vector.memset(halo[:, l, :, :], 0.0), W=halo_b[l])

        def prenorm(l, i, which):
            A = modA_ap(l, i, which)
            Bv = modB_ap(l, i, which)
            for s in range(NS):
                xs = xt[:, s, :]
                xn_t, xn_bb = xn[s % 2], xn_b[s % 2]
                st_t, st_b = small()
                ACT.op(lambda xn_t=xn_t, xs=xs, st_t=st_t: nc.scalar.activation(
                    out=xn_t[:, :], in_=xs, func=AF.Square, accum_out=st_t[:, 0:1]), R=[xt_b[s]], W=[xn_bb, st_b])
                ACT.op(lambda st_t=st_t: nc.scalar.activation(
                    out=st_t[:, 1:2], in_=st_t[:, 0:1], func=AF.Sqrt, scale=1.0 / D, bias=eps_ap), R=[st_b, cst_b], W=[st_b])
                DVE.op(lambda st_t=st_t: nc.vector.reciprocal(out=st_t[:, 2:3], in_=st_t[:, 1:2]), R=[st_b], W=[st_b])
                DVE.op(lambda xn_t=xn_t, xs=xs, st_t=st_t: nc.vector.tensor_scalar(
                    out=xn_t[:, :], in0=xs, scalar1=st_t[:, 2:3], scalar2=None, op0=ALU.mult),
                    R=[xt_b[s], st_b], W=[xn_bb])
                for half in range(2):
                    pb_ap, pb_b = bank()
                    for q in range(4):
                        kc = half * 4 + q
                        PE.op(lambda xn_t=xn_t, kc=kc, q=q, pb_ap=pb_ap: nc.tensor.transpose(
                            pb_ap[:, q * 128:(q + 1) * 128], xn_t[:, kc * 128:(kc + 1) * 128], ident),
                            R=[xn_bb, cst_b], W=[pb_b])
                    for q in range(4):
                        kc = half * 4 + q
                        if half == 0:
                            ACT.op(lambda kc=kc, q=q, pb_ap=pb_ap, s=s: nc.scalar.activation(
                                out=hT[:, kc, s * 128:(s + 1) * 128], in_=pb_ap[:, q * 128:(q + 1) * 128],
                                func=AF.Identity, scale=A[:, kc:kc + 1], bias=Bv[:, kc:kc + 1]),
                                R=[pb_b, modA_b, modT_b], W=[hT_b[s][half]])
                        else:
                            DVE.op(lambda kc=kc, q=q, pb_ap=pb_ap, s=s: nc.vector.tensor_scalar(
                                out=hT[:, kc, s * 128:(s + 1) * 128], in0=pb_ap[:, q * 128:(q + 1) * 128],
                                scalar1=A[:, kc:kc + 1], scalar2=Bv[:, kc:kc + 1], op0=ALU.mult, op1=ALU.add),
                                R=[pb_b, modA_b, modT_b], W=[hT_b[s][half]])

        epsc = sb("epsc", [128, 2])
        eps_b = Buf()
        DVE.op(lambda: nc.vector.memset(epsc[:, 0:1], EPS), W=[cst_b])
        DVE.op(lambda: nc.vector.memset(epsc[:, 1:2], 1.0), W=[cst_b])
        eps_ap = epsc[:, 0:1]
        one_ap = epsc[:, 1:2]

        def fgroup(wt, wb, c0, m, pb_ap, pb_b, s_lo=0, s_hi=NS):
            w3 = wt[:, :].rearrange("p (kc c) -> p kc c", c=512)
            n0, n1 = s_lo * 128, s_hi * 128
            for kc in range(8):
                PE.op(lambda kc=kc: nc.tensor.matmul(pb_ap[0:m, n0:n1], lhsT=w3[:, kc, c0:c0 + m], rhs=hT[:, kc, n0:n1],
                                                     start=(kc == 0), stop=(kc == 7)),
                      R=[wb] + [b_ for s_ in range(s_lo, s_hi) for b_ in hT_b[s_]], W=[pb_b])

        def tgroup(wt, wb, c0, n, s, pb_ap, pb_b):
            w3 = wt[:, :].rearrange("p (kc c) -> p kc c", c=512)
            for kc in range(8):
                PE.op(lambda kc=kc: nc.tensor.matmul(pb_ap[:, 0:n], lhsT=hT[:, kc, s * 128:(s + 1) * 128],
                                                     rhs=w3[:, kc, c0:c0 + n], start=(kc == 0), stop=(kc == 7)),
                      R=[wb] + hT_b[s], W=[pb_b])

        def token_mixer(l, i):
            pv = l * PVL
            br = l * BRL
            prenorm(l, i, 0)
            first = dbg and not dbg_state
            if first:
                dbg_state.append(1)
                dbg_dump("hT", hT[:, :, :], [b_ for x_ in hT_b for b_ in x_])
            if DBG_LEVEL < 1.1:
                return
            wt, wb = get_slot(l, 0)
            for nm, cbase, scale in (("mlq", 0, 0.125), ("mlk", 256, 1.0)):
                for c in range(2):
                    pb_ap, pb_b = bank()
                    fgroup(wt, wb, cbase + c * 128, 128, pb_ap, pb_b)
                    ACT.op(lambda nm=nm, c=c, pb_ap=pb_ap, scale=scale: nc.scalar.activation(
                        out=fq[nm][0][:, c, :], in_=pb_ap[:, 0:T], func=AF.Copy, scale=scale), R=[pb_b], W=[fq[nm][1][c]])
            for s in range(NS):
                pb_ap, pb_b = bank()
                tgroup(wt, wb, 256, 256, s, pb_ap, pb_b)
                ACT.op(lambda s=s, pb_ap=pb_ap: nc.scalar.copy(out=tk["mlk"][0][:, s, :], in_=pb_ap[:, 0:256]),
                       R=[pb_b], W=[tk["mlk"][1][s]])
            if DBG_LEVEL < 1.2:
                return
            wt, wb = get_slot(l, 1)
            for s in range(NS):
                pb_ap, pb_b = bank()
                tgroup(wt, wb, 0, 512, s, pb_ap, pb_b)
                DVE.op(lambda s=s, pb_ap=pb_ap: nc.vector.tensor_copy(out=tk["mlv"][0][:, s, :], in_=pb_ap[:, 0:256]),
                       R=[pb_b], W=[tk["mlv"][1][s]])
                ACT.op(lambda s=s, pb_ap=pb_ap: nc.scalar.activation(
                    out=tk["sigo"][0][:, s, :], in_=pb_ap[:, 256:512], func=AF.Sigmoid), R=[pb_b], W=[tk["sigo"][1][s]])
            if DBG_LEVEL < 1.4:
                return
            wt, wb = get_slot(l, 2)
            for s in range(NS):
                pb_ap, pb_b = bank()
                tgroup(wt, wb, 0, 8, s, pb_ap, pb_b)
                DVE.op(lambda s=s, pb_ap=pb_ap: nc.vector.tensor_tensor(
                    out=iff[:, s, :], in0=pb_ap[:, 0:8], in1=brow[:, br:br + 8], op=ALU.add),
                    R=[pb_b, cst_b], W=[iff_b[s]])
            pb_ap, pb_b = bank()
            fgroup(wt, wb, 8, 16, pb_ap, pb_b)
            ACT.op(lambda pb_ap=pb_ap: nc.scalar.copy(out=gaT[:, :], in_=pb_ap[0:16, 0:T]), R=[pb_b], W=[gaT_b])
            if DBG_LEVEL < 1.6:
                return
            wt, wb = get_slot(l, 3)
            for nm, cbase, scale in (("gq", 0, 0.125), ("gk", 256, 1.0)):
                for c in range(2):
                    pb_ap, pb_b = bank()
                    fgroup(wt, wb, cbase + c * 128, 128, pb_ap, pb_b)
                    ACT.op(lambda nm=nm, c=c, pb_ap=pb_ap, scale=scale: nc.scalar.activation(
                        out=fq[nm][0][:, c, :], in_=pb_ap[:, 0:T], func=AF.Copy, scale=scale), R=[pb_b], W=[fq[nm][1][c]])
            for s in range(NS):
                pb_ap, pb_b = bank()
                tgroup(wt, wb, 256, 256, s, pb_ap, pb_b)
                ACT.op(lambda s=s, pb_ap=pb_ap: nc.scalar.copy(out=tk["glk"][0][:, s, :], in_=pb_ap[:, 0:256]),
                       R=[pb_b], W=[tk["glk"][1][s]])
            wt, wb = get_slot(l, 4)
            for s in range(NS):
                pb_ap, pb_b = bank()
                tgroup(wt, wb, 0, 512, s, pb_ap, pb_b)
                DVE.op(lambda s=s, pb_ap=pb_ap: nc.vector.tensor_copy(out=tk["gv"][0][:, s, :], in_=pb_ap[:, 0:256]),
                       R=[pb_b], W=[tk["gv"][1][s]])
                ACT.op(lambda s=s, pb_ap=pb_ap: nc.scalar.activation(
                    out=tk["sgr"][0][:, s, :], in_=pb_ap[:, 256:512], func=AF.Silu), R=[pb_b], W=[tk["sgr"][1][s]])
            if DBG_LEVEL < 1.8:
                return
            wt, wb = get_slot(l, 5)
            for c in range(2):
                pg_ap, pg_b = bank()
                fgroup(wt, wb, 256 + c * 128, 128, pg_ap, pg_b)
                pa_ap, pa_b = bank()
                fgroup(wt, wb, c * 128, 128, pa_ap, pa_b)
                sg_t, sg_b = scratch()
                ACT.op(lambda pg_ap=pg_ap, sg_t=sg_t: nc.scalar.activation(out=sg_t[:, 0:T], in_=pg_ap[:, 0:T], func=AF.Sigmoid),
                       R=[pg_b], W=[sg_b])
                ACT.op(lambda c=c: nc.scalar.copy(out=zc[:, c, 0:30], in_=zch[:, l, c, :]), R=[zch_b[l][c]], W=[zc_b[c]])
                DVE.op(lambda c=c, pa_ap=pa_ap, sg_t=sg_t: nc.vector.tensor_tensor(
                    out=zc[:, c, 30:30 + T], in0=pa_ap[:, 0:T], in1=sg_t[:, 0:T], op=ALU.mult),
                    R=[pa_b, sg_b], W=[zc_b[c]])
                ACT.op(lambda c=c: nc.scalar.copy(out=zch[:, l, c, :], in_=zc[:, c, T:T + 30]), R=[zc_b[c]], W=[zch_b[l][c]])
            wt, wb = get_slot(l, 6)
            for c in range(2):
                pb_ap, pb_b = bank()
                fgroup(wt, wb, c * 128, 128, pb_ap, pb_b)
                ACT.op(lambda c=c, pb_ap=pb_ap: nc.scalar.copy(out=bgs[:, c, :], in_=pb_ap[:, 0:T]), R=[pb_b], W=[bgs_b[c]])
                pb_ap, pb_b = bank()
                fgroup(wt, wb, 256 + c * 128, 128, pb_ap, pb_b)
                ACT.op(lambda c=c, pb_ap=pb_ap: nc.scalar.copy(out=cgs[:, c, :], in_=pb_ap[:, 0:T]), R=[pb_b], W=[cgs_b[c]])
            wt, wb = get_slot(l, 7)
            for c in range(2):
                pb_ap, pb_b = bank()
                fgroup(wt, wb, c * 128, 128, pb_ap, pb_b)
                ACT.op(lambda c=c: nc.scalar.copy(out=zs[:, c, 0:2], in_=zsh[:, l, c, :]), R=[zsh_b[l][c]], W=[zs_b[c]])
                DVE.op(lambda c=c, pb_ap=pb_ap: nc.vector.tensor_tensor(
                    out=zs[:, c, 2:2 + T], in0=pb_ap[:, 0:T], in1=cgs[:, c, :], op=ALU.mult),
                    R=[pb_b, cgs_b[c]], W=[zs_b[c]])
                ACT.op(lambda c=c: nc.scalar.copy(out=zsh[:, l, c, :], in_=zs[:, c, T:T + 2]), R=[zs_b[c]], W=[zsh_b[l][c]])

            if DBG_LEVEL < 3:
                return
            for c in range(2):
                y_t, y_b = scratch()
                w0 = pvec[:, pv + 148 + c * 3:pv + 148 + c * 3 + 3]
                DVE.op(lambda c=c, y_t=y_t, w0=w0: nc.vector.tensor_scalar(
                    out=y_t[:, 0:T], in0=zs[:, c, 2:2 + T], scalar1=w0[:, 2:3], scalar2=None, op0=ALU.mult),
                    R=[zs_b[c], cst_b], W=[y_b])
                for j in (1, 0):
                    DVE.op(lambda c=c, y_t=y_t, w0=w0, j=j: nc.vector.scalar_tensor_tensor(
                        out=y_t[:, 0:T], in0=zs[:, c, j:j + T], scalar=w0[:, j:j + 1], in1=y_t[:, 0:T],
                        op0=ALU.mult, op1=ALU.add), R=[zs_b[c], cst_b, y_b], W=[y_b])
                DVE.op(lambda c=c, y_t=y_t: nc.vector.tensor_tensor(
                    out=zT["sc"][0][:, c, :], in0=y_t[:, 0:T], in1=bgs[:, c, :], op=ALU.mult),
                    R=[y_b, bgs_b[c]], W=[zT["sc"][1][c]])

            for c in range(2):
                wt, wb = get_slot(l, 37 + c)
                pb_ap, pb_b = bank()
                for j in range(31):
                    PE.op(lambda j=j, c=c, wt=wt, pb_ap=pb_ap: nc.tensor.matmul(
                        pb_ap[:, 0:T], lhsT=wt[:, j * 128:(j + 1) * 128], rhs=zc[:, c, j:j + T],
                        start=(j == 0), stop=(j == 30)), R=[wb, zc_b[c]], W=[pb_b])
                ACT.op(lambda c=c, pb_ap=pb_ap: nc.scalar.activation(
                    out=uconv[:, c, :], in_=pb_ap[:, 0:T], func=AF.Identity, bias=pvec[:, pv + 142 + c:pv + 143 + c]),
                    R=[pb_b, cst_b], W=[uconv_b[c]])
            usq_t, usq_b = mtile("usq", [128, 2, T])
            ACT.op(lambda: nc.scalar.activation(out=usq_t[:, :, :], in_=uconv[:, :, :], func=AF.Square),
                   R=uconv_b, W=[usq_b])
            psum_ap, psum_b = bank()
            psq_ap, psq_b = bank()
            for c in range(2):
                PE.op(lambda c=c: nc.tensor.matmul(psum_ap[:, 0:T], lhsT=ones, rhs=uconv[:, c, :], start=(c == 0), stop=(c == 1)),
                      R=[cst_b, uconv_b[c]], W=[psum_b])
            for c in range(2):
                PE.op(lambda c=c: nc.tensor.matmul(psq_ap[:, 0:T], lhsT=ones, rhs=usq_t[:, c, :], start=(c == 0), stop=(c == 1)),
                      R=[cst_b, usq_b], W=[psq_b])
            mean_t, mean_b = scratch()
            msq_t, msq_b = scratch()
            rs_t, rs_b = scratch()
            ACT.op(lambda: nc.scalar.activation(out=mean_t[:, 0:T], in_=psum_ap[:, 0:T], func=AF.Copy, scale=1.0 / MIX),
                   R=[psum_b], W=[mean_b])
            ACT.op(lambda: nc.scalar.activation(out=msq_t[:, 0:T], in_=psum_ap[:, 0:T], func=AF.Square, scale=1.0 / MIX),
                   R=[psum_b], W=[msq_b])
            DVE.op(lambda: nc.vector.scalar_tensor_tensor(
                out=rs_t[:, 0:T], in0=psq_ap[:, 0:T], scalar=1.0 / MIX, in1=msq_t[:, 0:T], op0=ALU.mult, op1=ALU.subtract),
                R=[psq_b, msq_b], W=[rs_b])
            ACT.op(lambda: nc.scalar.activation(out=rs_t[:, 0:T], in_=rs_t[:, 0:T], func=AF.Ln, bias=eps_ap),
                   R=[rs_b, cst_b], W=[rs_b])
            ACT.op(lambda: nc.scalar.activation(out=rs_t[:, 0:T], in_=rs_t[:, 0:T], func=AF.Exp, scale=-0.5),
                   R=[rs_b], W=[rs_b])
            for c in range(2):
                d_t, d_b = scratch()
                DVE.op(lambda c=c, d_t=d_t: nc.vector.tensor_tensor(
                    out=d_t[:, 0:T], in0=uconv[:, c, :], in1=mean_t[:, 0:T], op=ALU.subtract),
                    R=[uconv_b[c], mean_b], W=[d_b])
                DVE.op(lambda c=c, d_t=d_t: nc.vector.tensor_tensor(
                    out=d_t[:, 0:T], in0=d_t[:, 0:T], in1=rs_t[:, 0:T], op=ALU.mult), R=[d_b, rs_b], W=[d_b])
                ACT.op(lambda c=c, d_t=d_t: nc.scalar.activation(
                    out=zT["conf"][0][:, c, :], in_=d_t[:, 0:T], func=AF.Silu,
                    scale=pvec[:, pv + 144 + c:pv + 145 + c], bias=pvec[:, pv + 146 + c:pv + 147 + c]),
                    R=[d_b, cst_b], W=[zT["conf"][1][c]])

            if DBG_LEVEL >= 4:
                mixers(l, i)
            if first:
                for nm in ("ml", "gla", "conf", "sc"):
                    dbg_dump("z_" + nm, zT[nm][0][:, :, :], zT[nm][1])
            if DBG_LEVEL < 5:
                return
            merge_and_out(l, i, pv, first)

        def mixers(l, i):
            pv = l * PVL
            br = l * BRL
            def mix_gen(s):
                tok = slice(s * 128, (s + 1) * 128)
                e_t, e_b = small()
                ACT.op(lambda e_t=e_t: nc.scalar.activation(out=e_t[:, 0:4], in_=iff[:, s, 4:8], func=AF.Exp, scale=-1.0),
                       R=[iff_b[s]], W=[e_b])
                ACT.op(lambda e_t=e_t: nc.scalar.activation(out=e_t[:, 4:8], in_=e_t[:, 0:4], func=AF.Ln, bias=one_ap),
                       R=[e_b, cst_b], W=[e_b])
                cs_ap, cs_b = bank()
                PE.op(lambda e_t=e_t: nc.tensor.matmul(cs_ap[:, 0:4], lhsT=Umat, rhs=e_t[:, 4:8], start=True, stop=True),
                      R=[cst_b, e_b], W=[cs_b])
                PE.op(lambda e_t=e_t: nc.tensor.matmul(cs_ap[:, 8:12], lhsT=ones, rhs=e_t[:, 4:8], start=True, stop=True),
                      R=[cst_b, e_b], W=[cs_b])
                la_ap, la_b = bank()
                PE.op(lambda: nc.tensor.matmul(la_ap[:, 0:256], lhsT=gaT[:, tok], rhs=w2[:, l * 256:(l + 1) * 256],
                                               start=True, stop=True), R=[gaT_b, cst_b], W=[la_b])
                w_t, w_b = small()
                DVE.op(lambda w_t=w_t: nc.vector.tensor_tensor(out=w_t[:, 0:4], in0=cs_ap[:, 0:4], in1=iff[:, s, 0:4], op=ALU.add),
                       R=[cs_b, iff_b[s]], W=[w_b])
                ACT.op(lambda w_t=w_t: nc.scalar.activation(out=w_t[:, 4:8], in_=w_t[:, 0:4], func=AF.Exp), R=[w_b], W=[w_b])
                ACT.op(lambda w_t=w_t: nc.scalar.activation(out=w_t[:, 8:12], in_=cs_ap[:, 0:4], func=AF.Exp, scale=-1.0),
                       R=[cs_b], W=[w_b])
                ebl_t, ebl_b = small()
                for hh in range(2):
                    ACT.op(lambda hh=hh, ebl_t=ebl_t: nc.scalar.activation(
                        out=ebl_t[hh * 64:(hh + 1) * 64, 0:2],
                        in_=cs_ap[hh * 64:(hh + 1) * 64, 8:12].rearrange("p (a b) -> p a b", b=2)[:, :, hh],
                        func=AF.Exp, scale=-1.0), R=[cs_b], W=[ebl_b])
                xla_t, xla_b = mtile("xla", [128, 256])
                DVE.op(lambda xla_t=xla_t: nc.vector.tensor_tensor(
                    out=xla_t[:, :], in0=la_ap[:, 0:256], in1=brow[:, br + 520:br + 776], op=ALU.add),
                    R=[la_b, cst_b], W=[xla_b])
                ACT.op(lambda xla_t=xla_t: nc.scalar.activation(out=xla_t[:, :], in_=xla_t[:, :], func=AF.Exp, scale=-1.0),
                       R=[xla_b], W=[xla_b])
                ACT.op(lambda xla_t=xla_t: nc.scalar.activation(out=xla_t[:, :], in_=xla_t[:, :], func=AF.Ln, bias=one_ap),
                       R=[xla_b, cst_b], W=[xla_b])
                vx_t, vx_b = mtile("vext", [128, 4, 72], BF16)
                DVE.op(lambda vx_t=vx_t, w_t=w_t: nc.vector.tensor_tensor(
                    out=vx_t[:, :, 0:64], in0=tk["mlv"][0][:, s, :].rearrange("p (h e) -> p h e", e=64),
                    in1=w_t[:, 4:8].unsqueeze(2).to_broadcast([128, 4, 64]), op=ALU.mult),
                    R=[tk["mlv"][1][s], w_b], W=[vx_b])
                DVE.op(lambda vx_t=vx_t, w_t=w_t: nc.vector.tensor_copy(out=vx_t[:, :, 64:65], in_=w_t[:, 4:8].unsqueeze(2)),
                       R=[w_b], W=[vx_b])
                nb_ap, nb_b = bank()
                for p in range(2):
                    PE.op(lambda p=p, xla_t=xla_t: nc.tensor.matmul(
                        nb_ap[:, p * 128:(p + 1) * 128], lhsT=xla_t[:, p * 128:(p + 1) * 128], rhs=U16, start=True, stop=True),
                        R=[xla_b, cst_b], W=[nb_b])
                PE.op(lambda xla_t=xla_t: nc.tensor.matmul(nb_ap[:, 256:512], lhsT=SU16, rhs=xla_t[:, :], start=True, stop=True),
                      R=[xla_b, cst_b], W=[nb_b])
                yield 'a'
                st_ap, st_b = bank()
                for h in range(4):
                    p, hh = h // 2, h % 2
                    rows = slice(hh * 64, (hh + 1) * 64)
                    PE.op(lambda h=h, p=p, rows=rows: nc.tensor.matmul(
                        st_ap[:, h * 128:(h + 1) * 128], lhsT=fq["mlk"][0][rows, p, tok], rhs=fq["mlq"][0][rows, p, tok],
                        start=True, stop=True), R=[fq["mlk"][1][p], fq["mlq"][1][p]], W=[st_b], drain=True)
                pt_t, pt_b = mtile("PT", [128, 4, 128], BF16)
                DVE.op(lambda pt_t=pt_t: nc.vector.tensor_tensor(
                    out=pt_t[:, :, :], in0=st_ap.rearrange("p (h t) -> p h t", t=128), in1=mask4, op=ALU.mult),
                    R=[st_b, cst_b], W=[pt_b])
                yield 'a'
                eq_t, eq_b = mtile("eq", [128, 2, 128])
                ek_t, ek_b = mtile("ek", [128, 2, 128])
                nb3 = nb_ap[:, 0:256].rearrange("p (a t) -> p a t", t=128)
                ACT.op(lambda eq_t=eq_t: nc.scalar.activation(out=eq_t[:, :, :], in_=nb3, func=AF.Exp, scale=-1.0),
                       R=[nb_b], W=[eq_b])
                ACT.op(lambda ek_t=ek_t: nc.scalar.activation(out=ek_t[:, :, :], in_=nb3, func=AF.Exp), R=[nb_b], W=[ek_b])
                ebg_t, ebg_b = small()
                ACT.op(lambda ebg_t=ebg_t: nc.scalar.activation(out=ebg_t[:, 0:2], in_=nb3[:, :, 127], func=AF.Exp, scale=-1.0),
                       R=[nb_b], W=[ebg_b])
                er_t, er_b = mtile("erem", [128, 256])
                ACT.op(lambda er_t=er_t: nc.scalar.activation(out=er_t[:, :], in_=nb_ap[:, 256:512], func=AF.Exp, scale=-1.0),
                       R=[nb_b], W=[er_b])
                qd_t, qd_b = mtile("qd", [128, 2, 128], BF16)
                ki_t, ki_b = mtile("ki", [128, 2, 128], BF16)
                kd_t, kd_b = mtile("kd", [128, 256], BF16)
                DVE.op(lambda qd_t=qd_t, eq_t=eq_t: nc.vector.tensor_tensor(
                    out=qd_t[:, :, :], in0=fq["gq"][0][:, :, tok], in1=eq_t[:, :, :], op=ALU.mult),
                    R=fq["gq"][1] + [eq_b], W=[qd_b])
                DVE.op(lambda ki_t=ki_t, ek_t=ek_t: nc.vector.tensor_tensor(
                    out=ki_t[:, :, :], in0=fq["gk"][0][:, :, tok], in1=ek_t[:, :, :], op=ALU.mult),
                    R=fq["gk"][1] + [ek_b], W=[ki_b])
                DVE.op(lambda kd_t=kd_t, er_t=er_t: nc.vector.tensor_tensor(
                    out=kd_t[:, :], in0=tk["glk"][0][:, s, :], in1=er_t[:, :], op=ALU.mult),
                    R=[tk["glk"][1][s], er_b], W=[kd_b])
                yield 'endA'
                mo_ap, mo_b = bank()
                mo3 = mo_ap[:, 0:288].rearrange("p (h e) -> p h e", e=72)
                for h in range(4):
                    p, hh = h // 2, h % 2
                    rows = slice(hh * 64, (hh + 1) * 64)
                    PE.op(lambda h=h, pt_t=pt_t, vx_t=vx_t: nc.tensor.matmul(
                        mo3[:, h, 0:65], lhsT=pt_t[:, h, :], rhs=vx_t[:, h, 0:65], start=True, stop=False),
                        R=[pt_b, vx_b], W=[mo_b], drain=True)
                    PE.op(lambda h=h, p=p, rows=rows: nc.tensor.matmul(
                        mo3[:, h, 0:65], lhsT=fq["mlq"][0][rows, p, tok], rhs=Cbf[rows, l, p, 0:65], start=False, stop=True),
                        R=[fq["mlq"][1][p], Cbf_b[l][p]], W=[mo_b], drain=True)
                at_ap, at_b = bank()
                for h in range(4):
                    p, hh = h // 2, h % 2
                    rows = slice(hh * 64, (hh + 1) * 64)
                    PE.op(lambda h=h, p=p, rows=rows, ki_t=ki_t, qd_t=qd_t: nc.tensor.matmul(
                        at_ap[:, h * 128:(h + 1) * 128], lhsT=ki_t[rows, p, :], rhs=qd_t[rows, p, :], start=True, stop=True),
                        R=[ki_b, qd_b], W=[at_b], drain=True)
                at_t, at_tb = mtile("AT", [128, 4, 128], BF16)
                DVE.op(lambda at_t=at_t: nc.vector.tensor_tensor(
                    out=at_t[:, :, :], in0=at_ap.rearrange("p (h t) -> p h t", t=128), in1=mask4, op=ALU.mult),
                    R=[at_b, cst_b], W=[at_tb])
                cu_ap, cu_b = bank()
                cu3 = cu_ap[:, 0:144].rearrange("p (a e) -> p a e", e=72)
                for h in range(4):
                    p, hh = h // 2, h % 2
                    rows = slice(hh * 64, (hh + 1) * 64)
                    PE.op(lambda h=h, p=p, rows=rows, vx_t=vx_t: nc.tensor.matmul(
                        cu3[rows, p, 0:65], lhsT=tk["mlk"][0][:, s, h * 64:(h + 1) * 64], rhs=vx_t[:, h, 0:65],
                        start=True, stop=True), R=[tk["mlk"][1][s], vx_b], W=[cu_b], drain=True)
                yield 'endB'
                r_t, r_b = small()
                DVE.op(lambda r_t=r_t, w_t=w_t: nc.vector.tensor_tensor(
                    out=r_t[:, 0:4], in0=mo3[:, :, 64], in1=w_t[:, 8:12], op=ALU.mult), R=[mo_b, w_b], W=[r_b])
                DVE.op(lambda r_t=r_t: nc.vector.tensor_scalar(
                    out=r_t[:, 12:16], in0=r_t[:, 0:4], scalar1=-1.0, scalar2=1.0, op0=ALU.mult, op1=ALU.max), R=[r_b], W=[r_b])
                DVE.op(lambda r_t=r_t: nc.vector.tensor_tensor(
                    out=r_t[:, 0:4], in0=r_t[:, 0:4], in1=r_t[:, 12:16], op=ALU.max), R=[r_b], W=[r_b])
                DVE.op(lambda r_t=r_t: nc.vector.reciprocal(out=r_t[:, 4:8], in_=r_t[:, 0:4]), R=[r_b], W=[r_b])
                DVE.op(lambda r_t=r_t, w_t=w_t: nc.vector.tensor_tensor(
                    out=r_t[:, 8:12], in0=r_t[:, 4:8], in1=w_t[:, 8:12], op=ALU.mult), R=[r_b, w_b], W=[r_b])
                hm_t, hm_b = mtile("hm", [128, 4, 64])
                DVE.op(lambda hm_t=hm_t, r_t=r_t: nc.vector.tensor_tensor(
                    out=hm_t[:, :, :], in0=mo3[:, :, 0:64], in1=r_t[:, 8:12].unsqueeze(2).to_broadcast([128, 4, 64]), op=ALU.mult),
                    R=[mo_b, r_b], W=[hm_b])
                hsq_t, hsq_b = mtile("hsq", [128, 4, 64])
                ACT.op(lambda hm_t=hm_t, hsq_t=hsq_t: nc.scalar.activation(out=hsq_t[:, :, :], in_=hm_t[:, :, :], func=AF.Square),
                       R=[hm_b], W=[hsq_b])
                n_t, n_b = small()
                DVE.op(lambda n_t=n_t, hsq_t=hsq_t: nc.vector.tensor_reduce(out=n_t[:, 0:4], in_=hsq_t[:, :, :], axis=AX.X, op=ALU.add),
                       R=[hsq_b], W=[n_b])
                ACT.op(lambda n_t=n_t: nc.scalar.activation(out=n_t[:, 4:8], in_=n_t[:, 0:4], func=AF.Sqrt, scale=1.0 / 64, bias=eps_ap),
                       R=[n_b, cst_b], W=[n_b])
                DVE.op(lambda n_t=n_t: nc.vector.reciprocal(out=n_t[:, 8:12], in_=n_t[:, 4:8]), R=[n_b], W=[n_b])
                og_t, og_b = mtile("og", [128, 256])
                DVE.op(lambda og_t=og_t: nc.vector.tensor_tensor(
                    out=og_t[:, :], in0=tk["sigo"][0][:, s, :], in1=brow[:, br + 8:br + 264], op=ALU.mult),
                    R=[tk["sigo"][1][s], cst_b], W=[og_b])
                DVE.op(lambda hm_t=hm_t, n_t=n_t: nc.vector.tensor_tensor(
                    out=hm_t[:, :, :], in0=hm_t[:, :, :], in1=n_t[:, 8:12].unsqueeze(2).to_broadcast([128, 4, 64]), op=ALU.mult),
                    R=[hm_b, n_b], W=[hm_b])
                zt_t, zt_b = mtile("ztok", [128, 256])
                DVE.op(lambda hm_t=hm_t, og_t=og_t, zt_t=zt_t: nc.vector.tensor_tensor(
                    out=zt_t[:, :], in0=hm_t[:, :, :].rearrange("p h e -> p (h e)"), in1=og_t[:, :], op=ALU.mult),
                    R=[hm_b, og_b], W=[zt_b])
                tp_ap, tp_b = bank()
                for c in range(2):
                    PE.op(lambda c=c, zt_t=zt_t: nc.tensor.transpose(tp_ap[:, c * 128:(c + 1) * 128], zt_t[:, c * 128:(c + 1) * 128], ident),
                          R=[zt_b, cst_b], W=[tp_b])
                ACT.op(lambda: nc.scalar.copy(out=zT["ml"][0][:, :, tok], in_=tp_ap[:, 0:256].rearrange("p (c t) -> p c t", t=128)),
                       R=[tp_b], W=[zT["ml"][1][s]])
                for p in range(2):
                    ct_t, ct_b = mtile("ctmp", [128, 72])
                    DVE.op(lambda p=p, ct_t=ct_t: nc.vector.tensor_tensor(
                        out=ct_t[:, 0:65], in0=cu3[:, p, 0:65], in1=Cst[:, l, p, 0:65], op=ALU.add),
                        R=[cu_b, Cst_b[l][p]], W=[ct_b])
                    DVE.op(lambda p=p, ct_t=ct_t, ebl_t=ebl_t: nc.vector.tensor_scalar(
                        out=Cst[:, l, p, 0:65], in0=ct_t[:, 0:65], scalar1=ebl_t[:, p:p + 1], scalar2=None, op0=ALU.mult),
                        R=[ct_b, ebl_b], W=[Cst_b[l][p]])
                    ACT.op(lambda p=p, ct_t=ct_t, ebl_t=ebl_t: nc.scalar.activation(
                        out=Cbf[:, l, p, 0:65], in_=ct_t[:, 0:65], func=AF.Copy, scale=ebl_t[:, p:p + 1]),
                        R=[ct_b, ebl_b], W=[Cbf_b[l][p]])
                yield 'endC'
                go_ap, go_b = bank()
                for h in range(4):
                    p, hh = h // 2, h % 2
                    rows = slice(hh * 64, (hh + 1) * 64)
                    PE.op(lambda h=h, at_t=at_t: nc.tensor.matmul(
                        go_ap[:, h * 64:(h + 1) * 64], lhsT=at_t[:, h, :], rhs=tk["gv"][0][:, s, h * 64:(h + 1) * 64],
                        start=True, stop=False), R=[at_tb, tk["gv"][1][s]], W=[go_b], drain=True)
                    PE.op(lambda h=h, p=p, rows=rows, qd_t=qd_t: nc.tensor.matmul(
                        go_ap[:, h * 64:(h + 1) * 64], lhsT=qd_t[rows, p, :], rhs=Sbf[rows, l, p, :], start=False, stop=True),
                        R=[qd_b, Sbf_b[l][p]], W=[go_b], drain=True)
                su_ap, su_b = bank()
                su3 = su_ap[:, 0:128].rearrange("p (a e) -> p a e", e=64)
                for h in range(4):
                    p, hh = h // 2, h % 2
                    rows = slice(hh * 64, (hh + 1) * 64)
                    PE.op(lambda h=h, p=p, rows=rows, kd_t=kd_t: nc.tensor.matmul(
                        su3[rows, p, :], lhsT=kd_t[:, h * 64:(h + 1) * 64], rhs=tk["gv"][0][:, s, h * 64:(h + 1) * 64],
                        start=True, stop=True), R=[kd_b, tk["gv"][1][s]], W=[su_b], drain=True)
                gm_t, gm_b = mtile("gm", [128, 4, 64])
                ACT.op(lambda gm_t=gm_t: nc.scalar.copy(out=gm_t[:, :, :], in_=go_ap[:, 0:256].rearrange("p (h e) -> p h e", e=64)),
                       R=[go_b], W=[gm_b])
                gsq_t, gsq_b = mtile("gsq", [128, 4, 64])
                ACT.op(lambda gsq_t=gsq_t: nc.scalar.activation(
                    out=gsq_t[:, :, :], in_=go_ap[:, 0:256].rearrange("p (h e) -> p h e", e=64), func=AF.Square),
                    R=[go_b], W=[gsq_b])
                gn_t, gn_b = small()
                DVE.op(lambda gn_t=gn_t, gsq_t=gsq_t: nc.vector.tensor_reduce(out=gn_t[:, 0:4], in_=gsq_t[:, :, :], axis=AX.X, op=ALU.add),
                       R=[gsq_b], W=[gn_b])
                ACT.op(lambda gn_t=gn_t: nc.scalar.activation(out=gn_t[:, 4:8], in_=gn_t[:, 0:4], func=AF.Sqrt, scale=1.0 / 64, bias=eps_ap),
                       R=[gn_b, cst_b], W=[gn_b])
                DVE.op(lambda gn_t=gn_t: nc.vector.reciprocal(out=gn_t[:, 8:12], in_=gn_t[:, 4:8]), R=[gn_b], W=[gn_b])
                rg_t, rg_b = mtile("rg", [128, 256])
                DVE.op(lambda rg_t=rg_t: nc.vector.tensor_tensor(
                    out=rg_t[:, :], in0=tk["sgr"][0][:, s, :], in1=brow[:, br + 264:br + 520], op=ALU.mult),
                    R=[tk["sgr"][1][s], cst_b], W=[rg_b])
                DVE.op(lambda gm_t=gm_t, gn_t=gn_t: nc.vector.tensor_tensor(
                    out=gm_t[:, :, :], in0=gm_t[:, :, :], in1=gn_t[:, 8:12].unsqueeze(2).to_broadcast([128, 4, 64]), op=ALU.mult),
                    R=[gm_b, gn_b], W=[gm_b])
                zg_t, zg_b = mtile("zgtok", [128, 256])
                DVE.op(lambda gm_t=gm_t, rg_t=rg_t, zg_t=zg_t: nc.vector.tensor_tensor(
                    out=zg_t[:, :], in0=gm_t[:, :, :].rearrange("p h e -> p (h e)"), in1=rg_t[:, :], op=ALU.mult),
                    R=[gm_b, rg_b], W=[zg_b])
                tg_ap, tg_b = bank()
                for c in range(2):
                    PE.op(lambda c=c, zg_t=zg_t: nc.tensor.transpose(tg_ap[:, c * 128:(c + 1) * 128], zg_t[:, c * 128:(c + 1) * 128], ident),
                          R=[zg_b, cst_b], W=[tg_b])
                ACT.op(lambda: nc.scalar.copy(out=zT["gla"][0][:, :, tok], in_=tg_ap[:, 0:256].rearrange("p (c t) -> p c t", t=128)),
                       R=[tg_b], W=[zT["gla"][1][s]])
                for p in range(2):
                    DVE.op(lambda p=p, ebg_t=ebg_t: nc.vector.scalar_tensor_tensor(
                        out=Sst[:, l, p, :], in0=Sst[:, l, p, :], scalar=ebg_t[:, p:p + 1], in1=su3[:, p, :],
                        op0=ALU.mult, op1=ALU.add), R=[Sst_b[l][p], ebg_b, su_b], W=[Sst_b[l][p]])
                    ACT.op(lambda p=p: nc.scalar.copy(out=Sbf[:, l, p, :], in_=Sst[:, l, p, :]), R=[Sst_b[l][p]], W=[Sbf_b[l][p]])

            def run_until(g, tag):
                for t_ in g:
                    if t_ == tag:
                        return True
                return False

            if NS == 2:
                g0, g1 = mix_gen(0), mix_gen(1)
                a0 = a1 = True
                while a0 or a1:
                    if a0:
                        a0 = next(g0) != 'endA'
                    if a1:
                        a1 = next(g1) != 'endA'
                run_until(g0, 'endB')
                run_until(g0, 'endC')
                run_until(g1, 'endB')
                run_until(g0, None)
                run_until(g1, 'endC')
                run_until(g1, None)
            else:
                for s_ in range(NS):
                    run_until(mix_gen(s_), None)


        def merge_and_out(l, i, pv, first):
            wA, wAb = get_slot(l, 16, hold=0)
            wB, wBb = get_slot(l, 17, hold=1)
            wA3 = wA[:, :].rearrange("p (m c) -> p m c", c=1024)
            wB3 = wB[:, :].rearrange("p (m c) -> p m c", c=1024)
            branches = (("ml", wA3, 0, wAb), ("gla", wA3, 2, wAb), ("conf", wB3, 0, wBb), ("sc", wB3, 2, wBb))
            for j in range(8):
                wt, wb = get_slot(l, 8 + j)
                g_t, g_b = gates[j % 2], gates_b[j % 2]
                for b in range(4):
                    pb_ap, pb_b = bank()
                    fgroup(wt, wb, b * 128, 128, pb_ap, pb_b)
                    ACT.op(lambda b=b, pb_ap=pb_ap, g_t=g_t: nc.scalar.activation(
                        out=g_t[:, b, :], in_=pb_ap[:, 0:T], func=AF.Sigmoid, bias=pvec[:, pv + 162 + b * 8 + j:pv + 163 + b * 8 + j]),
                        R=[pb_b, cst_b], W=[g_b])
                acc_t, acc_b = scratch()
                for b, (nm, w3, m0, wbb) in enumerate(branches):
                    pb_ap, pb_b = bank()
                    for kc in range(2):
                        PE.op(lambda kc=kc, nm=nm, w3=w3, m0=m0, pb_ap=pb_ap: nc.tensor.matmul(
                            pb_ap[:, 0:T], lhsT=w3[:, m0 + kc, j * 128:(j + 1) * 128], rhs=zT[nm][0][:, kc, :],
                            start=(kc == 0), stop=(kc == 1)), R=[wbb] + zT[nm][1], W=[pb_b])
                    if b == 0:
                        DVE.op(lambda pb_ap=pb_ap, g_t=g_t, acc_t=acc_t: nc.vector.tensor_tensor(
                            out=acc_t[:, 0:T], in0=pb_ap[:, 0:T], in1=g_t[:, 0, :], op=ALU.mult), R=[pb_b, g_b], W=[acc_b])
                    else:
                        tm_t, tm_b = scratch()
                        if nm == "conf":
                            DVE.op(lambda pb_ap=pb_ap, g_t=g_t, tm_t=tm_t, b=b: nc.vector.scalar_tensor_tensor(
                                out=tm_t[:, 0:T], in0=pb_ap[:, 0:T], scalar=pvec[:, pv + 154 + j:pv + 155 + j], in1=g_t[:, b, :],
                                op0=ALU.add, op1=ALU.mult), R=[pb_b, g_b, cst_b], W=[tm_b])
                        else:
                            DVE.op(lambda pb_ap=pb_ap, g_t=g_t, tm_t=tm_t, b=b: nc.vector.tensor_tensor(
                                out=tm_t[:, 0:T], in0=pb_ap[:, 0:T], in1=g_t[:, b, :], op=ALU.mult), R=[pb_b, g_b], W=[tm_b])
                        if b < 3:
                            POOL.op(lambda tm_t=tm_t, acc_t=acc_t: nc.gpsimd.tensor_tensor(
                                out=acc_t[:, 0:T], in0=acc_t[:, 0:T], in1=tm_t[:, 0:T], op=ALU.add), R=[tm_b, acc_b], W=[acc_b])
                        else:
                            POOL.op(lambda tm_t=tm_t, acc_t=acc_t: nc.gpsimd.tensor_tensor(
                                out=mergedT[:, j, :], in0=acc_t[:, 0:T], in1=tm_t[:, 0:T], op=ALU.add),
                                R=[tm_b, acc_b], W=[merged_b[j]])
            if first:
                dbg_dump("merged", mergedT[:, :, :], merged_b)
                dbg_dump("grow", Grow[:, 0:2, :], Grow_b[0:2])
            if DBG_LEVEL < 6:
                return
            w0, w0b = get_slot(l, 18)
            w1, w1b = get_slot(l, 19)
            for s in range(NS):
                pbs = []
                for n, (wt, wb) in enumerate(((w0, w0b), (w1, w1b))):
                    pb_ap, pb_b = bank()
                    w3 = wt[:, :].rearrange("p (kc c) -> p kc c", c=512)
                    for kc in range(8):
                        PE.op(lambda kc=kc, w3=w3, pb_ap=pb_ap: nc.tensor.matmul(
                            pb_ap[:, :], lhsT=mergedT[:, kc, s * 128:(s + 1) * 128], rhs=w3[:, kc, :],
                            start=(kc == 0), stop=(kc == 7)), R=[wb, merged_b[kc]], W=[pb_b])
                    pbs.append((pb_ap, pb_b))
                postnorm(l, 0, s, pbs)

        def postnorm(l, which, s, pbs):
            st_t, st_b = small()
            ys = []
            for n, (pb_ap, pb_b) in enumerate(pbs):
                j_t, j_b = scratch()
                ACT.op(lambda n=n, pb_ap=pb_ap, j_t=j_t, st_t=st_t: nc.scalar.activation(
                    out=j_t[:, 0:512], in_=pb_ap[:, :], func=AF.Square, accum_out=st_t[:, n:n + 1]), R=[pb_b], W=[j_b, st_b])
                y_t, y_b = scratch()
                DVE.op(lambda n=n, pb_ap=pb_ap, y_t=y_t: nc.vector.tensor_tensor(
                    out=y_t[:, 0:512], in0=pb_ap[:, :], in1=Grow[:, l * 2 + which, n * 512:(n + 1) * 512], op=ALU.mult),
                    R=[pb_b, Grow_b[l * 2 + which]], W=[y_b])
                ys.append((y_t, y_b))
            DVE.op(lambda st_t=st_t: nc.vector.tensor_tensor(out=st_t[:, 2:3], in0=st_t[:, 0:1], in1=st_t[:, 1:2], op=ALU.add),
                   R=[st_b], W=[st_b])
            ACT.op(lambda st_t=st_t: nc.scalar.activation(out=st_t[:, 3:4], in_=st_t[:, 2:3], func=AF.Sqrt, scale=1.0 / D, bias=eps_ap),
                   R=[st_b, cst_b], W=[st_b])
            DVE.op(lambda st_t=st_t: nc.vector.reciprocal(out=st_t[:, 4:5], in_=st_t[:, 3:4]), R=[st_b], W=[st_b])
            for n, (pb_ap, pb_b) in enumerate(pbs):
                y_t, y_b = ys[n]
                DVE.op(lambda n=n, y_t=y_t, st_t=st_t: nc.vector.scalar_tensor_tensor(
                    out=xt[:, s, n * 512:(n + 1) * 512], in0=y_t[:, 0:512], scalar=st_t[:, 4:5], in1=xt[:, s, n * 512:(n + 1) * 512],
                    op0=ALU.mult, op1=ALU.add), R=[y_b, st_b, xt_b[s]], W=[xt_b[s]])

        def ffn(l, i):
            pv = l * PVL
            prenorm(l, i, 1)
            pending = []

            def emit_mult(ya_t, ya_b, yv_t, yv_b, j):
                POOL.op(lambda: nc.gpsimd.tensor_tensor(
                    out=gff[:, j, :], in0=ya_t[:, 0:T], in1=yv_t[:, 0:T], op=ALU.mult), R=[ya_b, yv_b], W=[gff_b[j]])

            for g in range(11):
                wt, wb = get_slot(l, 20 + g)
                for jj in range(2):
                    j = g * 2 + jj
                    ys = []
                    for part in range(2):
                        ch = part * 22 + j
                        pb_ap, pb_b = bank()
                        fgroup(wt, wb, part * 256 + jj * 128, 128, pb_ap, pb_b)
                        u_t, u_b = scratch()
                        uh_b = Buf()
                        POOL.op(lambda u_t=u_t, ch=ch: nc.gpsimd.tensor_copy(out=u_t[:, 0:2], in_=halo[:, l, ch, :]), R=[halo_b[l][ch]], W=[u_b])
                        ACT.op(lambda u_t=u_t, pb_ap=pb_ap: nc.scalar.copy(out=u_t[:, 2:2 + T], in_=pb_ap[:, 0:T]), R=[pb_b], W=[u_b])
                        POOL.op(lambda u_t=u_t, ch=ch: nc.gpsimd.tensor_copy(out=halo[:, l, ch, :], in_=u_t[:, T:T + 2]), R=[u_b], W=[halo_b[l][ch]])
                        wc = pvec[:, pv + 194 + ch * 3:pv + 194 + ch * 3 + 3]
                        y_t, y_b = scratch()
                        ACT.op(lambda pb_ap=pb_ap, y_t=y_t, wc=wc: nc.scalar.activation(
                            out=y_t[:, 0:T], in_=pb_ap[:, 0:T], func=AF.Identity, scale=wc[:, 2:3]),
                            R=[pb_b, cst_b], W=[y_b])
                        for q in (1, 0):
                            DVE.op(lambda u_t=u_t, y_t=y_t, wc=wc, q=q: nc.vector.scalar_tensor_tensor(
                                out=y_t[:, 0:T], in0=u_t[:, q:q + T], scalar=wc[:, q:q + 1], in1=y_t[:, 0:T],
                                op0=ALU.mult, op1=ALU.add), R=[u_b, cst_b, y_b], W=[y_b])
                        ys.append((y_t, y_b))
                    (ya_t, ya_b), (yv_t, yv_b) = ys
                    if pending:
                        emit_mult(*pending.pop())
                    ACT.op(lambda ya_t=ya_t: nc.scalar.activation(out=ya_t[:, 0:T], in_=ya_t[:, 0:T], func=AF.Silu), R=[ya_b], W=[ya_b])
                    pending.append((ya_t, ya_b, yv_t, yv_b, j))
            if pending:
                emit_mult(*pending.pop())
            accs = {}
            for s in range(NS):
                for n in range(2):
                    accs[(s, n)] = bank()
            for q in range(6):
                wt, wb = get_slot(l, 31 + q)
                w3 = wt[:, :].rearrange("p (m c) -> p m c", c=1024)
                nk = 4 if q < 5 else 2
                for s in range(NS):
                    for n in range(2):
                        pb_ap, pb_b = accs[(s, n)]
                        for m in range(nk):
                            j = q * 4 + m
                            PE.op(lambda m=m, j=j, w3=w3, pb_ap=pb_ap, s=s, n=n: nc.tensor.matmul(
                                pb_ap[:, :], lhsT=gff[:, j, s * 128:(s + 1) * 128], rhs=w3[:, m, n * 512:(n + 1) * 512],
                                start=(j == 0), stop=(j == 21)), R=[wb, gff_b[j]], W=[pb_b])
            for s in range(NS):
                postnorm(l, 1, s, [accs[(s, 0)], accs[(s, 1)]])

        for i in range(nseq):
            seq_init(i)
            for tl in range(ntiles):
                src = x_d[i, tl * T:(tl + 1) * T, :].rearrange("(s p) d -> p s d", p=128)
                POOL.dma(xt[:, :, :], src, s_xin, W=xt_b)
                for l in range(depth):
                    if DBG_LEVEL >= 1:
                        token_mixer(l, i)
                    if DBG_LEVEL >= 7:
                        ffn(l, i)
                dst = out_d[i, tl * T:(tl + 1) * T, :].rearrange("(s p) d -> p s d", p=128)
                POOL.dma(dst, xt[:, :, :], s_xout, R=xt_b)
        allsems = [PE.sem, ACT.sem, DVE.sem, POOL.sem, SP.sem, s_xin, s_xout, s_c] + slot_s + hold_s + list(prep_sems.values()) + list(dbg_sems.values()) + diag_sems
        POOL.wait_all(allsems)
        SP.wait_all([s_xout])
    return nc


def host_pack(inp, core):
    f = np.float32
    b0 = core * 2
    cT = np.ascontiguousarray(inp["c"][b0:b0 + 2].reshape(2, 8, 128).transpose(2, 1, 0).reshape(128, 16)).astype(f)
    pvec = np.zeros((128, DEPTH * PVL), f)
    brow = np.zeros((DEPTH * BRL,), f)
    w2 = np.zeros((16, DEPTH * 256), f)

    def fm(v, nch):
        return np.asarray(v, f).reshape(nch, 128).T

    for l in range(DEPTH):
        o = l * PVL
        pvec[:, o + 0:o + 8] = fm(inp["tm_pre_g"][l], 8)
        pvec[:, o + 8:o + 16] = fm(inp["tm_post_g"][l], 8)
        pvec[:, o + 16:o + 24] = fm(inp["cm_pre_g"][l], 8)
        pvec[:, o + 24:o + 32] = fm(inp["cm_post_g"][l], 8)
        pvec[:, o + 32:o + 80] = fm(inp["ada_b"][l], 48)
        cw = np.asarray(inp["conf_dw_w"][l], f)
        for c in range(2):
            pvec[:, o + 80 + c * 31:o + 80 + (c + 1) * 31] = cw[:, c * 128:(c + 1) * 128].T
        pvec[:, o + 142:o + 144] = fm(inp["conf_dw_b"][l], 2)
        pvec[:, o + 144:o + 146] = fm(inp["conf_ln_g"][l], 2)
        pvec[:, o + 146:o + 148] = fm(inp["conf_ln_b"][l], 2)
        sw = np.asarray(inp["sc_dw_w"][l], f)
        for c in range(2):
            pvec[:, o + 148 + c * 3:o + 148 + (c + 1) * 3] = sw[:, c * 128:(c + 1) * 128].T
        pvec[:, o + 154:o + 162] = fm(inp["conf_out_b"][l], 8)
        pvec[:, o + 162:o + 194] = fm(inp["merge_gate_b"][l], 32)
        fw = np.asarray(inp["ffn_dw_w"][l], f)
        for ch in range(44):
            pvec[:, o + 194 + ch * 3:o + 194 + (ch + 1) * 3] = fw[:, ch * 128:(ch + 1) * 128].T
        r = l * BRL
        brow[r + 0:r + 4] = inp["ml_i_bias"][l]
        brow[r + 4:r + 8] = inp["ml_f_bias"][l]
        brow[r + 8:r + 264] = inp["ml_norm_g"][l]
        brow[r + 264:r + 520] = inp["gla_norm_g"][l]
        brow[r + 520:r + 776] = inp["gla_a_bias"][l]
        w2[:, l * 256:(l + 1) * 256] = inp["gla_w_a2"][l]
    brow = np.ascontiguousarray(np.broadcast_to(brow[None, :], (128, DEPTH * BRL)))
    return cT, pvec, brow, w2


def make_consts():
    f = np.float32
    c = np.zeros((128, 1152), f)
    c[:, 0:128] = np.eye(128, dtype=f)
    c[:, 128:256] = 1.0
    s = np.arange(128)[:, None]
    t = np.arange(128)[None, :]
    U = (s <= t).astype(f)
    c[:, 256:384] = U
    c[:, 384:512] = U / 16.0
    c[:, 512:640] = (s > t).astype(f) / 16.0
    c[:, 640:1152] = np.tile(U, (1, 4))
    return c


BIG = ("ada_w", "w_in", "w_ml_out", "w_gla_out", "w_conf_out", "w_sc_out", "w_o", "ffn_w_up", "ffn_w_down")


def make_in_maps(inp, ncores, nseq, ntok):
    consts = make_consts()
    big = {k: np.ascontiguousarray(np.asarray(inp[k], np.float32)) for k in BIG}
    maps = []
    for core in range(ncores):
        cT, pvec, brow, w2 = host_pack(inp, core)
        m = {"x": np.ascontiguousarray(np.asarray(inp["x"][core * 2:core * 2 + nseq, :ntok], np.float32)),
             "cT": cT, "pvec": pvec, "brow": brow, "w2": w2, "consts": consts}
        m.update(big)
        maps.append(m)
    return maps


def kernel(**inputs):
    nc = build()
    maps = make_in_maps(inputs, NCORES, 2, SEQ)
    res = run_bass_kernel_spmd(nc, maps, core_ids=list(range(NCORES)))
    out = np.concatenate([np.asarray(r["out"]) for r in res.results], axis=0)
    return out.astype(np.float32)
```

```python
import contextlib
import numpy as np
import concourse.bass as bass
import concourse.mybir as mybir
from concourse.bass_utils import run_bass_kernel_spmd

F32 = mybir.dt.float32
BF16 = mybir.dt.bfloat16
AF = mybir.ActivationFunctionType
ALU = mybir.AluOpType
AX = mybir.AxisListType

D = 1024
MIX = 256
HID = 2816
INW = 7448
DEPTH = 2
SEQ = 4096
BATCH = 16
NCORES = 8
T = 256
NS = T // 128
NSLOT = 4
SLOTS_PER_LAYER = 39
EPS = 1e-6
PVL = 326
BRL = 776
GATE0 = 3352
DBG_LEVEL = 99


class Sem:
    def __init__(self, h):
        self.h = h
        self.count = 0


class Buf:
    __slots__ = ("w", "r", "excl")

    def __init__(self, excl=False):
        self.w = None
        self.r = {}
        self.excl = excl


class Eng:
    def __init__(self, e, sem, is_pe=False):
        self.e = e
        self.sem = sem
        self.seen = {}
        self.is_pe = is_pe
        self.hook = None
        self._inhook = False

    def _sync(self, reads, writes):
        need = {}
        for b in reads:
            if b.w is not None and need.get(b.w[0], 0) < b.w[1]:
                need[b.w[0]] = b.w[1]
        for b in writes:
            if b.w is not None and need.get(b.w[0], 0) < b.w[1]:
                need[b.w[0]] = b.w[1]
            for sm, v in b.r.items():
                if need.get(sm, 0) < v:
                    need[sm] = v
        for sm, v in need.items():
            if self.is_pe and sm is self.sem:
                continue
            if self.seen.get(sm, 0) < v:
                self.e.wait_ge(sm.h, v)
                self.seen[sm] = v

    @staticmethod
    def _mark(sm, reads, writes):
        v = sm.count
        for b in reads:
            if b.r.get(sm, 0) < v:
                b.r[sm] = v
        for b in writes:
            b.w = (sm, v)
            b.r = {}

    def op(self, fn, R=(), W=(), drain=False):
        if drain and self.sem.count > 0 and self.seen.get(self.sem, 0) < self.sem.count:
            self.e.wait_ge(self.sem.h, self.sem.count)
            self.seen[self.sem] = self.sem.count
        ex = [b for b in R if b.excl]
        if ex:
            W = list(W) + ex
        self._sync(R, W)
        inst = fn()
        self.sem.count += 1
        inst.then_inc(self.sem.h, 1)
        self._mark(self.sem, R, W)
        if self.hook is not None and not self._inhook:
            self._inhook = True
            self.hook()
            self._inhook = False

    def dma(self, out, in_, dsem, R=(), W=()):
        self._sync(R, W)
        inst = self.e.dma_start(out=out, in_=in_)
        dsem.count += 16
        inst.then_inc(dsem.h, 16)
        self._mark(dsem, R, W)

    def wait_all(self, sems):
        for sm in sems:
            if sm.count > 0 and self.seen.get(sm, 0) < sm.count:
                self.e.wait_ge(sm.h, sm.count)
                self.seen[sm] = sm.count


def build(nseq=2, ntiles=SEQ // T, depth=DEPTH, dbg=None):
    nc = bass.Bass("TRN2", target_bir_lowering=False)
    ntok = ntiles * T
    dt = nc.dram_tensor
    x_d = dt("x", [nseq, ntok, D], F32, kind="ExternalInput").ap()
    out_d = dt("out", [nseq, ntok, D], F32, kind="ExternalOutput").ap()
    cT_d = dt("cT", [128, 16], F32, kind="ExternalInput").ap()
    pvec_d = dt("pvec", [128, DEPTH * PVL], F32, kind="ExternalInput").ap()
    brow_d = dt("brow", [128, DEPTH * BRL], F32, kind="ExternalInput").ap()
    w2_d = dt("w2", [16, DEPTH * 256], F32, kind="ExternalInput").ap()
    const_d = dt("consts", [128, 1152], F32, kind="ExternalInput").ap()
    adaw_d = dt("ada_w", [DEPTH, D, 6 * D], F32, kind="ExternalInput").ap()
    win_d = dt("w_in", [DEPTH, D, INW], F32, kind="ExternalInput").ap()
    wml_d = dt("w_ml_out", [DEPTH, MIX, D], F32, kind="ExternalInput").ap()
    wgl_d = dt("w_gla_out", [DEPTH, MIX, D], F32, kind="ExternalInput").ap()
    wcf_d = dt("w_conf_out", [DEPTH, MIX, D], F32, kind="ExternalInput").ap()
    wsc_d = dt("w_sc_out", [DEPTH, MIX, D], F32, kind="ExternalInput").ap()
    wo_d = dt("w_o", [DEPTH, D, D], F32, kind="ExternalInput").ap()
    wup_d = dt("ffn_w_up", [DEPTH, D, 2 * HID], F32, kind="ExternalInput").ap()
    wdn_d = dt("ffn_w_down", [DEPTH, HID, D], F32, kind="ExternalInput").ap()
    scr_d = dt("wscratch", [DEPTH * SLOTS_PER_LAYER, 128, 4096], BF16, kind="Internal").ap()
    dbg_d = {}
    if dbg:
        for name, shape in dbg.items():
            dbg_d[name] = dt("dbg_" + name, list(shape), F32, kind="ExternalOutput").ap()

    es = contextlib.ExitStack()
    with es:
        def sb(name, shape, dtype=F32):
            return es.enter_context(nc.sbuf_tensor("sb_" + name, list(shape), dtype))

        def sem(name):
            return Sem(es.enter_context(nc.semaphore(name)))

        PE = Eng(nc.tensor, sem("s_pe"), is_pe=True)
        ACT = Eng(nc.scalar, sem("s_act"))
        DVE = Eng(nc.vector, sem("s_dve"))
        POOL = Eng(nc.gpsimd, sem("s_pool"))
        SP = Eng(nc.sync, sem("s_sp"))

        consts = sb("consts", [128, 1152])
        pvec = sb("pvec", [128, DEPTH * PVL])
        brow = sb("brow", [128, DEPTH * BRL])
        w2 = sb("w2", [16, DEPTH * 256])
        cact = sb("cact", [128, 16])
        cst_b = Buf()
        s_c = sem("s_const")
        for t_, d_ in ((consts, const_d), (pvec, pvec_d), (brow, brow_d), (w2, w2_d), (cact, cT_d)):
            SP.dma(t_[:, :], d_, s_c, W=[cst_b])
        ident = consts[:, 0:128]
        ones = consts[:, 128:256]
        Umat = consts[:, 256:384]
        U16 = consts[:, 384:512]
        SU16 = consts[:, 512:640]
        mask4 = consts[:, 640:1152].rearrange("p (h t) -> p h t", t=128)

        prep_sems = {}
        scr_b = {}

        def prep(l, s, out_ap, in_ap, grp):
            key = (l, grp)
            if key not in prep_sems:
                prep_sems[key] = sem("s_prep%d_%d" % key)
            sm = prep_sems[key]
            inst = nc.gpsimd.dma_start(out=out_ap, in_=in_ap)
            sm.count += 16
            inst.then_inc(sm.h, 16)
            scr_b.setdefault((l, s), Buf())

        slot_grp = {}

        def scr_view(l, s, pat, **kw):
            return scr_d[l * SLOTS_PER_LAYER + s].rearrange(pat, **kw)

        def k1024(src, c0, n):
            return src.rearrange("(kc p) c -> p kc c", p=128)[:, :, c0:c0 + n]

        for l in range(depth):
            wi = win_d[l]
            for s, (c0, n) in {0: (0, 512), 1: (512, 512), 3: (1032, 512), 4: (1544, 512),
                               5: (2072, 512), 6: (2584, 512), 7: (3096, 256)}.items():
                prep(l, s, scr_view(l, s, "p (kc c) -> p kc c", c=512)[:, :, 0:n], k1024(wi, c0, n), 0)
                slot_grp[(l, s)] = 0
            prep(l, 2, scr_view(l, 2, "p (kc c) -> p kc c", c=512)[:, :, 0:8], k1024(wi, 1024, 8), 0)
            prep(l, 2, scr_view(l, 2, "p (kc c) -> p kc c", c=512)[:, :, 8:24], k1024(wi, 2056, 16), 0)
            slot_grp[(l, 2)] = 0
            for j in range(8):
                for b in range(4):
                    prep(l, 8 + j, scr_view(l, 8 + j, "p (kc c) -> p kc c", c=512)[:, :, b * 128:(b + 1) * 128],
                         k1024(wi, GATE0 + b * 1024 + j * 128, 128), 1)
                slot_grp[(l, 8 + j)] = 1
            for s, (wa, wb) in {16: (wml_d, wgl_d), 17: (wcf_d, wsc_d)}.items():
                v = scr_view(l, s, "p (m c) -> p m c", c=1024)
                prep(l, s, v[:, 0:2, :], wa[l].rearrange("(kc p) c -> p kc c", p=128), 1)
                prep(l, s, v[:, 2:4, :], wb[l].rearrange("(kc p) c -> p kc c", p=128), 1)
                slot_grp[(l, s)] = 1
            for n in range(2):
                prep(l, 18 + n, scr_view(l, 18 + n, "p (kc c) -> p kc c", c=512), k1024(wo_d[l], n * 512, 512), 1)
                slot_grp[(l, 18 + n)] = 1
            for g in range(11):
                v = scr_view(l, 20 + g, "p (kc c) -> p kc c", c=512)
                prep(l, 20 + g, v[:, :, 0:256], k1024(wup_d[l], g * 256, 256), 2)
                prep(l, 20 + g, v[:, :, 256:512], k1024(wup_d[l], HID + g * 256, 256), 2)
                slot_grp[(l, 20 + g)] = 2
            for q in range(6):
                nk = 4 if q < 5 else 2
                v = scr_view(l, 31 + q, "p (m c) -> p m c", c=1024)
                prep(l, 31 + q, v[:, 0:nk, :], wdn_d[l].rearrange("(kc p) c -> p kc c", p=128)[:, 4 * q:4 * q + nk, :], 2)
                slot_grp[(l, 31 + q)] = 2
        for (l, s), b in scr_b.items():
            sm = prep_sems[(l, slot_grp[(l, s)])]
            b.w = (sm, sm.count)

        slots = [sb("wslot%d" % i, [128, 4096], BF16) for i in range(NSLOT)]
        slot_b = [Buf() for _ in range(NSLOT)]
        slot_s = [sem("s_slot%d" % i) for i in range(NSLOT)]
        slot_ctr = [0]

        def next_slot():
            i = slot_ctr[0] % NSLOT
            slot_ctr[0] += 1
            return i

        hold_t = [sb("whold%d" % i, [128, 4096], BF16) for i in range(2)]
        hold_b = [Buf(), Buf()]
        hold_s = [sem("s_hold%d" % i) for i in range(2)]

        def get_slot(l, s, hold=None):
            if hold is not None:
                SP.dma(hold_t[hold][:, :], scr_d[l * SLOTS_PER_LAYER + s], hold_s[hold], R=[scr_b[(l, s)]], W=[hold_b[hold]])
                return hold_t[hold], hold_b[hold]
            i = next_slot()
            dst = slots[i][:, :]
            src = scr_d[l * SLOTS_PER_LAYER + s]
            if s == 2:
                dst = dst.rearrange("p (kc c) -> p kc c", c=512)[:, :, 0:24]
                src = src.rearrange("p (kc c) -> p kc c", c=512)[:, :, 0:24]
            elif s == 7:
                dst = dst.rearrange("p (kc c) -> p kc c", c=512)[:, :, 0:256]
                src = src.rearrange("p (kc c) -> p kc c", c=512)[:, :, 0:256]
            elif s == 36:
                dst = dst[:, 0:2048]
                src = src[:, 0:2048]
            elif s >= 37:
                dst = dst[:, 0:3968]
                src = src[:, 0:3968]
            SP.dma(dst, src, slot_s[i], R=[scr_b[(l, s)]], W=[slot_b[i]])
            return slots[i], slot_b[i]

        ps_all = es.enter_context(nc.psum_tensor("ps", [128, 4096], F32))
        bank_b = [Buf(excl=True) for _ in range(8)]
        bank_ctr = [0]

        def bank():
            i = bank_ctr[0] % 8
            bank_ctr[0] += 1
            return ps_all[:, i * 512:(i + 1) * 512], bank_b[i]

        NSCR = 8
        scr_t = [sb("scr%d" % i, [128, 544]) for i in range(NSCR)]
        scr_bf = [Buf() for _ in range(NSCR)]
        scr_ctr = [0]

        def scratch():
            i = scr_ctr[0] % NSCR
            scr_ctr[0] += 1
            return scr_t[i], scr_bf[i]

        NSM = 16
        sm_t = [sb("sm%d" % i, [128, 16]) for i in range(NSM)]
        sm_bf = [Buf() for _ in range(NSM)]
        sm_ctr = [0]

        def small():
            i = sm_ctr[0] % NSM
            sm_ctr[0] += 1
            return sm_t[i], sm_bf[i]

        xt = sb("xt", [128, NS, D])
        xt_b = [Buf() for _ in range(NS)]
        s_xin = sem("s_xin")
        s_xout = sem("s_xout")
        xn = [sb("xn%d" % i, [128, D]) for i in range(2)]
        xn_b = [Buf(), Buf()]
        hT = sb("hT", [128, 8, T], BF16)
        hT_b = [[Buf(), Buf()] for _ in range(NS)]
        fq = {}
        for nm in ("mlq", "mlk", "gq", "gk"):
            fq[nm] = (sb(nm + "T", [128, 2, T], BF16), [Buf(), Buf()])
        tk = {}
        for nm in ("mlk", "mlv", "sigo", "glk", "gv", "sgr"):
            tk[nm] = (sb(nm + "_tok", [128, NS, 256], BF16), [Buf() for _ in range(NS)])
        iff = sb("iff", [128, NS, 8])
        iff_b = [Buf() for _ in range(NS)]
        gaT = sb("gaT", [16, T])
        gaT_b = Buf()
        zc = sb("zc", [128, 2, 30 + T], BF16)
        zc_b = [Buf(), Buf()]
        zs = sb("zs", [128, 2, 2 + T])
        zs_b = [Buf(), Buf()]
        bgs = sb("bgs", [128, 2, T], BF16)
        bgs_b = [Buf(), Buf()]
        cgs = sb("cgs", [128, 2, T])
        cgs_b = [Buf(), Buf()]
        uconv = sb("uconv", [128, 2, T])
        uconv_b = [Buf(), Buf()]
        zT = {}
        for nm in ("ml", "gla", "conf", "sc"):
            zT[nm] = (sb("z" + nm + "T", [128, 2, T], BF16), [Buf() for _ in range(max(NS, 2))])
        gates = [sb("gates%d" % i, [128, 4, T], BF16) for i in range(2)]
        gates_b = [Buf(), Buf()]
        mergedT = sb("mergedT", [128, 8, T], BF16)
        merged_b = [Buf() for _ in range(8)]
        gff = sb("gff", [128, 22, T], BF16)
        gff_b = [Buf() for _ in range(22)]
        halo = sb("halo", [128, DEPTH, 44, 2])
        halo_b = [[Buf() for _ in range(44)] for _ in range(DEPTH)]
        Cst = sb("Cst", [128, DEPTH, 2, 72])
        Cst_b = [[Buf(), Buf()] for _ in range(DEPTH)]
        Cbf = sb("Cbf", [128, DEPTH, 2, 72], BF16)
        Cbf_b = [[Buf(), Buf()] for _ in range(DEPTH)]
        Sst = sb("Sst", [128, DEPTH, 2, 64])
        Sst_b = [[Buf(), Buf()] for _ in range(DEPTH)]
        Sbf = sb("Sbf", [128, DEPTH, 2, 64], BF16)
        Sbf_b = [[Buf(), Buf()] for _ in range(DEPTH)]
        zch_b = [[Buf(), Buf()] for _ in range(DEPTH)]
        zch = sb("zch", [128, DEPTH, 2, 30], BF16)
        zsh = sb("zsh", [128, DEPTH, 2, 2])
        zsh_b = [[Buf(), Buf()] for _ in range(DEPTH)]
        Grow = sb("Grow", [128, DEPTH * 2, D])
        Grow_b = [Buf() for _ in range(DEPTH * 2)]
        modT = sb("modT", [128, DEPTH * 2 * 48])
        modT_b = Buf()
        modA = sb("modA", [128, DEPTH * 2 * 2 * 8])
        modA_b = Buf()
        mt = {}

        def mtile(name, shape, dtype=F32, n=None):
            if n is None:
                n = 2 if dtype == BF16 else 1
            if name not in mt:
                mt[name] = ([sb("%s_%d" % (name, i), shape, dtype) for i in range(n)], [Buf() for _ in range(n)], [0])
            tl, bl, c = mt[name]
            i = c[0] % n
            c[0] += 1
            return tl[i], bl[i]

        def dbg_dump(name, ap, b, idx=None):
            if dbg and name in dbg_d:
                dst = dbg_d[name] if idx is None else dbg_d[name][idx]
                sm_ = dbg_sems.setdefault(name, sem("s_dbg_" + name))
                POOL.dma(dst, ap, sm_, R=b)

        dbg_sems = {}
        dbg_state = []

        ACT.op(lambda: nc.scalar.activation(out=cact[:, :], in_=cact[:, :], func=AF.Silu), R=[cst_b], W=[cst_b])
        cact3 = cact[:, :].rearrange("p (kc i) -> p kc i", i=2)
        mod_ps, mod_pb = bank()
        for l in range(depth):
            for g in range(24):
                i = next_slot()
                sl32 = slots[i][:, :].bitcast(F32).rearrange("p (kc c) -> p kc c", c=256)
                SP.dma(sl32, adaw_d[l].rearrange("(kc p) c -> p kc c", p=128)[:, :, g * 256:(g + 1) * 256],
                       slot_s[i], W=[slot_b[i]])
                for fc in range(2):
                    col = (l * 48 + g * 2 + fc) * 2
                    for kc in range(8):
                        PE.op(lambda kc=kc, fc=fc, col=col, sl32=sl32: nc.tensor.matmul(
                            mod_ps[:, col:col + 2], lhsT=sl32[:, kc, fc * 128:(fc + 1) * 128], rhs=cact3[:, kc, :],
                            start=(kc == 0), stop=(kc == 7)), R=[slot_b[i], cst_b], W=[mod_pb])
        for l in range(depth):
            for i in range(2):
                o = (l * 2 + i) * 48
                src = mod_ps[:, l * 96:(l + 1) * 96].rearrange("p (v i) -> p v i", i=2)[:, :, i]
                DVE.op(lambda o=o, src=src, l=l: nc.vector.tensor_tensor(
                    out=modT[:, o:o + 48], in0=src, in1=pvec[:, l * PVL + 32:l * PVL + 80], op=ALU.add),
                    R=[mod_pb, cst_b], W=[modT_b])
        for l in range(depth):
            for i in range(2):
                o = (l * 2 + i) * 48
                for which in range(2):
                    a = ((l * 2 + i) * 2 + which) * 8
                    DVE.op(lambda o=o, a=a, which=which, l=l: nc.vector.scalar_tensor_tensor(
                        out=modA[:, a:a + 8], in0=modT[:, o + which * 24 + 8:o + which * 24 + 16], scalar=1.0,
                        in1=pvec[:, l * PVL + which * 16:l * PVL + which * 16 + 8], op0=ALU.add, op1=ALU.mult),
                        R=[modT_b, cst_b], W=[modA_b])

        diag_sems = []
        for l in range(depth):
            for c in range(2):
                s_diag = sem("s_diag%d_%d" % (l, c))
                diag_sems.append(s_diag)
                i_ = next_slot()
                for j in range(31):
                    DVE.op(lambda i_=i_, j=j, l=l, c=c: nc.vector.tensor_scalar(
                        out=slots[i_][:, j * 128:(j + 1) * 128], in0=ident,
                        scalar1=pvec[:, l * PVL + 80 + c * 31 + j:l * PVL + 80 + c * 31 + j + 1], scalar2=None, op0=ALU.mult),
                        R=[cst_b], W=[slot_b[i_]])
                scr_b[(l, 37 + c)] = Buf()
                SP.dma(scr_d[l * SLOTS_PER_LAYER + 37 + c][:, 0:3968], slots[i_][:, 0:3968], s_diag,
                       R=[slot_b[i_]], W=[scr_b[(l, 37 + c)]])
        def modA_ap(l, i, which):
            a = ((l * 2 + i) * 2 + which) * 8
            return modA[:, a:a + 8]

        def modB_ap(l, i, which):
            o = (l * 2 + i) * 48 + which * 24
            return modT[:, o:o + 8]

        def seq_init(i):
            for l in range(depth):
                for which in range(2):
                    o = (l * 2 + i) * 48 + which * 24 + 16
                    gv_t, gv_b = small()
                    DVE.op(lambda o=o, l=l, which=which, gv_t=gv_t: nc.vector.tensor_tensor(
                        out=gv_t[:, 0:8], in0=modT[:, o:o + 8],
                        in1=pvec[:, l * PVL + 8 + which * 16:l * PVL + 16 + which * 16], op=ALU.mult),
                        R=[modT_b, cst_b], W=[gv_b])
                    for half in range(2):
                        pb_ap, pb_b = bank()
                        for q in range(4):
                            kc = half * 4 + q
                            vb_t, vb_b = scratch()
                            DVE.op(lambda vb_t=vb_t, gv_t=gv_t, kc=kc: nc.vector.tensor_scalar(
                                out=vb_t[:, 0:128], in0=ones, scalar1=gv_t[:, kc:kc + 1], scalar2=None, op0=ALU.mult),
                                R=[cst_b, gv_b], W=[vb_b])
                            PE.op(lambda vb_t=vb_t, q=q, pb_ap=pb_ap: nc.tensor.matmul(
                                pb_ap[:, q * 128:(q + 1) * 128], lhsT=vb_t[:, 0:128], rhs=ident, start=True, stop=True),
                                R=[vb_b, cst_b], W=[pb_b])
                        ACT.op(lambda l=l, which=which, half=half, pb_ap=pb_ap: nc.scalar.copy(
                            out=Grow[:, l * 2 + which, half * 512:(half + 1) * 512], in_=pb_ap),
                            R=[pb_b], W=[Grow_b[l * 2 + which]])
            for l in range(depth):
                for p in range(2):
                    DVE.op(lambda l=l, p=p: nc.vector.memset(Cst[:, l, p, :], 0.0), W=[Cst_b[l][p]])
                    DVE.op(lambda l=l, p=p: nc.vector.memset(Cbf[:, l, p, :], 0.0), W=[Cbf_b[l][p]])
                    DVE.op(lambda l=l, p=p: nc.vector.memset(Sst[:, l, p, :], 0.0), W=[Sst_b[l][p]])
                    DVE.op(lambda l=l, p=p: nc.vector.memset(Sbf[:, l, p, :], 0.0), W=[Sbf_b[l][p]])
                    DVE.op(lambda l=l, p=p: nc.vector.memset(zch[:, l, p, :], 0.0), W=[zch_b[l][p]])
                    DVE.op(lambda l=l, p=p: nc.vector.memset(zsh[:, l, p, :], 0.0), W=[zsh_b[l][p]])
                for ch in range(44):
                    pass
                DVE.op(lambda l=l: nc.vector.memset(halo[:, l, :, :], 0.0), W=halo_b[l])

        def prenorm(l, i, which):
            A = modA_ap(l, i, which)
            Bv = modB_ap(l, i, which)
            for s in range(NS):
                xs = xt[:, s, :]
                xn_t, xn_bb = xn[s % 2], xn_b[s % 2]
                st_t, st_b = small()
                ACT.op(lambda xn_t=xn_t, xs=xs, st_t=st_t: nc.scalar.activation(
                    out=xn_t[:, :], in_=xs, func=AF.Square, accum_out=st_t[:, 0:1]), R=[xt_b[s]], W=[xn_bb, st_b])
                ACT.op(lambda st_t=st_t: nc.scalar.activation(
                    out=st_t[:, 1:2], in_=st_t[:, 0:1], func=AF.Sqrt, scale=1.0 / D, bias=eps_ap), R=[st_b, cst_b], W=[st_b])
                DVE.op(lambda st_t=st_t: nc.vector.reciprocal(out=st_t[:, 2:3], in_=st_t[:, 1:2]), R=[st_b], W=[st_b])
                DVE.op(lambda xn_t=xn_t, xs=xs, st_t=st_t: nc.vector.tensor_scalar(
                    out=xn_t[:, :], in0=xs, scalar1=st_t[:, 2:3], scalar2=None, op0=ALU.mult),
                    R=[xt_b[s], st_b], W=[xn_bb])
                for half in range(2):
                    pb_ap, pb_b = bank()
                    for q in range(4):
                        kc = half * 4 + q
                        PE.op(lambda xn_t=xn_t, kc=kc, q=q, pb_ap=pb_ap: nc.tensor.transpose(
                            pb_ap[:, q * 128:(q + 1) * 128], xn_t[:, kc * 128:(kc + 1) * 128], ident),
                            R=[xn_bb, cst_b], W=[pb_b])
                    for q in range(4):
                        kc = half * 4 + q
                        if half == 0:
                            ACT.op(lambda kc=kc, q=q, pb_ap=pb_ap, s=s: nc.scalar.activation(
                                out=hT[:, kc, s * 128:(s + 1) * 128], in_=pb_ap[:, q * 128:(q + 1) * 128],
                                func=AF.Identity, scale=A[:, kc:kc + 1], bias=Bv[:, kc:kc + 1]),
                                R=[pb_b, modA_b, modT_b], W=[hT_b[s][half]])
                        else:
                            DVE.op(lambda kc=kc, q=q, pb_ap=pb_ap, s=s: nc.vector.tensor_scalar(
                                out=hT[:, kc, s * 128:(s + 1) * 128], in0=pb_ap[:, q * 128:(q + 1) * 128],
                                scalar1=A[:, kc:kc + 1], scalar2=Bv[:, kc:kc + 1], op0=ALU.mult, op1=ALU.add),
                                R=[pb_b, modA_b, modT_b], W=[hT_b[s][half]])

        epsc = sb("epsc", [128, 2])
        eps_b = Buf()
        DVE.op(lambda: nc.vector.memset(epsc[:, 0:1], EPS), W=[cst_b])
        DVE.op(lambda: nc.vector.memset(epsc[:, 1:2], 1.0), W=[cst_b])
        eps_ap = epsc[:, 0:1]
        one_ap = epsc[:, 1:2]

        def fgroup(wt, wb, c0, m, pb_ap, pb_b, s_lo=0, s_hi=NS):
            w3 = wt[:, :].rearrange("p (kc c) -> p kc c", c=512)
            n0, n1 = s_lo * 128, s_hi * 128
            for kc in range(8):
                PE.op(lambda kc=kc: nc.tensor.matmul(pb_ap[0:m, n0:n1], lhsT=w3[:, kc, c0:c0 + m], rhs=hT[:, kc, n0:n1],
                                                     start=(kc == 0), stop=(kc == 7)),
                      R=[wb] + [b_ for s_ in range(s_lo, s_hi) for b_ in hT_b[s_]], W=[pb_b])

        def tgroup(wt, wb, c0, n, s, pb_ap, pb_b):
            w3 = wt[:, :].rearrange("p (kc c) -> p kc c", c=512)
            for kc in range(8):
                PE.op(lambda kc=kc: nc.tensor.matmul(pb_ap[:, 0:n], lhsT=hT[:, kc, s * 128:(s + 1) * 128],
                                                     rhs=w3[:, kc, c0:c0 + n], start=(kc == 0), stop=(kc == 7)),
                      R=[wb] + hT_b[s], W=[pb_b])

        def token_mixer(l, i):
            pv = l * PVL
            br = l * BRL
            prenorm(l, i, 0)
            first = dbg and not dbg_state
            if first:
                dbg_state.append(1)
                dbg_dump("hT", hT[:, :, :], [b_ for x_ in hT_b for b_ in x_])
            if DBG_LEVEL < 1.1:
                return
            wt, wb = get_slot(l, 0)
            for nm, cbase, scale in (("mlq", 0, 0.125), ("mlk", 256, 1.0)):
                for c in range(2):
                    pb_ap, pb_b = bank()
                    fgroup(wt, wb, cbase + c * 128, 128, pb_ap, pb_b)
                    ACT.op(lambda nm=nm, c=c, pb_ap=pb_ap, scale=scale: nc.scalar.activation(
                        out=fq[nm][0][:, c, :], in_=pb_ap[:, 0:T], func=AF.Copy, scale=scale), R=[pb_b], W=[fq[nm][1][c]])
            for s in range(NS):
                pb_ap, pb_b = bank()
                tgroup(wt, wb, 256, 256, s, pb_ap, pb_b)
                ACT.op(lambda s=s, pb_ap=pb_ap: nc.scalar.copy(out=tk["mlk"][0][:, s, :], in_=pb_ap[:, 0:256]),
                       R=[pb_b], W=[tk["mlk"][1][s]])
            if DBG_LEVEL < 1.2:
                return
            wt, wb = get_slot(l, 1)
            for s in range(NS):
                pb_ap, pb_b = bank()
                tgroup(wt, wb, 0, 512, s, pb_ap, pb_b)
                DVE.op(lambda s=s, pb_ap=pb_ap: nc.vector.tensor_copy(out=tk["mlv"][0][:, s, :], in_=pb_ap[:, 0:256]),
                       R=[pb_b], W=[tk["mlv"][1][s]])
                ACT.op(lambda s=s, pb_ap=pb_ap: nc.scalar.activation(
                    out=tk["sigo"][0][:, s, :], in_=pb_ap[:, 256:512], func=AF.Sigmoid), R=[pb_b], W=[tk["sigo"][1][s]])
            if DBG_LEVEL < 1.4:
                return
            wt, wb = get_slot(l, 2)
            for s in range(NS):
                pb_ap, pb_b = bank()
                tgroup(wt, wb, 0, 8, s, pb_ap, pb_b)
                DVE.op(lambda s=s, pb_ap=pb_ap: nc.vector.tensor_tensor(
                    out=iff[:, s, :], in0=pb_ap[:, 0:8], in1=brow[:, br:br + 8], op=ALU.add),
                    R=[pb_b, cst_b], W=[iff_b[s]])
            pb_ap, pb_b = bank()
            fgroup(wt, wb, 8, 16, pb_ap, pb_b)
            ACT.op(lambda pb_ap=pb_ap: nc.scalar.copy(out=gaT[:, :], in_=pb_ap[0:16, 0:T]), R=[pb_b], W=[gaT_b])
            if DBG_LEVEL < 1.6:
                return
            wt, wb = get_slot(l, 3)
            for nm, cbase, scale in (("gq", 0, 0.125), ("gk", 256, 1.0)):
                for c in range(2):
                    pb_ap, pb_b = bank()
                    fgroup(wt, wb, cbase + c * 128, 128, pb_ap, pb_b)
                    ACT.op(lambda nm=nm, c=c, pb_ap=pb_ap, scale=scale: nc.scalar.activation(
                        out=fq[nm][0][:, c, :], in_=pb_ap[:, 0:T], func=AF.Copy, scale=scale), R=[pb_b], W=[fq[nm][1][c]])
            for s in range(NS):
                pb_ap, pb_b = bank()
                tgroup(wt, wb, 256, 256, s, pb_ap, pb_b)
                ACT.op(lambda s=s, pb_ap=pb_ap: nc.scalar.copy(out=tk["glk"][0][:, s, :], in_=pb_ap[:, 0:256]),
                       R=[pb_b], W=[tk["glk"][1][s]])
            wt, wb = get_slot(l, 4)
            for s in range(NS):
                pb_ap, pb_b = bank()
                tgroup(wt, wb, 0, 512, s, pb_ap, pb_b)
                DVE.op(lambda s=s, pb_ap=pb_ap: nc.vector.tensor_copy(out=tk["gv"][0][:, s, :], in_=pb_ap[:, 0:256]),
                       R=[pb_b], W=[tk["gv"][1][s]])
                ACT.op(lambda s=s, pb_ap=pb_ap: nc.scalar.activation(
                    out=tk["sgr"][0][:, s, :], in_=pb_ap[:, 256:512], func=AF.Silu), R=[pb_b], W=[tk["sgr"][1][s]])
            if DBG_LEVEL < 1.8:
                return
            wt, wb = get_slot(l, 5)
            for c in range(2):
                pg_ap, pg_b = bank()
                fgroup(wt, wb, 256 + c * 128, 128, pg_ap, pg_b)
                pa_ap, pa_b = bank()
                fgroup(wt, wb, c * 128, 128, pa_ap, pa_b)
                sg_t, sg_b = scratch()
                ACT.op(lambda pg_ap=pg_ap, sg_t=sg_t: nc.scalar.activation(out=sg_t[:, 0:T], in_=pg_ap[:, 0:T], func=AF.Sigmoid),
                       R=[pg_b], W=[sg_b])
                ACT.op(lambda c=c: nc.scalar.copy(out=zc[:, c, 0:30], in_=zch[:, l, c, :]), R=[zch_b[l][c]], W=[zc_b[c]])
                DVE.op(lambda c=c, pa_ap=pa_ap, sg_t=sg_t: nc.vector.tensor_tensor(
                    out=zc[:, c, 30:30 + T], in0=pa_ap[:, 0:T], in1=sg_t[:, 0:T], op=ALU.mult),
                    R=[pa_b, sg_b], W=[zc_b[c]])
                ACT.op(lambda c=c: nc.scalar.copy(out=zch[:, l, c, :], in_=zc[:, c, T:T + 30]), R=[zc_b[c]], W=[zch_b[l][c]])
            wt, wb = get_slot(l, 6)
            for c in range(2):
                pb_ap, pb_b = bank()
                fgroup(wt, wb, c * 128, 128, pb_ap, pb_b)
                ACT.op(lambda c=c, pb_ap=pb_ap: nc.scalar.copy(out=bgs[:, c, :], in_=pb_ap[:, 0:T]), R=[pb_b], W=[bgs_b[c]])
                pb_ap, pb_b = bank()
                fgroup(wt, wb, 256 + c * 128, 128, pb_ap, pb_b)
                ACT.op(lambda c=c, pb_ap=pb_ap: nc.scalar.copy(out=cgs[:, c, :], in_=pb_ap[:, 0:T]), R=[pb_b], W=[cgs_b[c]])
            wt, wb = get_slot(l, 7)
            for c in range(2):
                pb_ap, pb_b = bank()
                fgroup(wt, wb, c * 128, 128, pb_ap, pb_b)
                ACT.op(lambda c=c: nc.scalar.copy(out=zs[:, c, 0:2], in_=zsh[:, l, c, :]), R=[zsh_b[l][c]], W=[zs_b[c]])
                DVE.op(lambda c=c, pb_ap=pb_ap: nc.vector.tensor_tensor(
                    out=zs[:, c, 2:2 + T], in0=pb_ap[:, 0:T], in1=cgs[:, c, :], op=ALU.mult),
                    R=[pb_b, cgs_b[c]], W=[zs_b[c]])
                ACT.op(lambda c=c: nc.scalar.copy(out=zsh[:, l, c, :], in_=zs[:, c, T:T + 2]), R=[zs_b[c]], W=[zsh_b[l][c]])

            if DBG_LEVEL < 3:
                return
            for c in range(2):
                y_t, y_b = scratch()
                w0 = pvec[:, pv + 148 + c * 3:pv + 148 + c * 3 + 3]
                DVE.op(lambda c=c, y_t=y_t, w0=w0: nc.vector.tensor_scalar(
                    out=y_t[:, 0:T], in0=zs[:, c, 2:2 + T], scalar1=w0[:, 2:3], scalar2=None, op0=ALU.mult),
                    R=[zs_b[c], cst_b], W=[y_b])
                for j in (1, 0):
                    DVE.op(lambda c=c, y_t=y_t, w0=w0, j=j: nc.vector.scalar_tensor_tensor(
                        out=y_t[:, 0:T], in0=zs[:, c, j:j + T], scalar=w0[:, j:j + 1], in1=y_t[:, 0:T],
                        op0=ALU.mult, op1=ALU.add), R=[zs_b[c], cst_b, y_b], W=[y_b])
                DVE.op(lambda c=c, y_t=y_t: nc.vector.tensor_tensor(
                    out=zT["sc"][0][:, c, :], in0=y_t[:, 0:T], in1=bgs[:, c, :], op=ALU.mult),
                    R=[y_b, bgs_b[c]], W=[zT["sc"][1][c]])

            for c in range(2):
                wt, wb = get_slot(l, 37 + c)
                pb_ap, pb_b = bank()
                for j in range(31):
                    PE.op(lambda j=j, c=c, wt=wt, pb_ap=pb_ap: nc.tensor.matmul(
                        pb_ap[:, 0:T], lhsT=wt[:, j * 128:(j + 1) * 128], rhs=zc[:, c, j:j + T],
                        start=(j == 0), stop=(j == 30)), R=[wb, zc_b[c]], W=[pb_b])
                ACT.op(lambda c=c, pb_ap=pb_ap: nc.scalar.activation(
                    out=uconv[:, c, :], in_=pb_ap[:, 0:T], func=AF.Identity, bias=pvec[:, pv + 142 + c:pv + 143 + c]),
                    R=[pb_b, cst_b], W=[uconv_b[c]])
            usq_t, usq_b = mtile("usq", [128, 2, T])
            ACT.op(lambda: nc.scalar.activation(out=usq_t[:, :, :], in_=uconv[:, :, :], func=AF.Square),
                   R=uconv_b, W=[usq_b])
            psum_ap, psum_b = bank()
            psq_ap, psq_b = bank()
            for c in range(2):
                PE.op(lambda c=c: nc.tensor.matmul(psum_ap[:, 0:T], lhsT=ones, rhs=uconv[:, c, :], start=(c == 0), stop=(c == 1)),
                      R=[cst_b, uconv_b[c]], W=[psum_b])
            for c in range(2):
                PE.op(lambda c=c: nc.tensor.matmul(psq_ap[:, 0:T], lhsT=ones, rhs=usq_t[:, c, :], start=(c == 0), stop=(c == 1)),
                      R=[cst_b, usq_b], W=[psq_b])
            mean_t, mean_b = scratch()
            msq_t, msq_b = scratch()
            rs_t, rs_b = scratch()
            ACT.op(lambda: nc.scalar.activation(out=mean_t[:, 0:T], in_=psum_ap[:, 0:T], func=AF.Copy, scale=1.0 / MIX),
                   R=[psum_b], W=[mean_b])
            ACT.op(lambda: nc.scalar.activation(out=msq_t[:, 0:T], in_=psum_ap[:, 0:T], func=AF.Square, scale=1.0 / MIX),
                   R=[psum_b], W=[msq_b])
            DVE.op(lambda: nc.vector.scalar_tensor_tensor(
                out=rs_t[:, 0:T], in0=psq_ap[:, 0:T], scalar=1.0 / MIX, in1=msq_t[:, 0:T], op0=ALU.mult, op1=ALU.subtract),
                R=[psq_b, msq_b], W=[rs_b])
            ACT.op(lambda: nc.scalar.activation(out=rs_t[:, 0:T], in_=rs_t[:, 0:T], func=AF.Ln, bias=eps_ap),
                   R=[rs_b, cst_b], W=[rs_b])
            ACT.op(lambda: nc.scalar.activation(out=rs_t[:, 0:T], in_=rs_t[:, 0:T], func=AF.Exp, scale=-0.5),
                   R=[rs_b], W=[rs_b])
            for c in range(2):
                d_t, d_b = scratch()
                DVE.op(lambda c=c, d_t=d_t: nc.vector.tensor_tensor(
                    out=d_t[:, 0:T], in0=uconv[:, c, :], in1=mean_t[:, 0:T], op=ALU.subtract),
                    R=[uconv_b[c], mean_b], W=[d_b])
                DVE.op(lambda c=c, d_t=d_t: nc.vector.tensor_tensor(
                    out=d_t[:, 0:T], in0=d_t[:, 0:T], in1=rs_t[:, 0:T], op=ALU.mult), R=[d_b, rs_b], W=[d_b])
                ACT.op(lambda c=c, d_t=d_t: nc.scalar.activation(
                    out=zT["conf"][0][:, c, :], in_=d_t[:, 0:T], func=AF.Silu,
                    scale=pvec[:, pv + 144 + c:pv + 145 + c], bias=pvec[:, pv + 146 + c:pv + 147 + c]),
                    R=[d_b, cst_b], W=[zT["conf"][1][c]])

            if DBG_LEVEL >= 4:
                mixers(l, i)
            if first:
                for nm in ("ml", "gla", "conf", "sc"):
                    dbg_dump("z_" + nm, zT[nm][0][:, :, :], zT[nm][1])
            if DBG_LEVEL < 5:
                return
            merge_and_out(l, i, pv, first)

        def mixers(l, i):
            pv = l * PVL
            br = l * BRL
            def mix_gen(s):
                tok = slice(s * 128, (s + 1) * 128)
                e_t, e_b = small()
                ACT.op(lambda e_t=e_t: nc.scalar.activation(out=e_t[:, 0:4], in_=iff[:, s, 4:8], func=AF.Exp, scale=-1.0),
                       R=[iff_b[s]], W=[e_b])
                ACT.op(lambda e_t=e_t: nc.scalar.activation(out=e_t[:, 4:8], in_=e_t[:, 0:4], func=AF.Ln, bias=one_ap),
                       R=[e_b, cst_b], W=[e_b])
                cs_ap, cs_b = bank()
                PE.op(lambda e_t=e_t: nc.tensor.matmul(cs_ap[:, 0:4], lhsT=Umat, rhs=e_t[:, 4:8], start=True, stop=True),
                      R=[cst_b, e_b], W=[cs_b])
                PE.op(lambda e_t=e_t: nc.tensor.matmul(cs_ap[:, 8:12], lhsT=ones, rhs=e_t[:, 4:8], start=True, stop=True),
                      R=[cst_b, e_b], W=[cs_b])
                la_ap, la_b = bank()
                PE.op(lambda: nc.tensor.matmul(la_ap[:, 0:256], lhsT=gaT[:, tok], rhs=w2[:, l * 256:(l + 1) * 256],
                                               start=True, stop=True), R=[gaT_b, cst_b], W=[la_b])
                w_t, w_b = small()
                DVE.op(lambda w_t=w_t: nc.vector.tensor_tensor(out=w_t[:, 0:4], in0=cs_ap[:, 0:4], in1=iff[:, s, 0:4], op=ALU.add),
                       R=[cs_b, iff_b[s]], W=[w_b])
                ACT.op(lambda w_t=w_t: nc.scalar.activation(out=w_t[:, 4:8], in_=w_t[:, 0:4], func=AF.Exp), R=[w_b], W=[w_b])
                ACT.op(lambda w_t=w_t: nc.scalar.activation(out=w_t[:, 8:12], in_=cs_ap[:, 0:4], func=AF.Exp, scale=-1.0),
                       R=[cs_b], W=[w_b])
                ebl_t, ebl_b = small()
                for hh in range(2):
                    ACT.op(lambda hh=hh, ebl_t=ebl_t: nc.scalar.activation(
                        out=ebl_t[hh * 64:(hh + 1) * 64, 0:2],
                        in_=cs_ap[hh * 64:(hh + 1) * 64, 8:12].rearrange("p (a b) -> p a b", b=2)[:, :, hh],
                        func=AF.Exp, scale=-1.0), R=[cs_b], W=[ebl_b])
                xla_t, xla_b = mtile("xla", [128, 256])
                DVE.op(lambda xla_t=xla_t: nc.vector.tensor_tensor(
                    out=xla_t[:, :], in0=la_ap[:, 0:256], in1=brow[:, br + 520:br + 776], op=ALU.add),
                    R=[la_b, cst_b], W=[xla_b])
                ACT.op(lambda xla_t=xla_t: nc.scalar.activation(out=xla_t[:, :], in_=xla_t[:, :], func=AF.Exp, scale=-1.0),
                       R=[xla_b], W=[xla_b])
                ACT.op(lambda xla_t=xla_t: nc.scalar.activation(out=xla_t[:, :], in_=xla_t[:, :], func=AF.Ln, bias=one_ap),
                       R=[xla_b, cst_b], W=[xla_b])
                vx_t, vx_b = mtile("vext", [128, 4, 72], BF16)
                DVE.op(lambda vx_t=vx_t, w_t=w_t: nc.vector.tensor_tensor(
                    out=vx_t[:, :, 0:64], in0=tk["mlv"][0][:, s, :].rearrange("p (h e) -> p h e", e=64),
                    in1=w_t[:, 4:8].unsqueeze(2).to_broadcast([128, 4, 64]), op=ALU.mult),
                    R=[tk["mlv"][1][s], w_b], W=[vx_b])
                DVE.op(lambda vx_t=vx_t, w_t=w_t: nc.vector.tensor_copy(out=vx_t[:, :, 64:65], in_=w_t[:, 4:8].unsqueeze(2)),
                       R=[w_b], W=[vx_b])
                nb_ap, nb_b = bank()
                for p in range(2):
                    PE.op(lambda p=p, xla_t=xla_t: nc.tensor.matmul(
                        nb_ap[:, p * 128:(p + 1) * 128], lhsT=xla_t[:, p * 128:(p + 1) * 128], rhs=U16, start=True, stop=True),
                        R=[xla_b, cst_b], W=[nb_b])
                PE.op(lambda xla_t=xla_t: nc.tensor.matmul(nb_ap[:, 256:512], lhsT=SU16, rhs=xla_t[:, :], start=True, stop=True),
                      R=[xla_b, cst_b], W=[nb_b])
                yield 'a'
                st_ap, st_b = bank()
                for h in range(4):
                    p, hh = h // 2, h % 2
                    rows = slice(hh * 64, (hh + 1) * 64)
                    PE.op(lambda h=h, p=p, rows=rows: nc.tensor.matmul(
                        st_ap[:, h * 128:(h + 1) * 128], lhsT=fq["mlk"][0][rows, p, tok], rhs=fq["mlq"][0][rows, p, tok],
                        start=True, stop=True), R=[fq["mlk"][1][p], fq["mlq"][1][p]], W=[st_b], drain=True)
                pt_t, pt_b = mtile("PT", [128, 4, 128], BF16)
                DVE.op(lambda pt_t=pt_t: nc.vector.tensor_tensor(
                    out=pt_t[:, :, :], in0=st_ap.rearrange("p (h t) -> p h t", t=128), in1=mask4, op=ALU.mult),
                    R=[st_b, cst_b], W=[pt_b])
                yield 'a'
                eq_t, eq_b = mtile("eq", [128, 2, 128])
                ek_t, ek_b = mtile("ek", [128, 2, 128])
                nb3 = nb_ap[:, 0:256].rearrange("p (a t) -> p a t", t=128)
                ACT.op(lambda eq_t=eq_t: nc.scalar.activation(out=eq_t[:, :, :], in_=nb3, func=AF.Exp, scale=-1.0),
                       R=[nb_b], W=[eq_b])
                ACT.op(lambda ek_t=ek_t: nc.scalar.activation(out=ek_t[:, :, :], in_=nb3, func=AF.Exp), R=[nb_b], W=[ek_b])
                ebg_t, ebg_b = small()
                ACT.op(lambda ebg_t=ebg_t: nc.scalar.activation(out=ebg_t[:, 0:2], in_=nb3[:, :, 127], func=AF.Exp, scale=-1.0),
                       R=[nb_b], W=[ebg_b])
                er_t, er_b = mtile("erem", [128, 256])
                ACT.op(lambda er_t=er_t: nc.scalar.activation(out=er_t[:, :], in_=nb_ap[:, 256:512], func=AF.Exp, scale=-1.0),
                       R=[nb_b], W=[er_b])
                qd_t, qd_b = mtile("qd", [128, 2, 128], BF16)
                ki_t, ki_b = mtile("ki", [128, 2, 128], BF16)
                kd_t, kd_b = mtile("kd", [128, 256], BF16)
                DVE.op(lambda qd_t=qd_t, eq_t=eq_t: nc.vector.tensor_tensor(
                    out=qd_t[:, :, :], in0=fq["gq"][0][:, :, tok], in1=eq_t[:, :, :], op=ALU.mult),
                    R=fq["gq"][1] + [eq_b], W=[qd_b])
                DVE.op(lambda ki_t=ki_t, ek_t=ek_t: nc.vector.tensor_tensor(
                    out=ki_t[:, :, :], in0=fq["gk"][0][:, :, tok], in1=ek_t[:, :, :], op=ALU.mult),
                    R=fq["gk"][1] + [ek_b], W=[ki_b])
                DVE.op(lambda kd_t=kd_t, er_t=er_t: nc.vector.tensor_tensor(
                    out=kd_t[:, :], in0=tk["glk"][0][:, s, :], in1=er_t[:, :], op=ALU.mult),
                    R=[tk["glk"][1][s], er_b], W=[kd_b])
                yield 'endA'
                mo_ap, mo_b = bank()
                mo3 = mo_ap[:, 0:288].rearrange("p (h e) -> p h e", e=72)
                for h in range(4):
                    p, hh = h // 2, h % 2
                    rows = slice(hh * 64, (hh + 1) * 64)
                    PE.op(lambda h=h, pt_t=pt_t, vx_t=vx_t: nc.tensor.matmul(
                        mo3[:, h, 0:65], lhsT=pt_t[:, h, :], rhs=vx_t[:, h, 0:65], start=True, stop=False),
                        R=[pt_b, vx_b], W=[mo_b], drain=True)
                    PE.op(lambda h=h, p=p, rows=rows: nc.tensor.matmul(
                        mo3[:, h, 0:65], lhsT=fq["mlq"][0][rows, p, tok], rhs=Cbf[rows, l, p, 0:65], start=False, stop=True),
                        R=[fq["mlq"][1][p], Cbf_b[l][p]], W=[mo_b], drain=True)
                at_ap, at_b = bank()
                for h in range(4):
                    p, hh = h // 2, h % 2
                    rows = slice(hh * 64, (hh + 1) * 64)
                    PE.op(lambda h=h, p=p, rows=rows, ki_t=ki_t, qd_t=qd_t: nc.tensor.matmul(
                        at_ap[:, h * 128:(h + 1) * 128], lhsT=ki_t[rows, p, :], rhs=qd_t[rows, p, :], start=True, stop=True),
                        R=[ki_b, qd_b], W=[at_b], drain=True)
                at_t, at_tb = mtile("AT", [128, 4, 128], BF16)
                DVE.op(lambda at_t=at_t: nc.vector.tensor_tensor(
                    out=at_t[:, :, :], in0=at_ap.rearrange("p (h t) -> p h t", t=128), in1=mask4, op=ALU.mult),
                    R=[at_b, cst_b], W=[at_tb])
                cu_ap, cu_b = bank()
                cu3 = cu_ap[:, 0:144].rearrange("p (a e) -> p a e", e=72)
                for h in range(4):
                    p, hh = h // 2, h % 2
                    rows = slice(hh * 64, (hh + 1) * 64)
                    PE.op(lambda h=h, p=p, rows=rows, vx_t=vx_t: nc.tensor.matmul(
                        cu3[rows, p, 0:65], lhsT=tk["mlk"][0][:, s, h * 64:(h + 1) * 64], rhs=vx_t[:, h, 0:65],
                        start=True, stop=True), R=[tk["mlk"][1][s], vx_b], W=[cu_b], drain=True)
                yield 'endB'
                r_t, r_b = small()
                DVE.op(lambda r_t=r_t, w_t=w_t: nc.vector.tensor_tensor(
                    out=r_t[:, 0:4], in0=mo3[:, :, 64], in1=w_t[:, 8:12], op=ALU.mult), R=[mo_b, w_b], W=[r_b])
                DVE.op(lambda r_t=r_t: nc.vector.tensor_scalar(
                    out=r_t[:, 12:16], in0=r_t[:, 0:4], scalar1=-1.0, scalar2=1.0, op0=ALU.mult, op1=ALU.max), R=[r_b], W=[r_b])
                DVE.op(lambda r_t=r_t: nc.vector.tensor_tensor(
                    out=r_t[:, 0:4], in0=r_t[:, 0:4], in1=r_t[:, 12:16], op=ALU.max), R=[r_b], W=[r_b])
                DVE.op(lambda r_t=r_t: nc.vector.reciprocal(out=r_t[:, 4:8], in_=r_t[:, 0:4]), R=[r_b], W=[r_b])
                DVE.op(lambda r_t=r_t, w_t=w_t: nc.vector.tensor_tensor(
                    out=r_t[:, 8:12], in0=r_t[:, 4:8], in1=w_t[:, 8:12], op=ALU.mult), R=[r_b, w_b], W=[r_b])
                hm_t, hm_b = mtile("hm", [128, 4, 64])
                DVE.op(lambda hm_t=hm_t, r_t=r_t: nc.vector.tensor_tensor(
                    out=hm_t[:, :, :], in0=mo3[:, :, 0:64], in1=r_t[:, 8:12].unsqueeze(2).to_broadcast([128, 4, 64]), op=ALU.mult),
                    R=[mo_b, r_b], W=[hm_b])
                hsq_t, hsq_b = mtile("hsq", [128, 4, 64])
                ACT.op(lambda hm_t=hm_t, hsq_t=hsq_t: nc.scalar.activation(out=hsq_t[:, :, :], in_=hm_t[:, :, :], func=AF.Square),
                       R=[hm_b], W=[hsq_b])
                n_t, n_b = small()
                DVE.op(lambda n_t=n_t, hsq_t=hsq_t: nc.vector.tensor_reduce(out=n_t[:, 0:4], in_=hsq_t[:, :, :], axis=AX.X, op=ALU.add),
                       R=[hsq_b], W=[n_b])
                ACT.op(lambda n_t=n_t: nc.scalar.activation(out=n_t[:, 4:8], in_=n_t[:, 0:4], func=AF.Sqrt, scale=1.0 / 64, bias=eps_ap),
                       R=[n_b, cst_b], W=[n_b])
                DVE.op(lambda n_t=n_t: nc.vector.reciprocal(out=n_t[:, 8:12], in_=n_t[:, 4:8]), R=[n_b], W=[n_b])
                og_t, og_b = mtile("og", [128, 256])
                DVE.op(lambda og_t=og_t: nc.vector.tensor_tensor(
                    out=og_t[:, :], in0=tk["sigo"][0][:, s, :], in1=brow[:, br + 8:br + 264], op=ALU.mult),
                    R=[tk["sigo"][1][s], cst_b], W=[og_b])
                DVE.op(lambda hm_t=hm_t, n_t=n_t: nc.vector.tensor_tensor(
                    out=hm_t[:, :, :], in0=hm_t[:, :, :], in1=n_t[:, 8:12].unsqueeze(2).to_broadcast([128, 4, 64]), op=ALU.mult),
                    R=[hm_b, n_b], W=[hm_b])
                zt_t, zt_b = mtile("ztok", [128, 256])
                DVE.op(lambda hm_t=hm_t, og_t=og_t, zt_t=zt_t: nc.vector.tensor_tensor(
                    out=zt_t[:, :], in0=hm_t[:, :, :].rearrange("p h e -> p (h e)"), in1=og_t[:, :], op=ALU.mult),
                    R=[hm_b, og_b], W=[zt_b])
                tp_ap, tp_b = bank()
                for c in range(2):
                    PE.op(lambda c=c, zt_t=zt_t: nc.tensor.transpose(tp_ap[:, c * 128:(c + 1) * 128], zt_t[:, c * 128:(c + 1) * 128], ident),
                          R=[zt_b, cst_b], W=[tp_b])
                ACT.op(lambda: nc.scalar.copy(out=zT["ml"][0][:, :, tok], in_=tp_ap[:, 0:256].rearrange("p (c t) -> p c t", t=128)),
                       R=[tp_b], W=[zT["ml"][1][s]])
                for p in range(2):
                    ct_t, ct_b = mtile("ctmp", [128, 72])
                    DVE.op(lambda p=p, ct_t=ct_t: nc.vector.tensor_tensor(
                        out=ct_t[:, 0:65], in0=cu3[:, p, 0:65], in1=Cst[:, l, p, 0:65], op=ALU.add),
                        R=[cu_b, Cst_b[l][p]], W=[ct_b])
                    DVE.op(lambda p=p, ct_t=ct_t, ebl_t=ebl_t: nc.vector.tensor_scalar(
                        out=Cst[:, l, p, 0:65], in0=ct_t[:, 0:65], scalar1=ebl_t[:, p:p + 1], scalar2=None, op0=ALU.mult),
                        R=[ct_b, ebl_b], W=[Cst_b[l][p]])
                    ACT.op(lambda p=p, ct_t=ct_t, ebl_t=ebl_t: nc.scalar.activation(
                        out=Cbf[:, l, p, 0:65], in_=ct_t[:, 0:65], func=AF.Copy, scale=ebl_t[:, p:p + 1]),
                        R=[ct_b, ebl_b], W=[Cbf_b[l][p]])
                yield 'endC'
                go_ap, go_b = bank()
                for h in range(4):
                    p, hh = h // 2, h % 2
                    rows = slice(hh * 64, (hh + 1) * 64)
                    PE.op(lambda h=h, at_t=at_t: nc.tensor.matmul(
                        go_ap[:, h * 64:(h + 1) * 64], lhsT=at_t[:, h, :], rhs=tk["gv"][0][:, s, h * 64:(h + 1) * 64],
                        start=True, stop=False), R=[at_tb, tk["gv"][1][s]], W=[go_b], drain=True)
                    PE.op(lambda h=h, p=p, rows=rows, qd_t=qd_t: nc.tensor.matmul(
                        go_ap[:, h * 64:(h + 1) * 64], lhsT=qd_t[rows, p, :], rhs=Sbf[rows, l, p, :], start=False, stop=True),
                        R=[qd_b, Sbf_b[l][p]], W=[go_b], drain=True)
                su_ap, su_b = bank()
                su3 = su_ap[:, 0:128].rearrange("p (a e) -> p a e", e=64)
                for h in range(4):
                    p, hh = h // 2, h % 2
                    rows = slice(hh * 64, (hh + 1) * 64)
                    PE.op(lambda h=h, p=p, rows=rows, kd_t=kd_t: nc.tensor.matmul(
                        su3[rows, p, :], lhsT=kd_t[:, h * 64:(h + 1) * 64], rhs=tk["gv"][0][:, s, h * 64:(h + 1) * 64],
                        start=True, stop=True), R=[kd_b, tk["gv"][1][s]], W=[su_b], drain=True)
                gm_t, gm_b = mtile("gm", [128, 4, 64])
                ACT.op(lambda gm_t=gm_t: nc.scalar.copy(out=gm_t[:, :, :], in_=go_ap[:, 0:256].rearrange("p (h e) -> p h e", e=64)),
                       R=[go_b], W=[gm_b])
                gsq_t, gsq_b = mtile("gsq", [128, 4, 64])
                ACT.op(lambda gsq_t=gsq_t: nc.scalar.activation(
                    out=gsq_t[:, :, :], in_=go_ap[:, 0:256].rearrange("p (h e) -> p h e", e=64), func=AF.Square),
                    R=[go_b], W=[gsq_b])
                gn_t, gn_b = small()
                DVE.op(lambda gn_t=gn_t, gsq_t=gsq_t: nc.vector.tensor_reduce(out=gn_t[:, 0:4], in_=gsq_t[:, :, :], axis=AX.X, op=ALU.add),
                       R=[gsq_b], W=[gn_b])
                ACT.op(lambda gn_t=gn_t: nc.scalar.activation(out=gn_t[:, 4:8], in_=gn_t[:, 0:4], func=AF.Sqrt, scale=1.0 / 64, bias=eps_ap),
                       R=[gn_b, cst_b], W=[gn_b])
                DVE.op(lambda gn_t=gn_t: nc.vector.reciprocal(out=gn_t[:, 8:12], in_=gn_t[:, 4:8]), R=[gn_b], W=[gn_b])
                rg_t, rg_b = mtile("rg", [128, 256])
                DVE.op(lambda rg_t=rg_t: nc.vector.tensor_tensor(
                    out=rg_t[:, :], in0=tk["sgr"][0][:, s, :], in1=brow[:, br + 264:br + 520], op=ALU.mult),
                    R=[tk["sgr"][1][s], cst_b], W=[rg_b])
                DVE.op(lambda gm_t=gm_t, gn_t=gn_t: nc.vector.tensor_tensor(
                    out=gm_t[:, :, :], in0=gm_t[:, :, :], in1=gn_t[:, 8:12].unsqueeze(2).to_broadcast([128, 4, 64]), op=ALU.mult),
                    R=[gm_b, gn_b], W=[gm_b])
                zg_t, zg_b = mtile("zgtok", [128, 256])
                DVE.op(lambda gm_t=gm_t, rg_t=rg_t, zg_t=zg_t: nc.vector.tensor_tensor(
                    out=zg_t[:, :], in0=gm_t[:, :, :].rearrange("p h e -> p (h e)"), in1=rg_t[:, :], op=ALU.mult),
                    R=[gm_b, rg_b], W=[zg_b])
                tg_ap, tg_b = bank()
                for c in range(2):
                    PE.op(lambda c=c, zg_t=zg_t: nc.tensor.transpose(tg_ap[:, c * 128:(c + 1) * 128], zg_t[:, c * 128:(c + 1) * 128], ident),
                          R=[zg_b, cst_b], W=[tg_b])
                ACT.op(lambda: nc.scalar.copy(out=zT["gla"][0][:, :, tok], in_=tg_ap[:, 0:256].rearrange("p (c t) -> p c t", t=128)),
                       R=[tg_b], W=[zT["gla"][1][s]])
                for p in range(2):
                    DVE.op(lambda p=p, ebg_t=ebg_t: nc.vector.scalar_tensor_tensor(
                        out=Sst[:, l, p, :], in0=Sst[:, l, p, :], scalar=ebg_t[:, p:p + 1], in1=su3[:, p, :],
                        op0=ALU.mult, op1=ALU.add), R=[Sst_b[l][p], ebg_b, su_b], W=[Sst_b[l][p]])
                    ACT.op(lambda p=p: nc.scalar.copy(out=Sbf[:, l, p, :], in_=Sst[:, l, p, :]), R=[Sst_b[l][p]], W=[Sbf_b[l][p]])

            def run_until(g, tag):
                for t_ in g:
                    if t_ == tag:
                        return True
                return False

            if NS == 2:
                g0, g1 = mix_gen(0), mix_gen(1)
                a0 = a1 = True
                while a0 or a1:
                    if a0:
                        a0 = next(g0) != 'endA'
                    if a1:
                        a1 = next(g1) != 'endA'
                run_until(g0, 'endB')
                run_until(g0, 'endC')
                run_until(g1, 'endB')
                run_until(g0, None)
                run_until(g1, 'endC')
                run_until(g1, None)
            else:
                for s_ in range(NS):
                    run_until(mix_gen(s_), None)


        def merge_and_out(l, i, pv, first):
            wA, wAb = get_slot(l, 16, hold=0)
            wB, wBb = get_slot(l, 17, hold=1)
            wA3 = wA[:, :].rearrange("p (m c) -> p m c", c=1024)
            wB3 = wB[:, :].rearrange("p (m c) -> p m c", c=1024)
            branches = (("ml", wA3, 0, wAb), ("gla", wA3, 2, wAb), ("conf", wB3, 0, wBb), ("sc", wB3, 2, wBb))
            for j in range(8):
                wt, wb = get_slot(l, 8 + j)
                g_t, g_b = gates[j % 2], gates_b[j % 2]
                for b in range(4):
                    pb_ap, pb_b = bank()
                    fgroup(wt, wb, b * 128, 128, pb_ap, pb_b)
                    ACT.op(lambda b=b, pb_ap=pb_ap, g_t=g_t: nc.scalar.activation(
                        out=g_t[:, b, :], in_=pb_ap[:, 0:T], func=AF.Sigmoid, bias=pvec[:, pv + 162 + b * 8 + j:pv + 163 + b * 8 + j]),
                        R=[pb_b, cst_b], W=[g_b])
                acc_t, acc_b = scratch()
                for b, (nm, w3, m0, wbb) in enumerate(branches):
                    pb_ap, pb_b = bank()
                    for kc in range(2):
                        PE.op(lambda kc=kc, nm=nm, w3=w3, m0=m0, pb_ap=pb_ap: nc.tensor.matmul(
                            pb_ap[:, 0:T], lhsT=w3[:, m0 + kc, j * 128:(j + 1) * 128], rhs=zT[nm][0][:, kc, :],
                            start=(kc == 0), stop=(kc == 1)), R=[wbb] + zT[nm][1], W=[pb_b])
                    if b == 0:
                        DVE.op(lambda pb_ap=pb_ap, g_t=g_t, acc_t=acc_t: nc.vector.tensor_tensor(
                            out=acc_t[:, 0:T], in0=pb_ap[:, 0:T], in1=g_t[:, 0, :], op=ALU.mult), R=[pb_b, g_b], W=[acc_b])
                    else:
                        tm_t, tm_b = scratch()
                        if nm == "conf":
                            DVE.op(lambda pb_ap=pb_ap, g_t=g_t, tm_t=tm_t, b=b: nc.vector.scalar_tensor_tensor(
                                out=tm_t[:, 0:T], in0=pb_ap[:, 0:T], scalar=pvec[:, pv + 154 + j:pv + 155 + j], in1=g_t[:, b, :],
                                op0=ALU.add, op1=ALU.mult), R=[pb_b, g_b, cst_b], W=[tm_b])
                        else:
                            DVE.op(lambda pb_ap=pb_ap, g_t=g_t, tm_t=tm_t, b=b: nc.vector.tensor_tensor(
                                out=tm_t[:, 0:T], in0=pb_ap[:, 0:T], in1=g_t[:, b, :], op=ALU.mult), R=[pb_b, g_b], W=[tm_b])
                        if b < 3:
                            POOL.op(lambda tm_t=tm_t, acc_t=acc_t: nc.gpsimd.tensor_tensor(
                                out=acc_t[:, 0:T], in0=acc_t[:, 0:T], in1=tm_t[:, 0:T], op=ALU.add), R=[tm_b, acc_b], W=[acc_b])
                        else:
                            POOL.op(lambda tm_t=tm_t, acc_t=acc_t: nc.gpsimd.tensor_tensor(
                                out=mergedT[:, j, :], in0=acc_t[:, 0:T], in1=tm_t[:, 0:T], op=ALU.add),
                                R=[tm_b, acc_b], W=[merged_b[j]])
            if first:
                dbg_dump("merged", mergedT[:, :, :], merged_b)
                dbg_dump("grow", Grow[:, 0:2, :], Grow_b[0:2])
            if DBG_LEVEL < 6:
                return
            w0, w0b = get_slot(l, 18)
            w1, w1b = get_slot(l, 19)
            for s in range(NS):
                pbs = []
                for n, (wt, wb) in enumerate(((w0, w0b), (w1, w1b))):
                    pb_ap, pb_b = bank()
                    w3 = wt[:, :].rearrange("p (kc c) -> p kc c", c=512)
                    for kc in range(8):
                        PE.op(lambda kc=kc, w3=w3, pb_ap=pb_ap: nc.tensor.matmul(
                            pb_ap[:, :], lhsT=mergedT[:, kc, s * 128:(s + 1) * 128], rhs=w3[:, kc, :],
                            start=(kc == 0), stop=(kc == 7)), R=[wb, merged_b[kc]], W=[pb_b])
                    pbs.append((pb_ap, pb_b))
                postnorm(l, 0, s, pbs)

        def postnorm(l, which, s, pbs):
            st_t, st_b = small()
            ys = []
            for n, (pb_ap, pb_b) in enumerate(pbs):
                j_t, j_b = scratch()
                ACT.op(lambda n=n, pb_ap=pb_ap, j_t=j_t, st_t=st_t: nc.scalar.activation(
                    out=j_t[:, 0:512], in_=pb_ap[:, :], func=AF.Square, accum_out=st_t[:, n:n + 1]), R=[pb_b], W=[j_b, st_b])
                y_t, y_b = scratch()
                DVE.op(lambda n=n, pb_ap=pb_ap, y_t=y_t: nc.vector.tensor_tensor(
                    out=y_t[:, 0:512], in0=pb_ap[:, :], in1=Grow[:, l * 2 + which, n * 512:(n + 1) * 512], op=ALU.mult),
                    R=[pb_b, Grow_b[l * 2 + which]], W=[y_b])
                ys.append((y_t, y_b))
            DVE.op(lambda st_t=st_t: nc.vector.tensor_tensor(out=st_t[:, 2:3], in0=st_t[:, 0:1], in1=st_t[:, 1:2], op=ALU.add),
                   R=[st_b], W=[st_b])
            ACT.op(lambda st_t=st_t: nc.scalar.activation(out=st_t[:, 3:4], in_=st_t[:, 2:3], func=AF.Sqrt, scale=1.0 / D, bias=eps_ap),
                   R=[st_b, cst_b], W=[st_b])
            DVE.op(lambda st_t=st_t: nc.vector.reciprocal(out=st_t[:, 4:5], in_=st_t[:, 3:4]), R=[st_b], W=[st_b])
            for n, (pb_ap, pb_b) in enumerate(pbs):
                y_t, y_b = ys[n]
                DVE.op(lambda n=n, y_t=y_t, st_t=st_t: nc.vector.scalar_tensor_tensor(
                    out=xt[:, s, n * 512:(n + 1) * 512], in0=y_t[:, 0:512], scalar=st_t[:, 4:5], in1=xt[:, s, n * 512:(n + 1) * 512],
                    op0=ALU.mult, op1=ALU.add), R=[y_b, st_b, xt_b[s]], W=[xt_b[s]])

        def ffn(l, i):
            pv = l * PVL
            prenorm(l, i, 1)
            for g in range(11):
                wt, wb = get_slot(l, 20 + g)
                for jj in range(2):
                    j = g * 2 + jj
                    ys = []
                    for part in range(2):
                        ch = part * 22 + j
                        pb_ap, pb_b = bank()
                        fgroup(wt, wb, part * 256 + jj * 128, 128, pb_ap, pb_b)
                        u_t, u_b = scratch()
                        uh_b = Buf()
                        POOL.op(lambda u_t=u_t, ch=ch: nc.gpsimd.tensor_copy(out=u_t[:, 0:2], in_=halo[:, l, ch, :]), R=[halo_b[l][ch]], W=[u_b])
                        ACT.op(lambda u_t=u_t, pb_ap=pb_ap: nc.scalar.copy(out=u_t[:, 2:2 + T], in_=pb_ap[:, 0:T]), R=[pb_b], W=[u_b])
                        POOL.op(lambda u_t=u_t, ch=ch: nc.gpsimd.tensor_copy(out=halo[:, l, ch, :], in_=u_t[:, T:T + 2]), R=[u_b], W=[halo_b[l][ch]])
                        wc = pvec[:, pv + 194 + ch * 3:pv + 194 + ch * 3 + 3]
                        y_t, y_b = scratch()
                        ACT.op(lambda pb_ap=pb_ap, y_t=y_t, wc=wc: nc.scalar.activation(
                            out=y_t[:, 0:T], in_=pb_ap[:, 0:T], func=AF.Identity, scale=wc[:, 2:3]),
                            R=[pb_b, cst_b], W=[y_b])
                        for q in (1, 0):
                            DVE.op(lambda u_t=u_t, y_t=y_t, wc=wc, q=q: nc.vector.scalar_tensor_tensor(
                                out=y_t[:, 0:T], in0=u_t[:, q:q + T], scalar=wc[:, q:q + 1], in1=y_t[:, 0:T],
                                op0=ALU.mult, op1=ALU.add), R=[u_b, cst_b, y_b], W=[y_b])
                        ys.append((y_t, y_b))
                    (ya_t, ya_b), (yv_t, yv_b) = ys
                    ACT.op(lambda ya_t=ya_t: nc.scalar.activation(out=ya_t[:, 0:T], in_=ya_t[:, 0:T], func=AF.Silu), R=[ya_b], W=[ya_b])
                    DVE.op(lambda ya_t=ya_t, yv_t=yv_t, j=j: nc.vector.tensor_tensor(
                        out=gff[:, j, :], in0=ya_t[:, 0:T], in1=yv_t[:, 0:T], op=ALU.mult), R=[ya_b, yv_b], W=[gff_b[j]])
            accs = {}
            for s in range(NS):
                for n in range(2):
                    accs[(s, n)] = bank()
            for q in range(6):
                wt, wb = get_slot(l, 31 + q)
                w3 = wt[:, :].rearrange("p (m c) -> p m c", c=1024)
                nk = 4 if q < 5 else 2
                for s in range(NS):
                    for n in range(2):
                        pb_ap, pb_b = accs[(s, n)]
                        for m in range(nk):
                            j = q * 4 + m
                            PE.op(lambda m=m, j=j, w3=w3, pb_ap=pb_ap, s=s, n=n: nc.tensor.matmul(
                                pb_ap[:, :], lhsT=gff[:, j, s * 128:(s + 1) * 128], rhs=w3[:, m, n * 512:(n + 1) * 512],
                                start=(j == 0), stop=(j == 21)), R=[wb, gff_b[j]], W=[pb_b])
            for s in range(NS):
                postnorm(l, 1, s, [accs[(s, 0)], accs[(s, 1)]])

        for i in range(nseq):
            seq_init(i)
            for tl in range(ntiles):
                src = x_d[i, tl * T:(tl + 1) * T, :].rearrange("(s p) d -> p s d", p=128)
                POOL.dma(xt[:, :, :], src, s_xin, W=xt_b)
                for l in range(depth):
                    if DBG_LEVEL >= 1:
                        token_mixer(l, i)
                    if DBG_LEVEL >= 7:
                        ffn(l, i)
                dst = out_d[i, tl * T:(tl + 1) * T, :].rearrange("(s p) d -> p s d", p=128)
                POOL.dma(dst, xt[:, :, :], s_xout, R=xt_b)
        allsems = [PE.sem, ACT.sem, DVE.sem, POOL.sem, SP.sem, s_xin, s_xout, s_c] + slot_s + hold_s + list(prep_sems.values()) + list(dbg_sems.values()) + diag_sems
        POOL.wait_all(allsems)
        SP.wait_all([s_xout])
    return nc


def host_pack(inp, core):
    f = np.float32
    b0 = core * 2
    cT = np.ascontiguousarray(inp["c"][b0:b0 + 2].reshape(2, 8, 128).transpose(2, 1, 0).reshape(128, 16)).astype(f)
    pvec = np.zeros((128, DEPTH * PVL), f)
    brow = np.zeros((DEPTH * BRL,), f)
    w2 = np.zeros((16, DEPTH * 256), f)

    def fm(v, nch):
        return np.asarray(v, f).reshape(nch, 128).T

    for l in range(DEPTH):
        o = l * PVL
        pvec[:, o + 0:o + 8] = fm(inp["tm_pre_g"][l], 8)
        pvec[:, o + 8:o + 16] = fm(inp["tm_post_g"][l], 8)
        pvec[:, o + 16:o + 24] = fm(inp["cm_pre_g"][l], 8)
        pvec[:, o + 24:o + 32] = fm(inp["cm_post_g"][l], 8)
        pvec[:, o + 32:o + 80] = fm(inp["ada_b"][l], 48)
        cw = np.asarray(inp["conf_dw_w"][l], f)
        for c in range(2):
            pvec[:, o + 80 + c * 31:o + 80 + (c + 1) * 31] = cw[:, c * 128:(c + 1) * 128].T
        pvec[:, o + 142:o + 144] = fm(inp["conf_dw_b"][l], 2)
        pvec[:, o + 144:o + 146] = fm(inp["conf_ln_g"][l], 2)
        pvec[:, o + 146:o + 148] = fm(inp["conf_ln_b"][l], 2)
        sw = np.asarray(inp["sc_dw_w"][l], f)
        for c in range(2):
            pvec[:, o + 148 + c * 3:o + 148 + (c + 1) * 3] = sw[:, c * 128:(c + 1) * 128].T
        pvec[:, o + 154:o + 162] = fm(inp["conf_out_b"][l], 8)
        pvec[:, o + 162:o + 194] = fm(inp["merge_gate_b"][l], 32)
        fw = np.asarray(inp["ffn_dw_w"][l], f)
        for ch in range(44):
            pvec[:, o + 194 + ch * 3:o + 194 + (ch + 1) * 3] = fw[:, ch * 128:(ch + 1) * 128].T
        r = l * BRL
        brow[r + 0:r + 4] = inp["ml_i_bias"][l]
        brow[r + 4:r + 8] = inp["ml_f_bias"][l]
        brow[r + 8:r + 264] = inp["ml_norm_g"][l]
        brow[r + 264:r + 520] = inp["gla_norm_g"][l]
        brow[r + 520:r + 776] = inp["gla_a_bias"][l]
        w2[:, l * 256:(l + 1) * 256] = inp["gla_w_a2"][l]
    brow = np.ascontiguousarray(np.broadcast_to(brow[None, :], (128, DEPTH * BRL)))
    return cT, pvec, brow, w2


def make_consts():
    f = np.float32
    c = np.zeros((128, 1152), f)
    c[:, 0:128] = np.eye(128, dtype=f)
    c[:, 128:256] = 1.0
    s = np.arange(128)[:, None]
    t = np.arange(128)[None, :]
    U = (s <= t).astype(f)
    c[:, 256:384] = U
    c[:, 384:512] = U / 16.0
    c[:, 512:640] = (s > t).astype(f) / 16.0
    c[:, 640:1152] = np.tile(U, (1, 4))
    return c


BIG = ("ada_w", "w_in", "w_ml_out", "w_gla_out", "w_conf_out", "w_sc_out", "w_o", "ffn_w_up", "ffn_w_down")


def make_in_maps(inp, ncores, nseq, ntok):
    consts = make_consts()
    big = {k: np.ascontiguousarray(np.asarray(inp[k], np.float32)) for k in BIG}
    maps = []
    for core in range(ncores):
        cT, pvec, brow, w2 = host_pack(inp, core)
        m = {"x": np.ascontiguousarray(np.asarray(inp["x"][core * 2:core * 2 + nseq, :ntok], np.float32)),
             "cT": cT, "pvec": pvec, "brow": brow, "w2": w2, "consts": consts}
        m.update(big)
        maps.append(m)
    return maps


def kernel(**inputs):
    nc = build()
    maps = make_in_maps(inputs, NCORES, 2, SEQ)
    res = run_bass_kernel_spmd(nc, maps, core_ids=list(range(NCORES)))
    out = np.concatenate([np.asarray(r["out"]) for r in res.results], axis=0)
    return out.astype(np.float32)
```

```python
import contextlib
import numpy as np
import concourse.bass as bass
import concourse.mybir as mybir
from concourse.bass_utils import run_bass_kernel_spmd

F32 = mybir.dt.float32
BF16 = mybir.dt.bfloat16
AF = mybir.ActivationFunctionType
ALU = mybir.AluOpType
AX = mybir.AxisListType

D = 1024
MIX = 256
HID = 2816
INW = 7448
DEPTH = 2
SEQ = 4096
BATCH = 16
NCORES = 8
T = 256
NS = T // 128
NSLOT = 4
SLOTS_PER_LAYER = 39
EPS = 1e-6
PVL = 326
BRL = 776
GATE0 = 3352
DBG_LEVEL = 99


class Sem:
    def __init__(self, h):
        self.h = h
        self.count = 0


class Buf:
    __slots__ = ("w", "r", "excl")

    def __init__(self, excl=False):
        self.w = None
        self.r = {}
        self.excl = excl


class Eng:
    def __init__(self, e, sem, is_pe=False):
        self.e = e
        self.sem = sem
        self.seen = {}
        self.is_pe = is_pe
        self.hook = None
        self._inhook = False

    def _sync(self, reads, writes):
        need = {}
        for b in reads:
            if b.w is not None and need.get(b.w[0], 0) < b.w[1]:
                need[b.w[0]] = b.w[1]
        for b in writes:
            if b.w is not None and need.get(b.w[0], 0) < b.w[1]:
                need[b.w[0]] = b.w[1]
            for sm, v in b.r.items():
                if need.get(sm, 0) < v:
                    need[sm] = v
        for sm, v in need.items():
            if self.is_pe and sm is self.sem:
                continue
            if self.seen.get(sm, 0) < v:
                self.e.wait_ge(sm.h, v)
                self.seen[sm] = v

    @staticmethod
    def _mark(sm, reads, writes):
        v = sm.count
        for b in reads:
            if b.r.get(sm, 0) < v:
                b.r[sm] = v
        for b in writes:
            b.w = (sm, v)
            b.r = {}

    def op(self, fn, R=(), W=(), drain=False):
        if drain and self.sem.count > 0 and self.seen.get(self.sem, 0) < self.sem.count:
            self.e.wait_ge(self.sem.h, self.sem.count)
            self.seen[self.sem] = self.sem.count
        ex = [b for b in R if b.excl]
        if ex:
            W = list(W) + ex
        self._sync(R, W)
        inst = fn()
        self.sem.count += 1
        inst.then_inc(self.sem.h, 1)
        self._mark(self.sem, R, W)
        if self.hook is not None and not self._inhook:
            self._inhook = True
            self.hook()
            self._inhook = False

    def dma(self, out, in_, dsem, R=(), W=()):
        self._sync(R, W)
        inst = self.e.dma_start(out=out, in_=in_)
        dsem.count += 16
        inst.then_inc(dsem.h, 16)
        self._mark(dsem, R, W)

    def wait_all(self, sems):
        for sm in sems:
            if sm.count > 0 and self.seen.get(sm, 0) < sm.count:
                self.e.wait_ge(sm.h, sm.count)
                self.seen[sm] = sm.count


def build(nseq=2, ntiles=SEQ // T, depth=DEPTH, dbg=None):
    nc = bass.Bass("TRN2", target_bir_lowering=False)
    ntok = ntiles * T
    dt = nc.dram_tensor
    x_d = dt("x", [nseq, ntok, D], F32, kind="ExternalInput").ap()
    out_d = dt("out", [nseq, ntok, D], F32, kind="ExternalOutput").ap()
    cT_d = dt("cT", [128, 16], F32, kind="ExternalInput").ap()
    pvec_d = dt("pvec", [128, DEPTH * PVL], F32, kind="ExternalInput").ap()
    brow_d = dt("brow", [128, DEPTH * BRL], F32, kind="ExternalInput").ap()
    w2_d = dt("w2", [16, DEPTH * 256], F32, kind="ExternalInput").ap()
    const_d = dt("consts", [128, 1152], F32, kind="ExternalInput").ap()
    adaw_d = dt("ada_w", [DEPTH, D, 6 * D], F32, kind="ExternalInput").ap()
    win_d = dt("w_in", [DEPTH, D, INW], F32, kind="ExternalInput").ap()
    wml_d = dt("w_ml_out", [DEPTH, MIX, D], F32, kind="ExternalInput").ap()
    wgl_d = dt("w_gla_out", [DEPTH, MIX, D], F32, kind="ExternalInput").ap()
    wcf_d = dt("w_conf_out", [DEPTH, MIX, D], F32, kind="ExternalInput").ap()
    wsc_d = dt("w_sc_out", [DEPTH, MIX, D], F32, kind="ExternalInput").ap()
    wo_d = dt("w_o", [DEPTH, D, D], F32, kind="ExternalInput").ap()
    wup_d = dt("ffn_w_up", [DEPTH, D, 2 * HID], F32, kind="ExternalInput").ap()
    wdn_d = dt("ffn_w_down", [DEPTH, HID, D], F32, kind="ExternalInput").ap()
    scr_d = dt("wscratch", [DEPTH * SLOTS_PER_LAYER, 128, 4096], BF16, kind="Internal").ap()
    dbg_d = {}
    if dbg:
        for name, shape in dbg.items():
            dbg_d[name] = dt("dbg_" + name, list(shape), F32, kind="ExternalOutput").ap()

    es = contextlib.ExitStack()
    with es:
        def sb(name, shape, dtype=F32):
            return es.enter_context(nc.sbuf_tensor("sb_" + name, list(shape), dtype))

        def sem(name):
            return Sem(es.enter_context(nc.semaphore(name)))

        PE = Eng(nc.tensor, sem("s_pe"), is_pe=True)
        ACT = Eng(nc.scalar, sem("s_act"))
        DVE = Eng(nc.vector, sem("s_dve"))
        POOL = Eng(nc.gpsimd, sem("s_pool"))
        SP = Eng(nc.sync, sem("s_sp"))

        consts = sb("consts", [128, 1152])
        pvec = sb("pvec", [128, DEPTH * PVL])
        brow = sb("brow", [128, DEPTH * BRL])
        w2 = sb("w2", [16, DEPTH * 256])
        cact = sb("cact", [128, 16])
        cst_b = Buf()
        s_c = sem("s_const")
        for t_, d_ in ((consts, const_d), (pvec, pvec_d), (brow, brow_d), (w2, w2_d), (cact, cT_d)):
            SP.dma(t_[:, :], d_, s_c, W=[cst_b])
        ident = consts[:, 0:128]
        ones = consts[:, 128:256]
        Umat = consts[:, 256:384]
        U16 = consts[:, 384:512]
        SU16 = consts[:, 512:640]
        mask4 = consts[:, 640:1152].rearrange("p (h t) -> p h t", t=128)

        prep_sems = {}
        scr_b = {}

        def prep(l, s, out_ap, in_ap, grp):
            key = (l, grp)
            if key not in prep_sems:
                prep_sems[key] = sem("s_prep%d_%d" % key)
            sm = prep_sems[key]
            inst = nc.gpsimd.dma_start(out=out_ap, in_=in_ap)
            sm.count += 16
            inst.then_inc(sm.h, 16)
            scr_b.setdefault((l, s), Buf())

        slot_grp = {}

        def scr_view(l, s, pat, **kw):
            return scr_d[l * SLOTS_PER_LAYER + s].rearrange(pat, **kw)

        def k1024(src, c0, n):
            return src.rearrange("(kc p) c -> p kc c", p=128)[:, :, c0:c0 + n]

        for l in range(depth):
            wi = win_d[l]
            for s, (c0, n) in {0: (0, 512), 1: (512, 512), 3: (1032, 512), 4: (1544, 512),
                               5: (2072, 512), 6: (2584, 512), 7: (3096, 256)}.items():
                prep(l, s, scr_view(l, s, "p (kc c) -> p kc c", c=512)[:, :, 0:n], k1024(wi, c0, n), 0)
                slot_grp[(l, s)] = 0
            prep(l, 2, scr_view(l, 2, "p (kc c) -> p kc c", c=512)[:, :, 0:8], k1024(wi, 1024, 8), 0)
            prep(l, 2, scr_view(l, 2, "p (kc c) -> p kc c", c=512)[:, :, 8:24], k1024(wi, 2056, 16), 0)
            slot_grp[(l, 2)] = 0
            for j in range(8):
                for b in range(4):
                    prep(l, 8 + j, scr_view(l, 8 + j, "p (kc c) -> p kc c", c=512)[:, :, b * 128:(b + 1) * 128],
                         k1024(wi, GATE0 + b * 1024 + j * 128, 128), 1)
                slot_grp[(l, 8 + j)] = 1
            for s, (wa, wb) in {16: (wml_d, wgl_d), 17: (wcf_d, wsc_d)}.items():
                v = scr_view(l, s, "p (m c) -> p m c", c=1024)
                prep(l, s, v[:, 0:2, :], wa[l].rearrange("(kc p) c -> p kc c", p=128), 1)
                prep(l, s, v[:, 2:4, :], wb[l].rearrange("(kc p) c -> p kc c", p=128), 1)
                slot_grp[(l, s)] = 1
            for n in range(2):
                prep(l, 18 + n, scr_view(l, 18 + n, "p (kc c) -> p kc c", c=512), k1024(wo_d[l], n * 512, 512), 1)
                slot_grp[(l, 18 + n)] = 1
            for g in range(11):
                v = scr_view(l, 20 + g, "p (kc c) -> p kc c", c=512)
                prep(l, 20 + g, v[:, :, 0:256], k1024(wup_d[l], g * 256, 256), 2)
                prep(l, 20 + g, v[:, :, 256:512], k1024(wup_d[l], HID + g * 256, 256), 2)
                slot_grp[(l, 20 + g)] = 2
            for q in range(6):
                nk = 4 if q < 5 else 2
                v = scr_view(l, 31 + q, "p (m c) -> p m c", c=1024)
                prep(l, 31 + q, v[:, 0:nk, :], wdn_d[l].rearrange("(kc p) c -> p kc c", p=128)[:, 4 * q:4 * q + nk, :], 2)
                slot_grp[(l, 31 + q)] = 2
        for (l, s), b in scr_b.items():
            sm = prep_sems[(l, slot_grp[(l, s)])]
            b.w = (sm, sm.count)

        slots = [sb("wslot%d" % i, [128, 4096], BF16) for i in range(NSLOT)]
        slot_b = [Buf() for _ in range(NSLOT)]
        slot_s = [sem("s_slot%d" % i) for i in range(NSLOT)]
        slot_ctr = [0]

        def next_slot():
            i = slot_ctr[0] % NSLOT
            slot_ctr[0] += 1
            return i

        hold_t = [sb("whold%d" % i, [128, 4096], BF16) for i in range(2)]
        hold_b = [Buf(), Buf()]
        hold_s = [sem("s_hold%d" % i) for i in range(2)]

        def get_slot(l, s, hold=None):
            if hold is not None:
                SP.dma(hold_t[hold][:, :], scr_d[l * SLOTS_PER_LAYER + s], hold_s[hold], R=[scr_b[(l, s)]], W=[hold_b[hold]])
                return hold_t[hold], hold_b[hold]
            i = next_slot()
            dst = slots[i][:, :]
            src = scr_d[l * SLOTS_PER_LAYER + s]
            if s == 2:
                dst = dst.rearrange("p (kc c) -> p kc c", c=512)[:, :, 0:24]
                src = src.rearrange("p (kc c) -> p kc c", c=512)[:, :, 0:24]
            elif s == 7:
                dst = dst.rearrange("p (kc c) -> p kc c", c=512)[:, :, 0:256]
                src = src.rearrange("p (kc c) -> p kc c", c=512)[:, :, 0:256]
            elif s == 36:
                dst = dst[:, 0:2048]
                src = src[:, 0:2048]
            elif s >= 37:
                dst = dst[:, 0:3968]
                src = src[:, 0:3968]
            SP.dma(dst, src, slot_s[i], R=[scr_b[(l, s)]], W=[slot_b[i]])
            return slots[i], slot_b[i]

        ps_all = es.enter_context(nc.psum_tensor("ps", [128, 4096], F32))
        bank_b = [Buf(excl=True) for _ in range(8)]
        bank_ctr = [0]

        def bank():
            i = bank_ctr[0] % 8
            bank_ctr[0] += 1
            return ps_all[:, i * 512:(i + 1) * 512], bank_b[i]

        NSCR = 6
        scr_t = [sb("scr%d" % i, [128, 544]) for i in range(NSCR)]
        scr_bf = [Buf() for _ in range(NSCR)]
        scr_ctr = [0]

        def scratch():
            i = scr_ctr[0] % NSCR
            scr_ctr[0] += 1
            return scr_t[i], scr_bf[i]

        NSM = 16
        sm_t = [sb("sm%d" % i, [128, 16]) for i in range(NSM)]
        sm_bf = [Buf() for _ in range(NSM)]
        sm_ctr = [0]

        def small():
            i = sm_ctr[0] % NSM
            sm_ctr[0] += 1
            return sm_t[i], sm_bf[i]

        xt = sb("xt", [128, NS, D])
        xt_b = [Buf() for _ in range(NS)]
        s_xin = sem("s_xin")
        s_xout = sem("s_xout")
        xn = [sb("xn%d" % i, [128, D]) for i in range(2)]
        xn_b = [Buf(), Buf()]
        hT = sb("hT", [128, 8, T], BF16)
        hT_b = [[Buf(), Buf()] for _ in range(NS)]
        fq = {}
        for nm in ("mlq", "mlk", "gq", "gk"):
            fq[nm] = (sb(nm + "T", [128, 2, T], BF16), [Buf(), Buf()])
        tk = {}
        for nm in ("mlk", "mlv", "sigo", "glk", "gv", "sgr"):
            tk[nm] = (sb(nm + "_tok", [128, NS, 256], BF16), [Buf() for _ in range(NS)])
        iff = sb("iff", [128, NS, 8])
        iff_b = [Buf() for _ in range(NS)]
        gaT = sb("gaT", [16, T])
        gaT_b = Buf()
        zc = sb("zc", [128, 2, 30 + T], BF16)
        zc_b = [Buf(), Buf()]
        zs = sb("zs", [128, 2, 2 + T])
        zs_b = [Buf(), Buf()]
        bgs = sb("bgs", [128, 2, T], BF16)
        bgs_b = [Buf(), Buf()]
        cgs = sb("cgs", [128, 2, T])
        cgs_b = [Buf(), Buf()]
        uconv = sb("uconv", [128, 2, T])
        uconv_b = [Buf(), Buf()]
        zT = {}
        for nm in ("ml", "gla", "conf", "sc"):
            zT[nm] = (sb("z" + nm + "T", [128, 2, T], BF16), [Buf() for _ in range(max(NS, 2))])
        gates = [sb("gates%d" % i, [128, 4, T], BF16) for i in range(2)]
        gates_b = [Buf(), Buf()]
        mergedT = sb("mergedT", [128, 8, T], BF16)
        merged_b = [Buf() for _ in range(8)]
        gff = sb("gff", [128, 22, T], BF16)
        gff_b = [Buf() for _ in range(22)]
        halo = sb("halo", [128, DEPTH, 44, 2])
        halo_b = [[Buf() for _ in range(44)] for _ in range(DEPTH)]
        Cst = sb("Cst", [128, DEPTH, 2, 72])
        Cst_b = [[Buf(), Buf()] for _ in range(DEPTH)]
        Cbf = sb("Cbf", [128, DEPTH, 2, 72], BF16)
        Cbf_b = [[Buf(), Buf()] for _ in range(DEPTH)]
        Sst = sb("Sst", [128, DEPTH, 2, 64])
        Sst_b = [[Buf(), Buf()] for _ in range(DEPTH)]
        Sbf = sb("Sbf", [128, DEPTH, 2, 64], BF16)
        Sbf_b = [[Buf(), Buf()] for _ in range(DEPTH)]
        zch_b = [[Buf(), Buf()] for _ in range(DEPTH)]
        zch = sb("zch", [128, DEPTH, 2, 30], BF16)
        zsh = sb("zsh", [128, DEPTH, 2, 2])
        zsh_b = [[Buf(), Buf()] for _ in range(DEPTH)]
        Grow = sb("Grow", [128, DEPTH * 2, D])
        Grow_b = [Buf() for _ in range(DEPTH * 2)]
        modT = sb("modT", [128, DEPTH * 2 * 48])
        modT_b = Buf()
        modA = sb("modA", [128, DEPTH * 2 * 2 * 8])
        modA_b = Buf()
        mt = {}

        def mtile(name, shape, dtype=F32, n=None):
            if n is None:
                n = 2 if dtype == BF16 else 1
            if name not in mt:
                mt[name] = ([sb("%s_%d" % (name, i), shape, dtype) for i in range(n)], [Buf() for _ in range(n)], [0])
            tl, bl, c = mt[name]
            i = c[0] % n
            c[0] += 1
            return tl[i], bl[i]

        def dbg_dump(name, ap, b, idx=None):
            if dbg and name in dbg_d:
                dst = dbg_d[name] if idx is None else dbg_d[name][idx]
                sm_ = dbg_sems.setdefault(name, sem("s_dbg_" + name))
                POOL.dma(dst, ap, sm_, R=b)

        dbg_sems = {}
        dbg_state = []

        ACT.op(lambda: nc.scalar.activation(out=cact[:, :], in_=cact[:, :], func=AF.Silu), R=[cst_b], W=[cst_b])
        cact3 = cact[:, :].rearrange("p (kc i) -> p kc i", i=2)
        mod_ps, mod_pb = bank()
        for l in range(depth):
            for g in range(24):
                i = next_slot()
                sl32 = slots[i][:, :].bitcast(F32).rearrange("p (kc c) -> p kc c", c=256)
                SP.dma(sl32, adaw_d[l].rearrange("(kc p) c -> p kc c", p=128)[:, :, g * 256:(g + 1) * 256],
                       slot_s[i], W=[slot_b[i]])
                for fc in range(2):
                    col = (l * 48 + g * 2 + fc) * 2
                    for kc in range(8):
                        PE.op(lambda kc=kc, fc=fc, col=col, sl32=sl32: nc.tensor.matmul(
                            mod_ps[:, col:col + 2], lhsT=sl32[:, kc, fc * 128:(fc + 1) * 128], rhs=cact3[:, kc, :],
                            start=(kc == 0), stop=(kc == 7)), R=[slot_b[i], cst_b], W=[mod_pb])
        for l in range(depth):
            for i in range(2):
                o = (l * 2 + i) * 48
                src = mod_ps[:, l * 96:(l + 1) * 96].rearrange("p (v i) -> p v i", i=2)[:, :, i]
                DVE.op(lambda o=o, src=src, l=l: nc.vector.tensor_tensor(
                    out=modT[:, o:o + 48], in0=src, in1=pvec[:, l * PVL + 32:l * PVL + 80], op=ALU.add),
                    R=[mod_pb, cst_b], W=[modT_b])
        for l in range(depth):
            for i in range(2):
                o = (l * 2 + i) * 48
                for which in range(2):
                    a = ((l * 2 + i) * 2 + which) * 8
                    DVE.op(lambda o=o, a=a, which=which, l=l: nc.vector.scalar_tensor_tensor(
                        out=modA[:, a:a + 8], in0=modT[:, o + which * 24 + 8:o + which * 24 + 16], scalar=1.0,
                        in1=pvec[:, l * PVL + which * 16:l * PVL + which * 16 + 8], op0=ALU.add, op1=ALU.mult),
                        R=[modT_b, cst_b], W=[modA_b])

        diag_sems = []
        for l in range(depth):
            for c in range(2):
                s_diag = sem("s_diag%d_%d" % (l, c))
                diag_sems.append(s_diag)
                i_ = next_slot()
                for j in range(31):
                    DVE.op(lambda i_=i_, j=j, l=l, c=c: nc.vector.tensor_scalar(
                        out=slots[i_][:, j * 128:(j + 1) * 128], in0=ident,
                        scalar1=pvec[:, l * PVL + 80 + c * 31 + j:l * PVL + 80 + c * 31 + j + 1], scalar2=None, op0=ALU.mult),
                        R=[cst_b], W=[slot_b[i_]])
                scr_b[(l, 37 + c)] = Buf()
                SP.dma(scr_d[l * SLOTS_PER_LAYER + 37 + c][:, 0:3968], slots[i_][:, 0:3968], s_diag,
                       R=[slot_b[i_]], W=[scr_b[(l, 37 + c)]])
        def modA_ap(l, i, which):
            a = ((l * 2 + i) * 2 + which) * 8
            return modA[:, a:a + 8]

        def modB_ap(l, i, which):
            o = (l * 2 + i) * 48 + which * 24
            return modT[:, o:o + 8]

        def seq_init(i):
            for l in range(depth):
                for which in range(2):
                    o = (l * 2 + i) * 48 + which * 24 + 16
                    gv_t, gv_b = small()
                    DVE.op(lambda o=o, l=l, which=which, gv_t=gv_t: nc.vector.tensor_tensor(
                        out=gv_t[:, 0:8], in0=modT[:, o:o + 8],
                        in1=pvec[:, l * PVL + 8 + which * 16:l * PVL + 16 + which * 16], op=ALU.mult),
                        R=[modT_b, cst_b], W=[gv_b])
                    for half in range(2):
                        pb_ap, pb_b = bank()
                        for q in range(4):
                            kc = half * 4 + q
                            vb_t, vb_b = scratch()
                            DVE.op(lambda vb_t=vb_t, gv_t=gv_t, kc=kc: nc.vector.tensor_scalar(
                                out=vb_t[:, 0:128], in0=ones, scalar1=gv_t[:, kc:kc + 1], scalar2=None, op0=ALU.mult),
                                R=[cst_b, gv_b], W=[vb_b])
                            PE.op(lambda vb_t=vb_t, q=q, pb_ap=pb_ap: nc.tensor.matmul(
                                pb_ap[:, q * 128:(q + 1) * 128], lhsT=vb_t[:, 0:128], rhs=ident, start=True, stop=True),
                                R=[vb_b, cst_b], W=[pb_b])
                        ACT.op(lambda l=l, which=which, half=half, pb_ap=pb_ap: nc.scalar.copy(
                            out=Grow[:, l * 2 + which, half * 512:(half + 1) * 512], in_=pb_ap),
                            R=[pb_b], W=[Grow_b[l * 2 + which]])
            for l in range(depth):
                for p in range(2):
                    DVE.op(lambda l=l, p=p: nc.vector.memset(Cst[:, l, p, :], 0.0), W=[Cst_b[l][p]])
                    DVE.op(lambda l=l, p=p: nc.vector.memset(Cbf[:, l, p, :], 0.0), W=[Cbf_b[l][p]])
                    DVE.op(lambda l=l, p=p: nc.vector.memset(Sst[:, l, p, :], 0.0), W=[Sst_b[l][p]])
                    DVE.op(lambda l=l, p=p: nc.vector.memset(Sbf[:, l, p, :], 0.0), W=[Sbf_b[l][p]])
                    DVE.op(lambda l=l, p=p: nc.vector.memset(zch[:, l, p, :], 0.0), W=[zch_b[l][p]])
                    DVE.op(lambda l=l, p=p: nc.vector.memset(zsh[:, l, p, :], 0.0), W=[zsh_b[l][p]])
                for ch in range(44):
                    pass
                DVE.op(lambda l=l: nc.vector.memset(halo[:, l, :, :], 0.0), W=halo_b[l])

        def prenorm(l, i, which):
            A = modA_ap(l, i, which)
            Bv = modB_ap(l, i, which)
            for s in range(NS):
                xs = xt[:, s, :]
                xn_t, xn_bb = xn[s % 2], xn_b[s % 2]
                st_t, st_b = small()
                ACT.op(lambda xn_t=xn_t, xs=xs, st_t=st_t: nc.scalar.activation(
                    out=xn_t[:, :], in_=xs, func=AF.Square, accum_out=st_t[:, 0:1]), R=[xt_b[s]], W=[xn_bb, st_b])
                ACT.op(lambda st_t=st_t: nc.scalar.activation(
                    out=st_t[:, 1:2], in_=st_t[:, 0:1], func=AF.Sqrt, scale=1.0 / D, bias=eps_ap), R=[st_b, cst_b], W=[st_b])
                DVE.op(lambda st_t=st_t: nc.vector.reciprocal(out=st_t[:, 2:3], in_=st_t[:, 1:2]), R=[st_b], W=[st_b])
                DVE.op(lambda xn_t=xn_t, xs=xs, st_t=st_t: nc.vector.tensor_scalar(
                    out=xn_t[:, :], in0=xs, scalar1=st_t[:, 2:3], scalar2=None, op0=ALU.mult),
                    R=[xt_b[s], st_b], W=[xn_bb])
                for half in range(2):
                    pb_ap, pb_b = bank()
                    for q in range(4):
                        kc = half * 4 + q
                        PE.op(lambda xn_t=xn_t, kc=kc, q=q, pb_ap=pb_ap: nc.tensor.transpose(
                            pb_ap[:, q * 128:(q + 1) * 128], xn_t[:, kc * 128:(kc + 1) * 128], ident),
                            R=[xn_bb, cst_b], W=[pb_b])
                    for q in range(4):
                        kc = half * 4 + q
                        if half == 0:
                            ACT.op(lambda kc=kc, q=q, pb_ap=pb_ap, s=s: nc.scalar.activation(
                                out=hT[:, kc, s * 128:(s + 1) * 128], in_=pb_ap[:, q * 128:(q + 1) * 128],
                                func=AF.Identity, scale=A[:, kc:kc + 1], bias=Bv[:, kc:kc + 1]),
                                R=[pb_b, modA_b, modT_b], W=[hT_b[s][half]])
                        else:
                            DVE.op(lambda kc=kc, q=q, pb_ap=pb_ap, s=s: nc.vector.tensor_scalar(
                                out=hT[:, kc, s * 128:(s + 1) * 128], in0=pb_ap[:, q * 128:(q + 1) * 128],
                                scalar1=A[:, kc:kc + 1], scalar2=Bv[:, kc:kc + 1], op0=ALU.mult, op1=ALU.add),
                                R=[pb_b, modA_b, modT_b], W=[hT_b[s][half]])

        epsc = sb("epsc", [128, 2])
        eps_b = Buf()
        DVE.op(lambda: nc.vector.memset(epsc[:, 0:1], EPS), W=[cst_b])
        DVE.op(lambda: nc.vector.memset(epsc[:, 1:2], 1.0), W=[cst_b])
        eps_ap = epsc[:, 0:1]
        one_ap = epsc[:, 1:2]

        def fgroup(wt, wb, c0, m, pb_ap, pb_b, s_lo=0, s_hi=NS):
            w3 = wt[:, :].rearrange("p (kc c) -> p kc c", c=512)
            n0, n1 = s_lo * 128, s_hi * 128
            for kc in range(8):
                PE.op(lambda kc=kc: nc.tensor.matmul(pb_ap[0:m, n0:n1], lhsT=w3[:, kc, c0:c0 + m], rhs=hT[:, kc, n0:n1],
                                                     start=(kc == 0), stop=(kc == 7)),
                      R=[wb] + [b_ for s_ in range(s_lo, s_hi) for b_ in hT_b[s_]], W=[pb_b])

        def tgroup(wt, wb, c0, n, s, pb_ap, pb_b):
            w3 = wt[:, :].rearrange("p (kc c) -> p kc c", c=512)
            for kc in range(8):
                PE.op(lambda kc=kc: nc.tensor.matmul(pb_ap[:, 0:n], lhsT=hT[:, kc, s * 128:(s + 1) * 128],
                                                     rhs=w3[:, kc, c0:c0 + n], start=(kc == 0), stop=(kc == 7)),
                      R=[wb] + hT_b[s], W=[pb_b])

        def token_mixer(l, i):
            pv = l * PVL
            br = l * BRL
            prenorm(l, i, 0)
            first = dbg and not dbg_state
            if first:
                dbg_state.append(1)
                dbg_dump("hT", hT[:, :, :], [b_ for x_ in hT_b for b_ in x_])
            if DBG_LEVEL < 1.1:
                return
            wt, wb = get_slot(l, 0)
            for nm, cbase, scale in (("mlq", 0, 0.125), ("mlk", 256, 1.0)):
                for c in range(2):
                    pb_ap, pb_b = bank()
                    fgroup(wt, wb, cbase + c * 128, 128, pb_ap, pb_b)
                    ACT.op(lambda nm=nm, c=c, pb_ap=pb_ap, scale=scale: nc.scalar.activation(
                        out=fq[nm][0][:, c, :], in_=pb_ap[:, 0:T], func=AF.Copy, scale=scale), R=[pb_b], W=[fq[nm][1][c]])
            for s in range(NS):
                pb_ap, pb_b = bank()
                tgroup(wt, wb, 256, 256, s, pb_ap, pb_b)
                ACT.op(lambda s=s, pb_ap=pb_ap: nc.scalar.copy(out=tk["mlk"][0][:, s, :], in_=pb_ap[:, 0:256]),
                       R=[pb_b], W=[tk["mlk"][1][s]])
            if DBG_LEVEL < 1.2:
                return
            wt, wb = get_slot(l, 1)
            for s in range(NS):
                pb_ap, pb_b = bank()
                tgroup(wt, wb, 0, 512, s, pb_ap, pb_b)
                DVE.op(lambda s=s, pb_ap=pb_ap: nc.vector.tensor_copy(out=tk["mlv"][0][:, s, :], in_=pb_ap[:, 0:256]),
                       R=[pb_b], W=[tk["mlv"][1][s]])
                ACT.op(lambda s=s, pb_ap=pb_ap: nc.scalar.activation(
                    out=tk["sigo"][0][:, s, :], in_=pb_ap[:, 256:512], func=AF.Sigmoid), R=[pb_b], W=[tk["sigo"][1][s]])
            if DBG_LEVEL < 1.4:
                return
            wt, wb = get_slot(l, 2)
            for s in range(NS):
                pb_ap, pb_b = bank()
                tgroup(wt, wb, 0, 8, s, pb_ap, pb_b)
                DVE.op(lambda s=s, pb_ap=pb_ap: nc.vector.tensor_tensor(
                    out=iff[:, s, :], in0=pb_ap[:, 0:8], in1=brow[:, br:br + 8], op=ALU.add),
                    R=[pb_b, cst_b], W=[iff_b[s]])
            pb_ap, pb_b = bank()
            fgroup(wt, wb, 8, 16, pb_ap, pb_b)
            ACT.op(lambda pb_ap=pb_ap: nc.scalar.copy(out=gaT[:, :], in_=pb_ap[0:16, 0:T]), R=[pb_b], W=[gaT_b])
            if DBG_LEVEL < 1.6:
                return
            wt, wb = get_slot(l, 3)
            for nm, cbase, scale in (("gq", 0, 0.125), ("gk", 256, 1.0)):
                for c in range(2):
                    pb_ap, pb_b = bank()
                    fgroup(wt, wb, cbase + c * 128, 128, pb_ap, pb_b)
                    ACT.op(lambda nm=nm, c=c, pb_ap=pb_ap, scale=scale: nc.scalar.activation(
                        out=fq[nm][0][:, c, :], in_=pb_ap[:, 0:T], func=AF.Copy, scale=scale), R=[pb_b], W=[fq[nm][1][c]])
            for s in range(NS):
                pb_ap, pb_b = bank()
                tgroup(wt, wb, 256, 256, s, pb_ap, pb_b)
                ACT.op(lambda s=s, pb_ap=pb_ap: nc.scalar.copy(out=tk["glk"][0][:, s, :], in_=pb_ap[:, 0:256]),
                       R=[pb_b], W=[tk["glk"][1][s]])
            wt, wb = get_slot(l, 4)
            for s in range(NS):
                pb_ap, pb_b = bank()
                tgroup(wt, wb, 0, 512, s, pb_ap, pb_b)
                DVE.op(lambda s=s, pb_ap=pb_ap: nc.vector.tensor_copy(out=tk["gv"][0][:, s, :], in_=pb_ap[:, 0:256]),
                       R=[pb_b], W=[tk["gv"][1][s]])
                ACT.op(lambda s=s, pb_ap=pb_ap: nc.scalar.activation(
                    out=tk["sgr"][0][:, s, :], in_=pb_ap[:, 256:512], func=AF.Silu), R=[pb_b], W=[tk["sgr"][1][s]])
            if DBG_LEVEL < 1.8:
                return
            wt, wb = get_slot(l, 5)
            for c in range(2):
                pg_ap, pg_b = bank()
                fgroup(wt, wb, 256 + c * 128, 128, pg_ap, pg_b)
                pa_ap, pa_b = bank()
                fgroup(wt, wb, c * 128, 128, pa_ap, pa_b)
                sg_t, sg_b = scratch()
                ACT.op(lambda pg_ap=pg_ap, sg_t=sg_t: nc.scalar.activation(out=sg_t[:, 0:T], in_=pg_ap[:, 0:T], func=AF.Sigmoid),
                       R=[pg_b], W=[sg_b])
                ACT.op(lambda c=c: nc.scalar.copy(out=zc[:, c, 0:30], in_=zch[:, l, c, :]), R=[zch_b[l][c]], W=[zc_b[c]])
                DVE.op(lambda c=c, pa_ap=pa_ap, sg_t=sg_t: nc.vector.tensor_tensor(
                    out=zc[:, c, 30:30 + T], in0=pa_ap[:, 0:T], in1=sg_t[:, 0:T], op=ALU.mult),
                    R=[pa_b, sg_b], W=[zc_b[c]])
                ACT.op(lambda c=c: nc.scalar.copy(out=zch[:, l, c, :], in_=zc[:, c, T:T + 30]), R=[zc_b[c]], W=[zch_b[l][c]])
            wt, wb = get_slot(l, 6)
            for c in range(2):
                pb_ap, pb_b = bank()
                fgroup(wt, wb, c * 128, 128, pb_ap, pb_b)
                ACT.op(lambda c=c, pb_ap=pb_ap: nc.scalar.copy(out=bgs[:, c, :], in_=pb_ap[:, 0:T]), R=[pb_b], W=[bgs_b[c]])
                pb_ap, pb_b = bank()
                fgroup(wt, wb, 256 + c * 128, 128, pb_ap, pb_b)
                ACT.op(lambda c=c, pb_ap=pb_ap: nc.scalar.copy(out=cgs[:, c, :], in_=pb_ap[:, 0:T]), R=[pb_b], W=[cgs_b[c]])
            wt, wb = get_slot(l, 7)
            for c in range(2):
                pb_ap, pb_b = bank()
                fgroup(wt, wb, c * 128, 128, pb_ap, pb_b)
                ACT.op(lambda c=c: nc.scalar.copy(out=zs[:, c, 0:2], in_=zsh[:, l, c, :]), R=[zsh_b[l][c]], W=[zs_b[c]])
                DVE.op(lambda c=c, pb_ap=pb_ap: nc.vector.tensor_tensor(
                    out=zs[:, c, 2:2 + T], in0=pb_ap[:, 0:T], in1=cgs[:, c, :], op=ALU.mult),
                    R=[pb_b, cgs_b[c]], W=[zs_b[c]])
                ACT.op(lambda c=c: nc.scalar.copy(out=zsh[:, l, c, :], in_=zs[:, c, T:T + 2]), R=[zs_b[c]], W=[zsh_b[l][c]])

            if DBG_LEVEL < 3:
                return
            for c in range(2):
                y_t, y_b = scratch()
                w0 = pvec[:, pv + 148 + c * 3:pv + 148 + c * 3 + 3]
                DVE.op(lambda c=c, y_t=y_t, w0=w0: nc.vector.tensor_scalar(
                    out=y_t[:, 0:T], in0=zs[:, c, 2:2 + T], scalar1=w0[:, 2:3], scalar2=None, op0=ALU.mult),
                    R=[zs_b[c], cst_b], W=[y_b])
                for j in (1, 0):
                    DVE.op(lambda c=c, y_t=y_t, w0=w0, j=j: nc.vector.scalar_tensor_tensor(
                        out=y_t[:, 0:T], in0=zs[:, c, j:j + T], scalar=w0[:, j:j + 1], in1=y_t[:, 0:T],
                        op0=ALU.mult, op1=ALU.add), R=[zs_b[c], cst_b, y_b], W=[y_b])
                DVE.op(lambda c=c, y_t=y_t: nc.vector.tensor_tensor(
                    out=zT["sc"][0][:, c, :], in0=y_t[:, 0:T], in1=bgs[:, c, :], op=ALU.mult),
                    R=[y_b, bgs_b[c]], W=[zT["sc"][1][c]])

            for c in range(2):
                wt, wb = get_slot(l, 37 + c)
                pb_ap, pb_b = bank()
                for j in range(31):
                    PE.op(lambda j=j, c=c, wt=wt, pb_ap=pb_ap: nc.tensor.matmul(
                        pb_ap[:, 0:T], lhsT=wt[:, j * 128:(j + 1) * 128], rhs=zc[:, c, j:j + T],
                        start=(j == 0), stop=(j == 30)), R=[wb, zc_b[c]], W=[pb_b])
                ACT.op(lambda c=c, pb_ap=pb_ap: nc.scalar.activation(
                    out=uconv[:, c, :], in_=pb_ap[:, 0:T], func=AF.Identity, bias=pvec[:, pv + 142 + c:pv + 143 + c]),
                    R=[pb_b, cst_b], W=[uconv_b[c]])
            usq_t, usq_b = mtile("usq", [128, 2, T])
            ACT.op(lambda: nc.scalar.activation(out=usq_t[:, :, :], in_=uconv[:, :, :], func=AF.Square),
                   R=uconv_b, W=[usq_b])
            psum_ap, psum_b = bank()
            psq_ap, psq_b = bank()
            for c in range(2):
                PE.op(lambda c=c: nc.tensor.matmul(psum_ap[:, 0:T], lhsT=ones, rhs=uconv[:, c, :], start=(c == 0), stop=(c == 1)),
                      R=[cst_b, uconv_b[c]], W=[psum_b])
            for c in range(2):
                PE.op(lambda c=c: nc.tensor.matmul(psq_ap[:, 0:T], lhsT=ones, rhs=usq_t[:, c, :], start=(c == 0), stop=(c == 1)),
                      R=[cst_b, usq_b], W=[psq_b])
            mean_t, mean_b = scratch()
            msq_t, msq_b = scratch()
            rs_t, rs_b = scratch()
            ACT.op(lambda: nc.scalar.activation(out=mean_t[:, 0:T], in_=psum_ap[:, 0:T], func=AF.Copy, scale=1.0 / MIX),
                   R=[psum_b], W=[mean_b])
            ACT.op(lambda: nc.scalar.activation(out=msq_t[:, 0:T], in_=psum_ap[:, 0:T], func=AF.Square, scale=1.0 / MIX),
                   R=[psum_b], W=[msq_b])
            DVE.op(lambda: nc.vector.scalar_tensor_tensor(
                out=rs_t[:, 0:T], in0=psq_ap[:, 0:T], scalar=1.0 / MIX, in1=msq_t[:, 0:T], op0=ALU.mult, op1=ALU.subtract),
                R=[psq_b, msq_b], W=[rs_b])
            ACT.op(lambda: nc.scalar.activation(out=rs_t[:, 0:T], in_=rs_t[:, 0:T], func=AF.Ln, bias=eps_ap),
                   R=[rs_b, cst_b], W=[rs_b])
            ACT.op(lambda: nc.scalar.activation(out=rs_t[:, 0:T], in_=rs_t[:, 0:T], func=AF.Exp, scale=-0.5),
                   R=[rs_b], W=[rs_b])
            for c in range(2):
                d_t, d_b = scratch()
                DVE.op(lambda c=c, d_t=d_t: nc.vector.tensor_tensor(
                    out=d_t[:, 0:T], in0=uconv[:, c, :], in1=mean_t[:, 0:T], op=ALU.subtract),
                    R=[uconv_b[c], mean_b], W=[d_b])
                DVE.op(lambda c=c, d_t=d_t: nc.vector.tensor_tensor(
                    out=d_t[:, 0:T], in0=d_t[:, 0:T], in1=rs_t[:, 0:T], op=ALU.mult), R=[d_b, rs_b], W=[d_b])
                ACT.op(lambda c=c, d_t=d_t: nc.scalar.activation(
                    out=zT["conf"][0][:, c, :], in_=d_t[:, 0:T], func=AF.Silu,
                    scale=pvec[:, pv + 144 + c:pv + 145 + c], bias=pvec[:, pv + 146 + c:pv + 147 + c]),
                    R=[d_b, cst_b], W=[zT["conf"][1][c]])

            if DBG_LEVEL >= 4:
                mixers(l, i)
            if first:
                for nm in ("ml", "gla", "conf", "sc"):
                    dbg_dump("z_" + nm, zT[nm][0][:, :, :], zT[nm][1])
            if DBG_LEVEL < 5:
                return
            merge_and_out(l, i, pv, first)

        def mixers(l, i):
            pv = l * PVL
            br = l * BRL
            def mix_gen(s):
                tok = slice(s * 128, (s + 1) * 128)
                e_t, e_b = small()
                ACT.op(lambda e_t=e_t: nc.scalar.activation(out=e_t[:, 0:4], in_=iff[:, s, 4:8], func=AF.Exp, scale=-1.0),
                       R=[iff_b[s]], W=[e_b])
                ACT.op(lambda e_t=e_t: nc.scalar.activation(out=e_t[:, 4:8], in_=e_t[:, 0:4], func=AF.Ln, bias=one_ap),
                       R=[e_b, cst_b], W=[e_b])
                cs_ap, cs_b = bank()
                PE.op(lambda e_t=e_t: nc.tensor.matmul(cs_ap[:, 0:4], lhsT=Umat, rhs=e_t[:, 4:8], start=True, stop=True),
                      R=[cst_b, e_b], W=[cs_b])
                PE.op(lambda e_t=e_t: nc.tensor.matmul(cs_ap[:, 8:12], lhsT=ones, rhs=e_t[:, 4:8], start=True, stop=True),
                      R=[cst_b, e_b], W=[cs_b])
                la_ap, la_b = bank()
                PE.op(lambda: nc.tensor.matmul(la_ap[:, 0:256], lhsT=gaT[:, tok], rhs=w2[:, l * 256:(l + 1) * 256],
                                               start=True, stop=True), R=[gaT_b, cst_b], W=[la_b])
                w_t, w_b = small()
                DVE.op(lambda w_t=w_t: nc.vector.tensor_tensor(out=w_t[:, 0:4], in0=cs_ap[:, 0:4], in1=iff[:, s, 0:4], op=ALU.add),
                       R=[cs_b, iff_b[s]], W=[w_b])
                ACT.op(lambda w_t=w_t: nc.scalar.activation(out=w_t[:, 4:8], in_=w_t[:, 0:4], func=AF.Exp), R=[w_b], W=[w_b])
                ACT.op(lambda w_t=w_t: nc.scalar.activation(out=w_t[:, 8:12], in_=cs_ap[:, 0:4], func=AF.Exp, scale=-1.0),
                       R=[cs_b], W=[w_b])
                ebl_t, ebl_b = small()
                for hh in range(2):
                    ACT.op(lambda hh=hh, ebl_t=ebl_t: nc.scalar.activation(
                        out=ebl_t[hh * 64:(hh + 1) * 64, 0:2],
                        in_=cs_ap[hh * 64:(hh + 1) * 64, 8:12].rearrange("p (a b) -> p a b", b=2)[:, :, hh],
                        func=AF.Exp, scale=-1.0), R=[cs_b], W=[ebl_b])
                xla_t, xla_b = mtile("xla", [128, 256])
                DVE.op(lambda xla_t=xla_t: nc.vector.tensor_tensor(
                    out=xla_t[:, :], in0=la_ap[:, 0:256], in1=brow[:, br + 520:br + 776], op=ALU.add),
                    R=[la_b, cst_b], W=[xla_b])
                ACT.op(lambda xla_t=xla_t: nc.scalar.activation(out=xla_t[:, :], in_=xla_t[:, :], func=AF.Exp, scale=-1.0),
                       R=[xla_b], W=[xla_b])
                ACT.op(lambda xla_t=xla_t: nc.scalar.activation(out=xla_t[:, :], in_=xla_t[:, :], func=AF.Ln, bias=one_ap),
                       R=[xla_b, cst_b], W=[xla_b])
                vx_t, vx_b = mtile("vext", [128, 4, 72], BF16)
                DVE.op(lambda vx_t=vx_t, w_t=w_t: nc.vector.tensor_tensor(
                    out=vx_t[:, :, 0:64], in0=tk["mlv"][0][:, s, :].rearrange("p (h e) -> p h e", e=64),
                    in1=w_t[:, 4:8].unsqueeze(2).to_broadcast([128, 4, 64]), op=ALU.mult),
                    R=[tk["mlv"][1][s], w_b], W=[vx_b])
                DVE.op(lambda vx_t=vx_t, w_t=w_t: nc.vector.tensor_copy(out=vx_t[:, :, 64:65], in_=w_t[:, 4:8].unsqueeze(2)),
                       R=[w_b], W=[vx_b])
                nb_ap, nb_b = bank()
                for p in range(2):
                    PE.op(lambda p=p, xla_t=xla_t: nc.tensor.matmul(
                        nb_ap[:, p * 128:(p + 1) * 128], lhsT=xla_t[:, p * 128:(p + 1) * 128], rhs=U16, start=True, stop=True),
                        R=[xla_b, cst_b], W=[nb_b])
                PE.op(lambda xla_t=xla_t: nc.tensor.matmul(nb_ap[:, 256:512], lhsT=SU16, rhs=xla_t[:, :], start=True, stop=True),
                      R=[xla_b, cst_b], W=[nb_b])
                yield 'a'
                st_ap, st_b = bank()
                for h in range(4):
                    p, hh = h // 2, h % 2
                    rows = slice(hh * 64, (hh + 1) * 64)
                    PE.op(lambda h=h, p=p, rows=rows: nc.tensor.matmul(
                        st_ap[:, h * 128:(h + 1) * 128], lhsT=fq["mlk"][0][rows, p, tok], rhs=fq["mlq"][0][rows, p, tok],
                        start=True, stop=True), R=[fq["mlk"][1][p], fq["mlq"][1][p]], W=[st_b], drain=True)
                pt_t, pt_b = mtile("PT", [128, 4, 128], BF16)
                DVE.op(lambda pt_t=pt_t: nc.vector.tensor_tensor(
                    out=pt_t[:, :, :], in0=st_ap.rearrange("p (h t) -> p h t", t=128), in1=mask4, op=ALU.mult),
                    R=[st_b, cst_b], W=[pt_b])
                yield 'a'
                eq_t, eq_b = mtile("eq", [128, 2, 128])
                ek_t, ek_b = mtile("ek", [128, 2, 128])
                nb3 = nb_ap[:, 0:256].rearrange("p (a t) -> p a t", t=128)
                ACT.op(lambda eq_t=eq_t: nc.scalar.activation(out=eq_t[:, :, :], in_=nb3, func=AF.Exp, scale=-1.0),
                       R=[nb_b], W=[eq_b])
                ACT.op(lambda ek_t=ek_t: nc.scalar.activation(out=ek_t[:, :, :], in_=nb3, func=AF.Exp), R=[nb_b], W=[ek_b])
                ebg_t, ebg_b = small()
                ACT.op(lambda ebg_t=ebg_t: nc.scalar.activation(out=ebg_t[:, 0:2], in_=nb3[:, :, 127], func=AF.Exp, scale=-1.0),
                       R=[nb_b], W=[ebg_b])
                er_t, er_b = mtile("erem", [128, 256])
                ACT.op(lambda er_t=er_t: nc.scalar.activation(out=er_t[:, :], in_=nb_ap[:, 256:512], func=AF.Exp, scale=-1.0),
                       R=[nb_b], W=[er_b])
                qd_t, qd_b = mtile("qd", [128, 2, 128], BF16)
                ki_t, ki_b = mtile("ki", [128, 2, 128], BF16)
                kd_t, kd_b = mtile("kd", [128, 256], BF16)
                DVE.op(lambda qd_t=qd_t, eq_t=eq_t: nc.vector.tensor_tensor(
                    out=qd_t[:, :, :], in0=fq["gq"][0][:, :, tok], in1=eq_t[:, :, :], op=ALU.mult),
                    R=fq["gq"][1] + [eq_b], W=[qd_b])
                DVE.op(lambda ki_t=ki_t, ek_t=ek_t: nc.vector.tensor_tensor(
                    out=ki_t[:, :, :], in0=fq["gk"][0][:, :, tok], in1=ek_t[:, :, :], op=ALU.mult),
                    R=fq["gk"][1] + [ek_b], W=[ki_b])
                DVE.op(lambda kd_t=kd_t, er_t=er_t: nc.vector.tensor_tensor(
                    out=kd_t[:, :], in0=tk["glk"][0][:, s, :], in1=er_t[:, :], op=ALU.mult),
                    R=[tk["glk"][1][s], er_b], W=[kd_b])
                yield 'endA'
                mo_ap, mo_b = bank()
                mo3 = mo_ap[:, 0:288].rearrange("p (h e) -> p h e", e=72)
                for h in range(4):
                    p, hh = h // 2, h % 2
                    rows = slice(hh * 64, (hh + 1) * 64)
                    PE.op(lambda h=h, pt_t=pt_t, vx_t=vx_t: nc.tensor.matmul(
                        mo3[:, h, 0:65], lhsT=pt_t[:, h, :], rhs=vx_t[:, h, 0:65], start=True, stop=False),
                        R=[pt_b, vx_b], W=[mo_b], drain=True)
                    PE.op(lambda h=h, p=p, rows=rows: nc.tensor.matmul(
                        mo3[:, h, 0:65], lhsT=fq["mlq"][0][rows, p, tok], rhs=Cbf[rows, l, p, 0:65], start=False, stop=True),
                        R=[fq["mlq"][1][p], Cbf_b[l][p]], W=[mo_b], drain=True)
                at_ap, at_b = bank()
                for h in range(4):
                    p, hh = h // 2, h % 2
                    rows = slice(hh * 64, (hh + 1) * 64)
                    PE.op(lambda h=h, p=p, rows=rows, ki_t=ki_t, qd_t=qd_t: nc.tensor.matmul(
                        at_ap[:, h * 128:(h + 1) * 128], lhsT=ki_t[rows, p, :], rhs=qd_t[rows, p, :], start=True, stop=True),
                        R=[ki_b, qd_b], W=[at_b], drain=True)
                at_t, at_tb = mtile("AT", [128, 4, 128], BF16)
                DVE.op(lambda at_t=at_t: nc.vector.tensor_tensor(
                    out=at_t[:, :, :], in0=at_ap.rearrange("p (h t) -> p h t", t=128), in1=mask4, op=ALU.mult),
                    R=[at_b, cst_b], W=[at_tb])
                cu_ap, cu_b = bank()
                cu3 = cu_ap[:, 0:144].rearrange("p (a e) -> p a e", e=72)
                for h in range(4):
                    p, hh = h // 2, h % 2
                    rows = slice(hh * 64, (hh + 1) * 64)
                    PE.op(lambda h=h, p=p, rows=rows, vx_t=vx_t: nc.tensor.matmul(
                        cu3[rows, p, 0:65], lhsT=tk["mlk"][0][:, s, h * 64:(h + 1) * 64], rhs=vx_t[:, h, 0:65],
                        start=True, stop=True), R=[tk["mlk"][1][s], vx_b], W=[cu_b], drain=True)
                yield 'endB'
                r_t, r_b = small()
                DVE.op(lambda r_t=r_t, w_t=w_t: nc.vector.tensor_tensor(
                    out=r_t[:, 0:4], in0=mo3[:, :, 64], in1=w_t[:, 8:12], op=ALU.mult), R=[mo_b, w_b], W=[r_b])
                DVE.op(lambda r_t=r_t: nc.vector.tensor_scalar(
                    out=r_t[:, 12:16], in0=r_t[:, 0:4], scalar1=-1.0, scalar2=1.0, op0=ALU.mult, op1=ALU.max), R=[r_b], W=[r_b])
                DVE.op(lambda r_t=r_t: nc.vector.tensor_tensor(
                    out=r_t[:, 0:4], in0=r_t[:, 0:4], in1=r_t[:, 12:16], op=ALU.max), R=[r_b], W=[r_b])
                DVE.op(lambda r_t=r_t: nc.vector.reciprocal(out=r_t[:, 4:8], in_=r_t[:, 0:4]), R=[r_b], W=[r_b])
                DVE.op(lambda r_t=r_t, w_t=w_t: nc.vector.tensor_tensor(
                    out=r_t[:, 8:12], in0=r_t[:, 4:8], in1=w_t[:, 8:12], op=ALU.mult), R=[r_b, w_b], W=[r_b])
                hm_t, hm_b = mtile("hm", [128, 4, 64])
                DVE.op(lambda hm_t=hm_t, r_t=r_t: nc.vector.tensor_tensor(
                    out=hm_t[:, :, :], in0=mo3[:, :, 0:64], in1=r_t[:, 8:12].unsqueeze(2).to_broadcast([128, 4, 64]), op=ALU.mult),
                    R=[mo_b, r_b], W=[hm_b])
                hsq_t, hsq_b = mtile("hsq", [128, 4, 64])
                ACT.op(lambda hm_t=hm_t, hsq_t=hsq_t: nc.scalar.activation(out=hsq_t[:, :, :], in_=hm_t[:, :, :], func=AF.Square),
                       R=[hm_b], W=[hsq_b])
                n_t, n_b = small()
                DVE.op(lambda n_t=n_t, hsq_t=hsq_t: nc.vector.tensor_reduce(out=n_t[:, 0:4], in_=hsq_t[:, :, :], axis=AX.X, op=ALU.add),
                       R=[hsq_b], W=[n_b])
                ACT.op(lambda n_t=n_t: nc.scalar.activation(out=n_t[:, 4:8], in_=n_t[:, 0:4], func=AF.Sqrt, scale=1.0 / 64, bias=eps_ap),
                       R=[n_b, cst_b], W=[n_b])
                DVE.op(lambda n_t=n_t: nc.vector.reciprocal(out=n_t[:, 8:12], in_=n_t[:, 4:8]), R=[n_b], W=[n_b])
                og_t, og_b = mtile("og", [128, 256])
                DVE.op(lambda og_t=og_t: nc.vector.tensor_tensor(
                    out=og_t[:, :], in0=tk["sigo"][0][:, s, :], in1=brow[:, br + 8:br + 264], op=ALU.mult),
                    R=[tk["sigo"][1][s], cst_b], W=[og_b])
                DVE.op(lambda hm_t=hm_t, n_t=n_t: nc.vector.tensor_tensor(
                    out=hm_t[:, :, :], in0=hm_t[:, :, :], in1=n_t[:, 8:12].unsqueeze(2).to_broadcast([128, 4, 64]), op=ALU.mult),
                    R=[hm_b, n_b], W=[hm_b])
                zt_t, zt_b = mtile("ztok", [128, 256])
                DVE.op(lambda hm_t=hm_t, og_t=og_t, zt_t=zt_t: nc.vector.tensor_tensor(
                    out=zt_t[:, :], in0=hm_t[:, :, :].rearrange("p h e -> p (h e)"), in1=og_t[:, :], op=ALU.mult),
                    R=[hm_b, og_b], W=[zt_b])
                tp_ap, tp_b = bank()
                for c in range(2):
                    PE.op(lambda c=c, zt_t=zt_t: nc.tensor.transpose(tp_ap[:, c * 128:(c + 1) * 128], zt_t[:, c * 128:(c + 1) * 128], ident),
                          R=[zt_b, cst_b], W=[tp_b])
                ACT.op(lambda: nc.scalar.copy(out=zT["ml"][0][:, :, tok], in_=tp_ap[:, 0:256].rearrange("p (c t) -> p c t", t=128)),
                       R=[tp_b], W=[zT["ml"][1][s]])
                for p in range(2):
                    ct_t, ct_b = mtile("ctmp", [128, 72])
                    DVE.op(lambda p=p, ct_t=ct_t: nc.vector.tensor_tensor(
                        out=ct_t[:, 0:65], in0=cu3[:, p, 0:65], in1=Cst[:, l, p, 0:65], op=ALU.add),
                        R=[cu_b, Cst_b[l][p]], W=[ct_b])
                    DVE.op(lambda p=p, ct_t=ct_t, ebl_t=ebl_t: nc.vector.tensor_scalar(
                        out=Cst[:, l, p, 0:65], in0=ct_t[:, 0:65], scalar1=ebl_t[:, p:p + 1], scalar2=None, op0=ALU.mult),
                        R=[ct_b, ebl_b], W=[Cst_b[l][p]])
                    ACT.op(lambda p=p, ct_t=ct_t, ebl_t=ebl_t: nc.scalar.activation(
                        out=Cbf[:, l, p, 0:65], in_=ct_t[:, 0:65], func=AF.Copy, scale=ebl_t[:, p:p + 1]),
                        R=[ct_b, ebl_b], W=[Cbf_b[l][p]])
                yield 'endC'
                go_ap, go_b = bank()
                for h in range(4):
                    p, hh = h // 2, h % 2
                    rows = slice(hh * 64, (hh + 1) * 64)
                    PE.op(lambda h=h, at_t=at_t: nc.tensor.matmul(
                        go_ap[:, h * 64:(h + 1) * 64], lhsT=at_t[:, h, :], rhs=tk["gv"][0][:, s, h * 64:(h + 1) * 64],
                        start=True, stop=False), R=[at_tb, tk["gv"][1][s]], W=[go_b], drain=True)
                    PE.op(lambda h=h, p=p, rows=rows, qd_t=qd_t: nc.tensor.matmul(
                        go_ap[:, h * 64:(h + 1) * 64], lhsT=qd_t[rows, p, :], rhs=Sbf[rows, l, p, :], start=False, stop=True),
                        R=[qd_b, Sbf_b[l][p]], W=[go_b], drain=True)
                su_ap, su_b = bank()
                su3 = su_ap[:, 0:128].rearrange("p (a e) -> p a e", e=64)
                for h in range(4):
                    p, hh = h // 2, h % 2
                    rows = slice(hh * 64, (hh + 1) * 64)
                    PE.op(lambda h=h, p=p, rows=rows, kd_t=kd_t: nc.tensor.matmul(
                        su3[rows, p, :], lhsT=kd_t[:, h * 64:(h + 1) * 64], rhs=tk["gv"][0][:, s, h * 64:(h + 1) * 64],
                        start=True, stop=True), R=[kd_b, tk["gv"][1][s]], W=[su_b], drain=True)
                gm_t, gm_b = mtile("gm", [128, 4, 64])
                ACT.op(lambda gm_t=gm_t: nc.scalar.copy(out=gm_t[:, :, :], in_=go_ap[:, 0:256].rearrange("p (h e) -> p h e", e=64)),
                       R=[go_b], W=[gm_b])
                gsq_t, gsq_b = mtile("gsq", [128, 4, 64])
                ACT.op(lambda gsq_t=gsq_t: nc.scalar.activation(
                    out=gsq_t[:, :, :], in_=go_ap[:, 0:256].rearrange("p (h e) -> p h e", e=64), func=AF.Square),
                    R=[go_b], W=[gsq_b])
                gn_t, gn_b = small()
                DVE.op(lambda gn_t=gn_t, gsq_t=gsq_t: nc.vector.tensor_reduce(out=gn_t[:, 0:4], in_=gsq_t[:, :, :], axis=AX.X, op=ALU.add),
                       R=[gsq_b], W=[gn_b])
                ACT.op(lambda gn_t=gn_t: nc.scalar.activation(out=gn_t[:, 4:8], in_=gn_t[:, 0:4], func=AF.Sqrt, scale=1.0 / 64, bias=eps_ap),
                       R=[gn_b, cst_b], W=[gn_b])
                DVE.op(lambda gn_t=gn_t: nc.vector.reciprocal(out=gn_t[:, 8:12], in_=gn_t[:, 4:8]), R=[gn_b], W=[gn_b])
                rg_t, rg_b = mtile("rg", [128, 256])
                DVE.op(lambda rg_t=rg_t: nc.vector.tensor_tensor(
                    out=rg_t[:, :], in0=tk["sgr"][0][:, s, :], in1=brow[:, br + 264:br + 520], op=ALU.mult),
                    R=[tk["sgr"][1][s], cst_b], W=[rg_b])
                DVE.op(lambda gm_t=gm_t, gn_t=gn_t: nc.vector.tensor_tensor(
                    out=gm_t[:, :, :], in0=gm_t[:, :, :], in1=gn_t[:, 8:12].unsqueeze(2).to_broadcast([128, 4, 64]), op=ALU.mult),
                    R=[gm_b, gn_b], W=[gm_b])
                zg_t, zg_b = mtile("zgtok", [128, 256])
                DVE.op(lambda gm_t=gm_t, rg_t=rg_t, zg_t=zg_t: nc.vector.tensor_tensor(
                    out=zg_t[:, :], in0=gm_t[:, :, :].rearrange("p h e -> p (h e)"), in1=rg_t[:, :], op=ALU.mult),
                    R=[gm_b, rg_b], W=[zg_b])
                tg_ap, tg_b = bank()
                for c in range(2):
                    PE.op(lambda c=c, zg_t=zg_t: nc.tensor.transpose(tg_ap[:, c * 128:(c + 1) * 128], zg_t[:, c * 128:(c + 1) * 128], ident),
                          R=[zg_b, cst_b], W=[tg_b])
                ACT.op(lambda: nc.scalar.copy(out=zT["gla"][0][:, :, tok], in_=tg_ap[:, 0:256].rearrange("p (c t) -> p c t", t=128)),
                       R=[tg_b], W=[zT["gla"][1][s]])
                for p in range(2):
                    DVE.op(lambda p=p, ebg_t=ebg_t: nc.vector.scalar_tensor_tensor(
                        out=Sst[:, l, p, :], in0=Sst[:, l, p, :], scalar=ebg_t[:, p:p + 1], in1=su3[:, p, :],
                        op0=ALU.mult, op1=ALU.add), R=[Sst_b[l][p], ebg_b, su_b], W=[Sst_b[l][p]])
                    ACT.op(lambda p=p: nc.scalar.copy(out=Sbf[:, l, p, :], in_=Sst[:, l, p, :]), R=[Sst_b[l][p]], W=[Sbf_b[l][p]])

            def run_until(g, tag):
                for t_ in g:
                    if t_ == tag:
                        return True
                return False

            if NS == 2:
                g0, g1 = mix_gen(0), mix_gen(1)
                a0 = a1 = True
                while a0 or a1:
                    if a0:
                        a0 = next(g0) != 'endA'
                    if a1:
                        a1 = next(g1) != 'endA'
                run_until(g0, 'endB')
                run_until(g0, 'endC')
                run_until(g1, 'endB')
                run_until(g0, None)
                run_until(g1, 'endC')
                run_until(g1, None)
            else:
                for s_ in range(NS):
                    run_until(mix_gen(s_), None)


        def merge_and_out(l, i, pv, first):
            wA, wAb = get_slot(l, 16, hold=0)
            wB, wBb = get_slot(l, 17, hold=1)
            wA3 = wA[:, :].rearrange("p (m c) -> p m c", c=1024)
            wB3 = wB[:, :].rearrange("p (m c) -> p m c", c=1024)
            branches = (("ml", wA3, 0, wAb), ("gla", wA3, 2, wAb), ("conf", wB3, 0, wBb), ("sc", wB3, 2, wBb))
            for j in range(8):
                wt, wb = get_slot(l, 8 + j)
                g_t, g_b = gates[j % 2], gates_b[j % 2]
                for b in range(4):
                    pb_ap, pb_b = bank()
                    fgroup(wt, wb, b * 128, 128, pb_ap, pb_b)
                    ACT.op(lambda b=b, pb_ap=pb_ap, g_t=g_t: nc.scalar.activation(
                        out=g_t[:, b, :], in_=pb_ap[:, 0:T], func=AF.Sigmoid, bias=pvec[:, pv + 162 + b * 8 + j:pv + 163 + b * 8 + j]),
                        R=[pb_b, cst_b], W=[g_b])
                acc_t, acc_b = scratch()
                for b, (nm, w3, m0, wbb) in enumerate(branches):
                    pb_ap, pb_b = bank()
                    for kc in range(2):
                        PE.op(lambda kc=kc, nm=nm, w3=w3, m0=m0, pb_ap=pb_ap: nc.tensor.matmul(
                            pb_ap[:, 0:T], lhsT=w3[:, m0 + kc, j * 128:(j + 1) * 128], rhs=zT[nm][0][:, kc, :],
                            start=(kc == 0), stop=(kc == 1)), R=[wbb] + zT[nm][1], W=[pb_b])
                    if b == 0:
                        DVE.op(lambda pb_ap=pb_ap, g_t=g_t, acc_t=acc_t: nc.vector.tensor_tensor(
                            out=acc_t[:, 0:T], in0=pb_ap[:, 0:T], in1=g_t[:, 0, :], op=ALU.mult), R=[pb_b, g_b], W=[acc_b])
                    else:
                        tm_t, tm_b = scratch()
                        if nm == "conf":
                            DVE.op(lambda pb_ap=pb_ap, g_t=g_t, tm_t=tm_t, b=b: nc.vector.scalar_tensor_tensor(
                                out=tm_t[:, 0:T], in0=pb_ap[:, 0:T], scalar=pvec[:, pv + 154 + j:pv + 155 + j], in1=g_t[:, b, :],
                                op0=ALU.add, op1=ALU.mult), R=[pb_b, g_b, cst_b], W=[tm_b])
                        else:
                            DVE.op(lambda pb_ap=pb_ap, g_t=g_t, tm_t=tm_t, b=b: nc.vector.tensor_tensor(
                                out=tm_t[:, 0:T], in0=pb_ap[:, 0:T], in1=g_t[:, b, :], op=ALU.mult), R=[pb_b, g_b], W=[tm_b])
                        if b < 3:
                            POOL.op(lambda tm_t=tm_t, acc_t=acc_t: nc.gpsimd.tensor_tensor(
                                out=acc_t[:, 0:T], in0=acc_t[:, 0:T], in1=tm_t[:, 0:T], op=ALU.add), R=[tm_b, acc_b], W=[acc_b])
                        else:
                            POOL.op(lambda tm_t=tm_t, acc_t=acc_t: nc.gpsimd.tensor_tensor(
                                out=mergedT[:, j, :], in0=acc_t[:, 0:T], in1=tm_t[:, 0:T], op=ALU.add),
                                R=[tm_b, acc_b], W=[merged_b[j]])
            if first:
                dbg_dump("merged", mergedT[:, :, :], merged_b)
                dbg_dump("grow", Grow[:, 0:2, :], Grow_b[0:2])
            if DBG_LEVEL < 6:
                return
            w0, w0b = get_slot(l, 18)
            w1, w1b = get_slot(l, 19)
            for s in range(NS):
                pbs = []
                for n, (wt, wb) in enumerate(((w0, w0b), (w1, w1b))):
                    pb_ap, pb_b = bank()
                    w3 = wt[:, :].rearrange("p (kc c) -> p kc c", c=512)
                    for kc in range(8):
                        PE.op(lambda kc=kc, w3=w3, pb_ap=pb_ap: nc.tensor.matmul(
                            pb_ap[:, :], lhsT=mergedT[:, kc, s * 128:(s + 1) * 128], rhs=w3[:, kc, :],
                            start=(kc == 0), stop=(kc == 7)), R=[wb, merged_b[kc]], W=[pb_b])
                    pbs.append((pb_ap, pb_b))
                postnorm(l, 0, s, pbs)

        def postnorm(l, which, s, pbs):
            st_t, st_b = small()
            ys = []
            for n, (pb_ap, pb_b) in enumerate(pbs):
                j_t, j_b = scratch()
                ACT.op(lambda n=n, pb_ap=pb_ap, j_t=j_t, st_t=st_t: nc.scalar.activation(
                    out=j_t[:, 0:512], in_=pb_ap[:, :], func=AF.Square, accum_out=st_t[:, n:n + 1]), R=[pb_b], W=[j_b, st_b])
                y_t, y_b = scratch()
                DVE.op(lambda n=n, pb_ap=pb_ap, y_t=y_t: nc.vector.tensor_tensor(
                    out=y_t[:, 0:512], in0=pb_ap[:, :], in1=Grow[:, l * 2 + which, n * 512:(n + 1) * 512], op=ALU.mult),
                    R=[pb_b, Grow_b[l * 2 + which]], W=[y_b])
                ys.append((y_t, y_b))
            DVE.op(lambda st_t=st_t: nc.vector.tensor_tensor(out=st_t[:, 2:3], in0=st_t[:, 0:1], in1=st_t[:, 1:2], op=ALU.add),
                   R=[st_b], W=[st_b])
            ACT.op(lambda st_t=st_t: nc.scalar.activation(out=st_t[:, 3:4], in_=st_t[:, 2:3], func=AF.Sqrt, scale=1.0 / D, bias=eps_ap),
                   R=[st_b, cst_b], W=[st_b])
            DVE.op(lambda st_t=st_t: nc.vector.reciprocal(out=st_t[:, 4:5], in_=st_t[:, 3:4]), R=[st_b], W=[st_b])
            for n, (pb_ap, pb_b) in enumerate(pbs):
                y_t, y_b = ys[n]
                DVE.op(lambda n=n, y_t=y_t, st_t=st_t: nc.vector.scalar_tensor_tensor(
                    out=xt[:, s, n * 512:(n + 1) * 512], in0=y_t[:, 0:512], scalar=st_t[:, 4:5], in1=xt[:, s, n * 512:(n + 1) * 512],
                    op0=ALU.mult, op1=ALU.add), R=[y_b, st_b, xt_b[s]], W=[xt_b[s]])

        NU = 4
        u_tiles = [sb("ubuf%d" % k, [128, 2 + T]) for k in range(NU)]
        um_b = [Buf() for _ in range(NU)]
        uh_b = [Buf() for _ in range(NU)]
        u_ctr = [0]

        def ffn(l, i):
            pv = l * PVL
            prenorm(l, i, 1)
            for g in range(11):
                wt, wb = get_slot(l, 20 + g)
                for jj in range(2):
                    j = g * 2 + jj
                    ys = []
                    for part in range(2):
                        ch = part * 22 + j
                        pb_ap, pb_b = bank()
                        fgroup(wt, wb, part * 256 + jj * 128, 128, pb_ap, pb_b)
                        k_ = u_ctr[0] % NU
                        u_ctr[0] += 1
                        u_t, u_b, u_hb = u_tiles[k_], um_b[k_], uh_b[k_]
                        POOL.op(lambda u_t=u_t, ch=ch: nc.gpsimd.tensor_copy(out=u_t[:, 0:2], in_=halo[:, l, ch, :]), R=[halo_b[l][ch]], W=[u_hb])
                        ACT.op(lambda u_t=u_t, pb_ap=pb_ap: nc.scalar.copy(out=u_t[:, 2:2 + T], in_=pb_ap[:, 0:T]), R=[pb_b], W=[u_b])
                        POOL.op(lambda u_t=u_t, ch=ch: nc.gpsimd.tensor_copy(out=halo[:, l, ch, :], in_=u_t[:, T:T + 2]), R=[u_b], W=[halo_b[l][ch]])
                        wc = pvec[:, pv + 194 + ch * 3:pv + 194 + ch * 3 + 3]
                        y_t, y_b = scratch()
                        ACT.op(lambda pb_ap=pb_ap, y_t=y_t, wc=wc: nc.scalar.activation(
                            out=y_t[:, 0:T], in_=pb_ap[:, 0:T], func=AF.Identity, scale=wc[:, 2:3]),
                            R=[pb_b, cst_b], W=[y_b])
                        for q in (1, 0):
                            DVE.op(lambda u_t=u_t, y_t=y_t, wc=wc, q=q: nc.vector.scalar_tensor_tensor(
                                out=y_t[:, 0:T], in0=u_t[:, q:q + T], scalar=wc[:, q:q + 1], in1=y_t[:, 0:T],
                                op0=ALU.mult, op1=ALU.add), R=[u_b, u_hb, cst_b, y_b], W=[y_b])
                        ys.append((y_t, y_b))
                    (ya_t, ya_b), (yv_t, yv_b) = ys
                    ACT.op(lambda ya_t=ya_t: nc.scalar.activation(out=ya_t[:, 0:T], in_=ya_t[:, 0:T], func=AF.Silu), R=[ya_b], W=[ya_b])
                    DVE.op(lambda ya_t=ya_t, yv_t=yv_t, j=j: nc.vector.tensor_tensor(
                        out=gff[:, j, :], in0=ya_t[:, 0:T], in1=yv_t[:, 0:T], op=ALU.mult), R=[ya_b, yv_b], W=[gff_b[j]])
            accs = {}
            for s in range(NS):
                for n in range(2):
                    accs[(s, n)] = bank()
            for q in range(6):
                wt, wb = get_slot(l, 31 + q)
                w3 = wt[:, :].rearrange("p (m c) -> p m c", c=1024)
                nk = 4 if q < 5 else 2
                for s in range(NS):
                    for n in range(2):
                        pb_ap, pb_b = accs[(s, n)]
                        for m in range(nk):
                            j = q * 4 + m
                            PE.op(lambda m=m, j=j, w3=w3, pb_ap=pb_ap, s=s, n=n: nc.tensor.matmul(
                                pb_ap[:, :], lhsT=gff[:, j, s * 128:(s + 1) * 128], rhs=w3[:, m, n * 512:(n + 1) * 512],
                                start=(j == 0), stop=(j == 21)), R=[wb, gff_b[j]], W=[pb_b])
            for s in range(NS):
                postnorm(l, 1, s, [accs[(s, 0)], accs[(s, 1)]])

        for i in range(nseq):
            seq_init(i)
            for tl in range(ntiles):
                src = x_d[i, tl * T:(tl + 1) * T, :].rearrange("(s p) d -> p s d", p=128)
                POOL.dma(xt[:, :, :], src, s_xin, W=xt_b)
                for l in range(depth):
                    if DBG_LEVEL >= 1:
                        token_mixer(l, i)
                    if DBG_LEVEL >= 7:
                        ffn(l, i)
                dst = out_d[i, tl * T:(tl + 1) * T, :].rearrange("(s p) d -> p s d", p=128)
                POOL.dma(dst, xt[:, :, :], s_xout, R=xt_b)
        allsems = [PE.sem, ACT.sem, DVE.sem, POOL.sem, SP.sem, s_xin, s_xout, s_c] + slot_s + hold_s + list(prep_sems.values()) + list(dbg_sems.values()) + diag_sems
        POOL.wait_all(allsems)
        SP.wait_all([s_xout])
    return nc


def host_pack(inp, core):
    f = np.float32
    b0 = core * 2
    cT = np.ascontiguousarray(inp["c"][b0:b0 + 2].reshape(2, 8, 128).transpose(2, 1, 0).reshape(128, 16)).astype(f)
    pvec = np.zeros((128, DEPTH * PVL), f)
    brow = np.zeros((DEPTH * BRL,), f)
    w2 = np.zeros((16, DEPTH * 256), f)

    def fm(v, nch):
        return np.asarray(v, f).reshape(nch, 128).T

    for l in range(DEPTH):
        o = l * PVL
        pvec[:, o + 0:o + 8] = fm(inp["tm_pre_g"][l], 8)
        pvec[:, o + 8:o + 16] = fm(inp["tm_post_g"][l], 8)
        pvec[:, o + 16:o + 24] = fm(inp["cm_pre_g"][l], 8)
        pvec[:, o + 24:o + 32] = fm(inp["cm_post_g"][l], 8)
        pvec[:, o + 32:o + 80] = fm(inp["ada_b"][l], 48)
        cw = np.asarray(inp["conf_dw_w"][l], f)
        for c in range(2):
            pvec[:, o + 80 + c * 31:o + 80 + (c + 1) * 31] = cw[:, c * 128:(c + 1) * 128].T
        pvec[:, o + 142:o + 144] = fm(inp["conf_dw_b"][l], 2)
        pvec[:, o + 144:o + 146] = fm(inp["conf_ln_g"][l], 2)
        pvec[:, o + 146:o + 148] = fm(inp["conf_ln_b"][l], 2)
        sw = np.asarray(inp["sc_dw_w"][l], f)
        for c in range(2):
            pvec[:, o + 148 + c * 3:o + 148 + (c + 1) * 3] = sw[:, c * 128:(c + 1) * 128].T
        pvec[:, o + 154:o + 162] = fm(inp["conf_out_b"][l], 8)
        pvec[:, o + 162:o + 194] = fm(inp["merge_gate_b"][l], 32)
        fw = np.asarray(inp["ffn_dw_w"][l], f)
        for ch in range(44):
            pvec[:, o + 194 + ch * 3:o + 194 + (ch + 1) * 3] = fw[:, ch * 128:(ch + 1) * 128].T
        r = l * BRL
        brow[r + 0:r + 4] = inp["ml_i_bias"][l]
        brow[r + 4:r + 8] = inp["ml_f_bias"][l]
        brow[r + 8:r + 264] = inp["ml_norm_g"][l]
        brow[r + 264:r + 520] = inp["gla_norm_g"][l]
        brow[r + 520:r + 776] = inp["gla_a_bias"][l]
        w2[:, l * 256:(l + 1) * 256] = inp["gla_w_a2"][l]
    brow = np.ascontiguousarray(np.broadcast_to(brow[None, :], (128, DEPTH * BRL)))
    return cT, pvec, brow, w2


def make_consts():
    f = np.float32
    c = np.zeros((128, 1152), f)
    c[:, 0:128] = np.eye(128, dtype=f)
    c[:, 128:256] = 1.0
    s = np.arange(128)[:, None]
    t = np.arange(128)[None, :]
    U = (s <= t).astype(f)
    c[:, 256:384] = U
    c[:, 384:512] = U / 16.0
    c[:, 512:640] = (s > t).astype(f) / 16.0
    c[:, 640:1152] = np.tile(U, (1, 4))
    return c


BIG = ("ada_w", "w_in", "w_ml_out", "w_gla_out", "w_conf_out", "w_sc_out", "w_o", "ffn_w_up", "ffn_w_down")


def make_in_maps(inp, ncores, nseq, ntok):
    consts = make_consts()
    big = {k: np.ascontiguousarray(np.asarray(inp[k], np.float32)) for k in BIG}
    maps = []
    for core in range(ncores):
        cT, pvec, brow, w2 = host_pack(inp, core)
        m = {"x": np.ascontiguousarray(np.asarray(inp["x"][core * 2:core * 2 + nseq, :ntok], np.float32)),
             "cT": cT, "pvec": pvec, "brow": brow, "w2": w2, "consts": consts}
        m.update(big)
        maps.append(m)
    return maps


def kernel(**inputs):
    nc = build()
    maps = make_in_maps(inputs, NCORES, 2, SEQ)
    res = run_bass_kernel_spmd(nc, maps, core_ids=list(range(NCORES)))
    out = np.concatenate([np.asarray(r["out"]) for r in res.results], axis=0)
    return out.astype(np.float32)
```
